# Optimizing a Trainium2 kernel written in Bass

```python
import math
import jax, jax.numpy as jnp
from jax import lax
import numpy as np

D_MODEL = 2048
BATCH = 16
SEQ = 2048
DEPTH = 2

CHUNK = 64
Q_BLOCK = 128
N_BRANCH = 4
BRANCH_WIDTH = D_MODEL // N_BRANCH
GMLP_BLOCK = 128
A_GROUP_W = 128
A_GROUPS = BRANCH_WIDTH // A_GROUP_W
B_HEAD_DIM = 64
B_HEADS = BRANCH_WIDTH // B_HEAD_DIM
C_KDIM = 128
C_VDIM = 128
C_HEADS = BRANCH_WIDTH // C_VDIM
C_FDIM = C_HEADS * C_KDIM
D_HEAD = 64
D_HEADS = BRANCH_WIDTH // (2 * D_HEAD)
D_FF = 5632
CONV_W = 3
ROPE_THETA = 10000.0
EPS = 1e-6

A_COLS = 2 * BRANCH_WIDTH
B_COLS = 3 * BRANCH_WIDTH + B_HEADS
C_COLS = 2 * C_FDIM + 2 * BRANCH_WIDTH
D_COLS = 3 * BRANCH_WIDTH
IN_COLS = A_COLS + B_COLS + C_COLS + D_COLS
IN_SPLITS = [A_COLS, A_COLS + B_COLS, A_COLS + B_COLS + C_COLS]

kernel_name = 'hybrid_chunk_causal_encoder'


def rms_norm(x, g):
    xf = x.astype(jnp.float32)
    y = xf * lax.rsqrt(jnp.mean(xf * xf, axis=-1, keepdims=True) + EPS)
    return (y * g.astype(jnp.float32)).astype(x.dtype)


def layer_norm(x, g, b):
    xf = x.astype(jnp.float32)
    mu = jnp.mean(xf, axis=-1, keepdims=True)
    var = jnp.mean(jnp.square(xf - mu), axis=-1, keepdims=True)
    y = (xf - mu) * lax.rsqrt(var + EPS) * g.astype(jnp.float32) + b.astype(jnp.float32)
    return y.astype(x.dtype)


def split_heads(t, n_heads):
    b, s, _ = t.shape
    return t.reshape(b, s, n_heads, -1).transpose(0, 2, 1, 3)


def merge_heads(t):
    b, h, s, d = t.shape
    return t.transpose(0, 2, 1, 3).reshape(b, s, h * d)


def rope(t, positions):
    half = t.shape[-1] // 2
    inv_freq = ROPE_THETA ** (-jnp.arange(half, dtype=jnp.float32) / half)
    ang = positions.astype(jnp.float32)[:, None] * inv_freq[None, :]
    cos, sin = jnp.cos(ang), jnp.sin(ang)
    tf = t.astype(jnp.float32)
    t1, t2 = tf[..., :half], tf[..., half:]
    return jnp.concatenate([t1 * cos - t2 * sin, t1 * sin + t2 * cos], axis=-1).astype(t.dtype)


def masked_softmax(logits, mask):
    return jax.nn.softmax(jnp.where(mask, logits, -jnp.inf), axis=-1)


def gmlp_spatial_gating(u, v, ln_g, ln_b, w_s, b_s):
    b, s, _ = v.shape
    v = layer_norm(v, ln_g, ln_b)
    vb = v.reshape(b, s // GMLP_BLOCK, GMLP_BLOCK, A_GROUPS, A_GROUP_W)
    cid = jnp.arange(GMLP_BLOCK) // CHUNK
    mask = cid[:, None] >= cid[None, :]
    w = jnp.where(mask[None], w_s, jnp.zeros((), w_s.dtype))
    mixed = jnp.einsum('gts,bnsgc->bntgc', w, vb) + b_s.T[:, :, None]
    return u * mixed.reshape(b, s, A_GROUPS * A_GROUP_W)


def forgetting_attention(q, k, v, log_f):
    s = q.shape[2]
    scale = q.shape[-1] ** -0.5
    c = jnp.cumsum(log_f, axis=-1)
    pos = jnp.arange(s)
    outs = []
    for i in range(s // Q_BLOCK):
        q0, qe = i * Q_BLOCK, (i + 1) * Q_BLOCK
        logits = (jnp.einsum('bhqd,bhkd->bhqk', q[:, :, q0:qe], k[:, :, :qe]).astype(jnp.float32) * scale
                  + c[:, :, q0:qe, None] - c[:, :, None, :qe])
        mask = pos[q0:qe, None] >= pos[None, :qe]
        p = masked_softmax(logits, mask)
        outs.append(jnp.einsum('bhqk,bhkd->bhqd', p.astype(v.dtype), v[:, :, :qe]))
    return jnp.concatenate(outs, axis=2)


def differential_attention(q1, q2, k1, k2, v, lam):
    s = q1.shape[2]
    scale = q1.shape[-1] ** -0.5
    cid = jnp.arange(s) // CHUNK
    outs = []
    for i in range(s // Q_BLOCK):
        q0, qe = i * Q_BLOCK, (i + 1) * Q_BLOCK
        mask = cid[q0:qe, None] >= cid[None, :qe]
        l1 = jnp.einsum('bhqd,bhkd->bhqk', q1[:, :, q0:qe], k1[:, :, :qe]).astype(jnp.float32) * scale
        l2 = jnp.einsum('bhqd,bhkd->bhqk', q2[:, :, q0:qe], k2[:, :, :qe]).astype(jnp.float32) * scale
        p = masked_softmax(l1, mask) - lam * masked_softmax(l2, mask)
        outs.append(jnp.einsum('bhqk,bhkd->bhqd', p.astype(v.dtype), v[:, :, :qe]))
    return jnp.concatenate(outs, axis=2)


def hgrn2_scan(q, k, v, log_f):
    b, h, s, dk = q.shape
    dv = v.shape[-1]
    n = s // CHUNK

    def chunks(t):
        return t.reshape(b, h, n, CHUNK, t.shape[-1]).transpose(2, 0, 1, 3, 4)

    tri = jnp.tril(jnp.ones((CHUNK, CHUNK), dtype=bool))[:, :, None]

    def step(state, inp):
        qc, kc, vc, lfc = inp
        qc = qc.astype(jnp.float32)
        kc = kc.astype(jnp.float32)
        vc = vc.astype(jnp.float32)
        cum = jnp.cumsum(lfc.astype(jnp.float32), axis=2)
        decay = jnp.exp(jnp.where(tri, cum[:, :, :, None, :] - cum[:, :, None, :, :], -jnp.inf))
        scores = jnp.einsum('bhtk,bhsk,bhtsk->bhts', qc, kc, decay)
        out = (jnp.einsum('bhts,bhsv->bhtv', scores, vc)
               + jnp.einsum('bhtk,bhkv->bhtv', qc * jnp.exp(cum), state))
        last = cum[:, :, -1:, :]
        state = (jnp.exp(last[:, :, 0, :])[..., None] * state
                 + jnp.einsum('bhsk,bhsv->bhkv', kc * jnp.exp(last - cum), vc))
        return state, out

    state0 = jnp.zeros((b, h, dk, dv), jnp.float32)
    _, outs = lax.scan(step, state0, (chunks(q), chunks(k), chunks(v), chunks(log_f)))
    return outs.transpose(1, 2, 0, 3, 4).reshape(b, h, s, dv)


def conv_ffn(x, w_up, conv_w, conv_b, w_down):
    h = x @ w_up
    h = lax.conv_general_dilated(h, conv_w[:, None, :], window_strides=(1,), padding=[(CONV_W - 1, 0)],
                                 dimension_numbers=('NWC', 'WIO', 'NWC'),
                                 feature_group_count=h.shape[-1]) + conv_b
    a, g = jnp.split(h, 2, axis=-1)
    return (jax.nn.gelu(a) * g) @ w_down


def setup_inputs(seed: int = 0) -> dict:
    key = jax.random.key(seed)
    ks = jax.random.split(key, 22)

    def nrm(k, shape, scale):
        return jax.random.normal(k, shape, jnp.float32) * scale

    def gain(k, shape):
        return 1.0 + nrm(k, shape, 0.02)

    return {
        'x': nrm(ks[0], (BATCH, SEQ, D_MODEL), 1.0),
        'norm_mix_g': gain(ks[1], (DEPTH, D_MODEL)),
        'w_in': nrm(ks[2], (DEPTH, D_MODEL, IN_COLS), D_MODEL ** -0.5),
        'fox_b_f': nrm(ks[3], (DEPTH, B_HEADS), 0.1),
        'gmlp_ln_g': gain(ks[4], (DEPTH, BRANCH_WIDTH)),
        'gmlp_ln_b': nrm(ks[5], (DEPTH, BRANCH_WIDTH), 0.02),
        'gmlp_w_s': nrm(ks[6], (DEPTH, A_GROUPS, GMLP_BLOCK, GMLP_BLOCK), GMLP_BLOCK ** -0.5),
        'gmlp_b_s': 1.0 + nrm(ks[7], (DEPTH, A_GROUPS, GMLP_BLOCK), 0.02),
        'hgrn_lb_logits': nrm(ks[8], (DEPTH, C_FDIM), 0.1),
        'hgrn_norm_g': gain(ks[9], (DEPTH, C_VDIM)),
        'diff_lambda': nrm(ks[10], (DEPTH, 4, D_HEAD), 0.1),
        'diff_norm_g': gain(ks[11], (DEPTH, 2 * D_HEAD)),
        'w_branch': nrm(ks[12], (DEPTH, N_BRANCH, BRANCH_WIDTH, D_MODEL), BRANCH_WIDTH ** -0.5),
        'w_gate': nrm(ks[13], (DEPTH, N_BRANCH, D_MODEL, D_MODEL), D_MODEL ** -0.5),
        'b_gate': nrm(ks[14], (DEPTH, N_BRANCH, D_MODEL), 0.1),
        'w_out': nrm(ks[15], (DEPTH, D_MODEL, D_MODEL), D_MODEL ** -0.5),
        'norm_ffn_g': gain(ks[16], (DEPTH, D_MODEL)),
        'ffn_w_up': nrm(ks[17], (DEPTH, D_MODEL, 2 * D_FF), D_MODEL ** -0.5),
        'ffn_conv_w': nrm(ks[18], (DEPTH, CONV_W, 2 * D_FF), CONV_W ** -0.5),
        'ffn_conv_b': nrm(ks[19], (DEPTH, 2 * D_FF), 0.02),
        'ffn_w_down': nrm(ks[20], (DEPTH, D_FF, D_MODEL), D_FF ** -0.5),
        'norm_final_g': gain(ks[21], (D_MODEL,)),
    }


def reference(x, norm_mix_g, w_in, fox_b_f, gmlp_ln_g, gmlp_ln_b, gmlp_w_s, gmlp_b_s,
              hgrn_lb_logits, hgrn_norm_g, diff_lambda, diff_norm_g, w_branch, w_gate, b_gate,
              w_out, norm_ffn_g, ffn_w_up, ffn_conv_w, ffn_conv_b, ffn_w_down, norm_final_g):
    b, s, _ = x.shape
    dt = x.dtype
    positions = jnp.arange(s, dtype=jnp.int32)
    lb_p = jax.nn.softmax(hgrn_lb_logits.astype(jnp.float32), axis=0)
    lower_bounds = jnp.cumsum(lb_p, axis=0) - lb_p[0]

    for l in range(DEPTH):
        xn = rms_norm(x, norm_mix_g[l])
        pa, pb, pc, pd = jnp.split(xn @ w_in[l], IN_SPLITS, axis=-1)

        ua, va = jnp.split(jax.nn.gelu(pa), 2, axis=-1)
        y_a = gmlp_spatial_gating(ua, va, gmlp_ln_g[l], gmlp_ln_b[l], gmlp_w_s[l], gmlp_b_s[l])

        bq, bk, bv, bz = jnp.split(pb, [BRANCH_WIDTH, 2 * BRANCH_WIDTH, 3 * BRANCH_WIDTH], axis=-1)
        fox_log_f = jax.nn.log_sigmoid((bz + fox_b_f[l]).astype(jnp.float32)).transpose(0, 2, 1)
        y_b = merge_heads(forgetting_attention(split_heads(bq, B_HEADS), split_heads(bk, B_HEADS),
                                               split_heads(bv, B_HEADS), fox_log_f))

        cq, cz, ci, cg = jnp.split(pc, [C_FDIM, 2 * C_FDIM, 2 * C_FDIM + BRANCH_WIDTH], axis=-1)
        lb = lower_bounds[l]
        cz32 = cz.astype(jnp.float32)
        c_f = lb + (1.0 - lb) * jax.nn.sigmoid(cz32)
        c_log_f = jnp.log(c_f)
        c_k = (1.0 - lb) * jax.nn.sigmoid(-cz32)
        o_c = hgrn2_scan(split_heads(cq * C_KDIM ** -0.5, C_HEADS), split_heads(c_k, C_HEADS),
                         split_heads(ci, C_HEADS), split_heads(c_log_f, C_HEADS)).astype(dt)
        y_c = merge_heads(rms_norm(o_c, hgrn_norm_g[l])) * jax.nn.sigmoid(cg)

        dq, dk, dv = jnp.split(pd, 3, axis=-1)
        dq = dq.reshape(b, s, D_HEADS, 2, D_HEAD).transpose(3, 0, 2, 1, 4)
        dk = dk.reshape(b, s, D_HEADS, 2, D_HEAD).transpose(3, 0, 2, 1, 4)
        lam_init = 0.8 - 0.6 * math.exp(-0.3 * l)
        lam_p = diff_lambda[l].astype(jnp.float32)
        lam = jnp.exp(jnp.sum(lam_p[0] * lam_p[1])) - jnp.exp(jnp.sum(lam_p[2] * lam_p[3])) + lam_init
        o_d = differential_attention(rope(dq[0], positions), rope(dq[1], positions),
                                     rope(dk[0], positions), rope(dk[1], positions),
                                     split_heads(dv, D_HEADS), lam)
        y_d = merge_heads(rms_norm(o_d, diff_norm_g[l]) * (1.0 - lam_init))

        mixed = jnp.zeros_like(x)
        for n, y_br in enumerate((y_a, y_b, y_c, y_d)):
            gate = jax.nn.sigmoid(xn @ w_gate[l, n] + b_gate[l, n])
            mixed = mixed + gate * (y_br @ w_branch[l, n])
        x = x + mixed @ w_out[l]

        x = x + conv_ffn(rms_norm(x, norm_ffn_g[l]), ffn_w_up[l], ffn_conv_w[l], ffn_conv_b[l], ffn_w_down[l])

    return rms_norm(x, norm_final_g)
```

```python
import numpy as np
import ml_dtypes
import concourse.bass as bass
import concourse.mybir as mybir
from concourse.bass_utils import run_bass_kernel_spmd

F32 = mybir.dt.float32
BF16 = mybir.dt.bfloat16
ALU = mybir.AluOpType
AF = mybir.ActivationFunctionType
AX = mybir.AxisListType

D = 2048
T = 2048
DEPTH = 2
BW = 512
DFF = 5632
EPS = 1e-6
A0 = 0
B0 = 1024
C0 = 1024 + 1544
D0 = C0 + 2048
IN_COLS = 6152
NCORES = 8


class Tok:
    __slots__ = ("lw", "rd", "name")

    def __init__(self, name=""):
        self.lw = None
        self.rd = []
        self.name = name


class Op:
    __slots__ = ("eng", "fn", "deps", "sig", "val", "dma", "sem", "lane_wait")


CE = ("pe", "act", "dve", "pool")
FULLSYNC = True
NLANE = 12


class Sched:
    def __init__(self, nc):
        self.nc = nc
        self.sem = {e: nc.alloc_semaphore("c_" + e) for e in CE}
        self.cnt = {e: 0 for e in CE}
        self.lanes = {q: [[nc.alloc_semaphore("l_%s%d" % (q, i)), 0] for i in range(NLANE)] for q in ("sp", "pool")}
        self.rr = {"sp": 0, "pool": 0}
        self.ops = {e: [] for e in ("pe", "act", "dve", "pool", "sp")}
        self.toks = set()
        self.waited = {}
        self.nphase = 0
        self.pool_out = []

    def _deps(self, o, r, w):
        deps = []
        seen = set()

        def add(d, raw):
            if d is None or d is o or id(d) in seen:
                return
            if (not d.dma) and (not o.dma) and d.eng == o.eng and (d.eng == "pe" or (not raw and not FULLSYNC)):
                return
            seen.add(id(d))
            deps.append(d)
            if not d.dma:
                d.sig = True

        for t in r:
            add(t.lw, True)
        for t in w:
            add(t.lw, False)
            for d in t.rd:
                add(d, False)
        o.deps = deps
        for t in r:
            t.rd.append(o)
            self.toks.add(t)
        for t in w:
            t.lw = o
            t.rd = []
            self.toks.add(t)

    def op(self, eng, fn, r=(), w=()):
        o = Op()
        o.eng = eng
        o.fn = fn
        o.dma = False
        o.sig = False
        o.val = None
        o.sem = None
        o.lane_wait = 0
        self._deps(o, r, w)
        self.ops[eng].append(o)
        return o

    def dma(self, q, fn, r=(), w=(), ndesc=0):
        o = Op()
        o.eng = q
        o.fn = fn
        o.dma = True
        o.sig = True
        extra = []
        if q == "pool" and ndesc:
            while self.pool_out and sum(n for _, n in self.pool_out) + ndesc > 640:
                extra.append(self.pool_out.pop(0)[0])
            self.pool_out.append((o, ndesc))
        lane = self.lanes[q][self.rr[q]]
        self.rr[q] = (self.rr[q] + 1) % NLANE
        o.lane_wait = 16 * lane[1]
        lane[1] += 1
        o.sem = lane[0]
        o.val = 16 * lane[1]
        self._deps(o, r, w)
        for d in extra:
            if d not in o.deps:
                o.deps.append(d)
        self.ops[q].append(o)
        return o

    def flush(self):
        self.pool_out = []
        for e in CE:
            c = self.cnt[e]
            for o in self.ops[e]:
                if (not o.dma) and o.sig:
                    c += 1
                    o.val = c
            self.cnt[e] = c
        ops = self.ops
        self.ops = {e: [] for e in ("pe", "act", "dve", "pool", "sp")}
        waited = self.waited
        sems = self.sem
        lanes = self.lanes

        def emit(e, name):
            def w8(sem, val):
                key = (name, sem.num)
                if waited.get(key, 0) >= val:
                    return
                e.wait_ge(sem, val)
                waited[key] = val

            for o in ops[name]:
                for d in o.deps:
                    if d.dma:
                        w8(d.sem, d.val)
                    else:
                        w8(sems[d.eng], d.val)
                if o.dma and o.lane_wait:
                    w8(o.sem, o.lane_wait)
                ins = o.fn(e)
                if o.dma:
                    ins.then_inc(o.sem, 16)
                elif o.sig:
                    ins.then_inc(sems[o.eng], 1)
            if name in lanes:
                for sem, cnt in lanes[name]:
                    if cnt:
                        w8(sem, 16 * cnt)

        with self.nc.Block() as blk:
            @blk.sync
            def _(e):
                emit(e, "sp")

            @blk.gpsimd
            def _(e):
                emit(e, "pool")

            @blk.tensor
            def _(e):
                emit(e, "pe")

            @blk.scalar
            def _(e):
                emit(e, "act")

            @blk.vector
            def _(e):
                emit(e, "dve")
        for t in self.toks:
            t.lw = None
            t.rd = []
        self.toks = set()
        self.nphase += 1


class Ring:
    def __init__(self, items):
        self.items = [(t, Tok()) for t in items]
        self.i = 0

    def next(self):
        it = self.items[self.i]
        self.i = (self.i + 1) % len(self.items)
        return it


def mm(S, out_ap, pairs, r, w, start=True, stop=True):
    def fn(e, pairs=pairs, out_ap=out_ap, start=start, stop=stop):
        n = len(pairs)
        ins = None
        for i, (l, rh) in enumerate(pairs):
            ins = e.matmul(out_ap, l, rh, start=(start and i == 0), stop=(stop and i == n - 1))
        return ins
    return S.op("pe", fn, r=r, w=w)


from contextlib import ExitStack

_uid = [0]


def build_nc(NS=2, NLAY=2, dbg=False, STOP=99):
    nc = bass.Bass("TRN2", target_bir_lowering=False)

    def din(name, shape, dt=F32):
        return nc.dram_tensor(name, list(shape), dt, kind="ExternalInput").ap()

    x_in = din("x", [NS, T, D])
    norm_mix_g = din("norm_mix_g", [DEPTH, D])
    w_in = din("w_in", [DEPTH, D, IN_COLS])
    fox_b_f = din("fox_b_f", [DEPTH, 8])
    gmlp_ln_g = din("gmlp_ln_g", [DEPTH, BW])
    gmlp_ln_b = din("gmlp_ln_b", [DEPTH, BW])
    gmlp_w_sT = din("gmlp_w_sT", [DEPTH, 4, 128, 128])
    gmlp_b_s = din("gmlp_b_s", [DEPTH, 512])
    hgrn_lb = din("hgrn_lb_logits", [DEPTH, 512])
    hgrn_norm_g = din("hgrn_norm_g", [DEPTH, 128])
    diff_lambda = din("diff_lambda", [DEPTH, 256])
    diff_norm_g = din("diff_norm_g", [DEPTH, 128])
    w_branch = din("w_branch", [DEPTH, 4, BW, D])
    w_gate = din("w_gate", [DEPTH, 4, D, D])
    b_gate = din("b_gate", [DEPTH, 4, D])
    w_out = din("w_out", [DEPTH, D, D])
    norm_ffn_g = din("norm_ffn_g", [DEPTH, D])
    ffn_w_up = din("ffn_w_up", [DEPTH, D, 2 * DFF])
    ffn_conv_w = din("ffn_conv_w", [DEPTH, 3, 2 * DFF])
    ffn_conv_b = din("ffn_conv_b", [DEPTH, 2 * DFF])
    ffn_w_down = din("ffn_w_down", [DEPTH, DFF, D])
    norm_final_g = din("norm_final_g", [D])
    c_ident = din("c_ident", [128, 128], BF16)
    c_cos = din("c_cos", [128, T])
    c_sinA = din("c_sinA", [128, T])
    c_tri = din("c_tri", [128, 64])
    c_mk = din("c_mk", [128, 2])
    c_identf = din("c_identf", [128, 128])
    c_rm = din("c_rm", [128, 2])
    c_tri2 = din("c_tri2", [128, 2, 64])

    out = nc.dram_tensor("out", [NS, T, D], F32, kind="ExternalOutput").ap()
    okind = "ExternalOutput" if dbg else "Internal"

    def dscr(name, shape, dt):
        if dbg:
            return nc.dram_tensor(name, list(shape), dt, kind="ExternalOutput").ap()
        return nc.dram_tensor(name, list(shape), dt).ap()

    yT_d = dscr("yT_d", [D, T], BF16)
    mixT_d = dscr("mixT_d", [D, T], BF16)
    xa_d = dscr("xa_d", [T, D], F32)
    xb_d = dscr("xb_d", [T, D], F32)
    xc_d = dscr("xc_d", [T, D], F32)
    actT_d = dscr("actT_d", [DFF, T], BF16)
    ex_d = dscr("ex_d", [2, 8, 6, T], BF16)

    S = Sched(nc)

    def sb(k, shape, dt=F32):
        _uid[0] += 1
        return k.enter_context(nc.sbuf_tensor("s%d" % _uid[0], list(shape), dt))

    def pst(k, shape, dt=F32):
        _uid[0] += 1
        return k.enter_context(nc.psum_tensor("p%d" % _uid[0], list(shape), dt))

    def dma(q, out_ap, in_ap, r=(), w=(), slow=False, ndesc=0):
        if slow:
            return S.dma(q, lambda e, o=out_ap, i=in_ap: e.dma_start(out=o, in_=i, allow_slow_non_contiguous=True), r=r, w=w, ndesc=ndesc)
        return S.dma(q, lambda e, o=out_ap, i=in_ap: e.dma_start(out=o, in_=i), r=r, w=w, ndesc=ndesc)

    def act(out_ap, in_ap, func, r, w, **kw):
        return S.op("act", lambda e, o=out_ap, i=in_ap, f=func, kw=kw: e.activation(out=o, in_=i, func=f, **kw), r=r, w=w)

    def tt(eng, out_ap, a, b, op, r, w):
        return S.op(eng, lambda e, o=out_ap, a=a, b=b, op=op: e.tensor_tensor(out=o, in0=a, in1=b, op=op), r=r, w=w)

    def ts(eng, out_ap, a, s1, s2, op0, op1, r, w):
        if op1 is None:
            return S.op(eng, lambda e, o=out_ap, a=a, s1=s1, op0=op0: e.tensor_scalar(out=o, in0=a, scalar1=s1, scalar2=None, op0=op0), r=r, w=w)
        return S.op(eng, lambda e, o=out_ap, a=a, s1=s1, s2=s2, op0=op0, op1=op1: e.tensor_scalar(out=o, in0=a, scalar1=s1, scalar2=s2, op0=op0, op1=op1), r=r, w=w)

    def stt(out_ap, a, sc, b, op0, op1, r, w):
        return S.op("dve", lambda e, o=out_ap, a=a, sc=sc, b=b, op0=op0, op1=op1: e.scalar_tensor_tensor(out=o, in0=a, scalar=sc, in1=b, op0=op0, op1=op1), r=r, w=w)

    def recip(out_ap, in_ap, r, w):
        return S.op("dve", lambda e, o=out_ap, i=in_ap: e.reciprocal(out=o, in_=i), r=r, w=w)

    def cp(eng, out_ap, in_ap, r, w):
        return S.op(eng, lambda e, o=out_ap, i=in_ap: e.tensor_copy(out=o, in_=i), r=r, w=w)

    def memset(eng, ap, v, w):
        return S.op(eng, lambda e, ap=ap, v=v: e.memset(ap, v), w=w)

    G = ExitStack()
    with G:
        ident = sb(G, [128, 128], BF16); t_ident = Tok()
        ones_bf = sb(G, [128, 128], BF16); t_ones = Tok()
        epst = sb(G, [128, 1]); t_eps = Tok()
        dma("sp", ident[:], c_ident, w=[t_ident])
        memset("dve", ones_bf[:], 1.0, [t_ones])
        memset("dve", epst[:], EPS, [t_eps])
        CONST_R = [t_ident, t_ones, t_eps]

        def load_w(ring, wap, c0, ncol, KC=16):
            wt, tw = ring.next()
            dma("pool", wt[:, 0:KC, 0:ncol], wap[:, c0:c0 + ncol].rearrange("(kc p) n -> p kc n", p=128), w=[tw], ndesc=KC * 8)
            return wt, tw

        def proj_fm(xT, xtok, wt, tw, col_off, M, KC, psr, evac):
            for tti in range(4):
                p, tp = psr.next()
                pairs = [(wt[:, kc, col_off:col_off + M], xT[:, kc, tti * 512:(tti + 1) * 512]) for kc in range(KC)]
                mm(S, p[0:M, :], pairs, r=[xtok, tw], w=[tp])
                evac(tti, p, tp)

        def proj_tm(xT, xtok, wt, tw, ncol, KC, psr, evac):
            for tc in range(16):
                p, tp = psr.next()
                pairs = [(xT[:, kc, tc * 128:(tc + 1) * 128], wt[:, kc, 0:ncol]) for kc in range(KC)]
                mm(S, p[:, 0:ncol], pairs, r=[xtok, tw], w=[tp])
                evac(tc, p, tp)

        def phase_norm(src, gvec, xnT, xtok):
            with ExitStack() as k:
                Gt = sb(k, [128, D]); tG = Tok()
                xin = Ring([sb(k, [128, D]) for _ in range(3)])
                xs = Ring([sb(k, [128, D], BF16) for _ in range(3)])
                junk = sb(k, [128, D], BF16); tj = Tok()
                st = Ring([sb(k, [128, 4]) for _ in range(3)])
                ptr = Ring([pst(k, [128, 1024], BF16) for _ in range(4)])
                dma("sp", Gt[:], gvec.partition_broadcast(128), w=[tG])
                def stage_a(tc):
                    xi, txi = xin.next()
                    xo, txo = xs.next()
                    sv, tsv = st.next()
                    dma("sp", xi[:], src[tc * 128:(tc + 1) * 128, :], w=[txi])
                    act(junk[:], xi[:], AF.Square, [txi], [tj, tsv], accum_out=sv[:, 0:1])
                    act(sv[:, 1:2], sv[:, 0:1], AF.Sqrt, [tsv, t_eps], [tsv], scale=1.0 / D, bias=epst[:])
                    recip(sv[:, 2:3], sv[:, 1:2], [tsv], [tsv])
                    stt(xo[:], xi[:], sv[:, 2:3], Gt[:], ALU.mult, ALU.mult, [txi, tsv, tG], [txo])
                    return xo, txo

                def stage_b(tc, xo, txo):
                    for half in range(2):
                        p, tp = ptr.next()

                        def fn(e, p=p, xo=xo, half=half):
                            ins = None
                            for j in range(8):
                                c = (half * 8 + j) * 128
                                ins = e.transpose(out=p[:, j * 128:(j + 1) * 128], in_=xo[:, c:c + 128], identity=ident[:])
                            return ins
                        S.op("pe", fn, r=[txo, t_ident], w=[tp])
                        act(xnT[:, half * 8:(half + 1) * 8, tc * 128:(tc + 1) * 128],
                            p[:].rearrange("p (k t) -> p k t", k=8), AF.Copy, [tp], [xtok])
                cur = stage_a(0)
                for tc in range(16):
                    nxt = stage_a(tc + 1) if tc + 1 < 16 else None
                    stage_b(tc, *cur)
                    cur = nxt
                S.flush()

        def rms_part(k_sq, o_ap, ncol, psr, r_o, rs_t, t_rs):
            sq, tsq = k_sq
            act(sq[:, 0:ncol], o_ap, AF.Square, r_o, [tsq])
            p, tp = psr.next()
            mm(S, p[:, 0:ncol], [(ones_bf[:], sq[:, 0:ncol])], r=[tsq, t_ones], w=[tp])
            act(rs_t[:, 0:ncol], p[:, 0:ncol], AF.Sqrt, [tp, t_eps], [t_rs], scale=1.0 / 128, bias=epst[:])
            recip(rs_t[:, 0:ncol], rs_t[:, 0:ncol], [t_rs], [t_rs])

        def layer(l, src_rows, dst_rows):
            win = w_in[l]
            LK = ExitStack()
            with LK:
                xnT = sb(LK, [128, 16, T], BF16); xtok = Tok()
                phase_norm(src_rows, norm_mix_g[l], xnT, xtok)
                if STOP >= 2: mixer_a(l, win, xnT, xtok)
                if STOP >= 3: mixer_b(l, win, xnT, xtok)
                if STOP >= 4: mixer_c(l, win, xnT, xtok)
                if STOP >= 5: mixer_d(l, win, xnT, xtok)
                if STOP >= 6: phase_gate(l, xnT, xtok)
            if STOP >= 7: phase_wout(l, src_rows)
            if STOP >= 8:
                with ExitStack() as k2:
                    hnT = sb(k2, [128, 16, T], BF16); htok = Tok()
                    phase_norm(xa_d, norm_ffn_g[l], hnT, htok)
                    phase_ffn_up(l, hnT, htok)
            if STOP >= 9: phase_ffn_down(l, dst_rows)

        def mixer_a(l, win, xnT, xtok):
            with ExitStack() as k:
                wr = Ring([sb(k, [128, 16, 512], BF16) for _ in range(2)])
                uaT = sb(k, [128, 4, T]); t_ua = Tok()
                yaT = sb(k, [128, 4, T], BF16); t_ya = Tok()
                WsT = sb(k, [128, 4, 128], BF16); t_ws = Tok()
                Wsf = sb(k, [128, 4, 128]); t_wsf = Tok()
                BS = sb(k, [128, 512]); t_bs = Tok()
                Gl = sb(k, [128, 512]); t_gl = Tok()
                Bl = sb(k, [128, 512]); t_bl = Tok()
                vg = Ring([sb(k, [128, 512]) for _ in range(2)])
                vc = Ring([sb(k, [128, 512]) for _ in range(2)])
                vj = sb(k, [128, 512], BF16); t_vj = Tok()
                vn = Ring([sb(k, [128, 512], BF16) for _ in range(4)])
                sv_r = Ring([sb(k, [128, 8]) for _ in range(2)])
                mx = Ring([sb(k, [128, 512]) for _ in range(2)])
                psr = Ring([pst(k, [128, 512]) for _ in range(6)])
                dma("sp", Wsf[:], gmlp_w_sT[l].rearrange("g s t -> s g t"), w=[t_wsf])
                memset("pool", Wsf[64:128, :, 0:64], 0.0, [t_wsf])
                cp("pool", WsT[:], Wsf[:], [t_wsf], [t_ws])
                dma("sp", BS[:], gmlp_b_s[l].partition_broadcast(128), w=[t_bs])
                dma("sp", Gl[:], gmlp_ln_g[l].partition_broadcast(128), w=[t_gl])
                dma("sp", Bl[:], gmlp_ln_b[l].partition_broadcast(128), w=[t_bl])
                wt, tw = load_w(wr, win, A0, 512)
                for oc in range(4):
                    def ev(tti, p, tp, oc=oc):
                        act(uaT[:, oc, tti * 512:(tti + 1) * 512], p[:], AF.Gelu_apprx_tanh, [tp], [t_ua])
                    proj_fm(xnT, xtok, wt, tw, oc * 128, 128, 16, psr, ev)
                wt2, tw2 = load_w(wr, win, A0 + 512, 512)

                def ev2(tc, p, tp):
                    g_, tg_ = vg.next()
                    c_, tc_ = vc.next()
                    n_, tn_ = vn.next()
                    sv, tsv = sv_r.next()
                    act(g_[:], p[:], AF.Gelu_apprx_tanh, [tp], [tg_, tsv], accum_out=sv[:, 0:1])
                    ts("dve", sv[:, 1:2], sv[:, 0:1], 1.0 / 512, None, ALU.mult, None, [tsv], [tsv])
                    ts("dve", c_[:], g_[:], sv[:, 1:2], None, ALU.subtract, None, [tg_, tsv], [tc_])
                    act(vj[:], c_[:], AF.Square, [tc_], [t_vj, tsv], accum_out=sv[:, 2:3])
                    act(sv[:, 3:4], sv[:, 2:3], AF.Sqrt, [tsv, t_eps], [tsv], scale=1.0 / 512, bias=epst[:])
                    recip(sv[:, 4:5], sv[:, 3:4], [tsv], [tsv])
                    stt(c_[:], c_[:], sv[:, 4:5], Gl[:], ALU.mult, ALU.mult, [tc_, tsv, t_gl], [tc_])
                    tt("dve", n_[:], c_[:], Bl[:], ALU.add, [tc_, t_bl], [tn_])

                    def stage_b(tc=tc, n_=n_, tn_=tn_):
                        m_, tm_ = mx.next()
                        p2, tp2 = psr.next()

                        def fn(e, p2=p2, n_=n_):
                            ins = None
                            for g in range(4):
                                ins = e.matmul(p2[:, g * 128:(g + 1) * 128], n_[:, g * 128:(g + 1) * 128], WsT[:, g, :], start=True, stop=True)
                            return ins
                        S.op("pe", fn, r=[tn_, t_ws], w=[tp2])
                        tt("dve", m_[:], p2[:], BS[:], ALU.add, [tp2, t_bs], [tm_])
                        tt("dve", yaT[:, :, tc * 128:(tc + 1) * 128], m_[:].rearrange("p (g t) -> p g t", g=4),
                           uaT[:, :, tc * 128:(tc + 1) * 128], ALU.mult, [tm_, t_ua], [t_ya])
                    pendA.append(stage_b)
                    if len(pendA) > 2:
                        pendA.pop(0)()
                pendA = []
                proj_tm(xnT, xtok, wt2, tw2, 512, 16, psr, ev2)
                while pendA:
                    pendA.pop(0)()
                dma("sp", yT_d[0:512, :].rearrange("(g p) t -> p g t", p=128), yaT[:], r=[t_ya])
                S.flush()

        def mixer_b(l, win, xnT, xtok):
            with ExitStack() as k:
                wr = Ring([sb(k, [128, 16, 128], BF16) for _ in range(6)])
                wz = sb(k, [128, 16, 8], BF16); t_wz = Tok()
                psr = Ring([pst(k, [128, 512]) for _ in range(3)])
                with ExitStack() as k1:
                    cn = sb(k1, [8, T]); t_cn = Tok()
                    ex = sb(k1, [8, T]); t_ex = Tok()
                    rr_ = sb(k1, [8, T]); t_rr = Tok()
                    onesf = sb(k1, [8, T]); t_of = Tok()
                    nbf = sb(k1, [8, 2]); t_nbf = Tok()
                    cbs = [sb(k1, [8, T], BF16) for _ in range(3)]; t_cb = [Tok() for _ in range(3)]
                    nbs = [sb(k1, [8, T], BF16) for _ in range(3)]; t_nb = [Tok() for _ in range(3)]
                    onesb = sb(k1, [8, T], BF16); t_ob = Tok()
                    dma("pool", wz[:], win[:, B0 + 1536:B0 + 1544].rearrange("(kc p) n -> p kc n", p=128), w=[t_wz], ndesc=128)
                    dma("sp", nbf[:, 0:1], fox_b_f[l].rearrange("(h o) -> h o", o=1), w=[t_nbf])
                    ts("dve", nbf[:, 1:2], nbf[:, 0:1], -1.0, None, ALU.mult, None, [t_nbf], [t_nbf])
                    memset("pool", onesf[:], 1.0, [t_of])
                    memset("pool", onesb[:], 1.0, [t_ob])
                    for tti in range(4):
                        p, tp = psr.next()
                        pairs = [(wz[:, kc, :], xnT[:, kc, tti * 512:(tti + 1) * 512]) for kc in range(16)]
                        mm(S, p[0:8, :], pairs, r=[xtok, t_wz], w=[tp])
                        act(ex[:, tti * 512:(tti + 1) * 512], p[0:8, :], AF.Exp, [tp, t_nbf], [t_ex], scale=-1.0, bias=nbf[:, 1:2])
                    act(ex[:], ex[:], AF.Ln, [t_ex], [t_ex], bias=1.0)
                    S.op("dve", lambda e: e.tensor_tensor_scan(out=cn[:], data0=onesf[:], data1=ex[:], initial=0.0, op0=ALU.mult, op1=ALU.add),
                         r=[t_ex, t_of], w=[t_cn])
                    cp("dve", cbs[0][:], cn[:], [t_cn], [t_cb[0]])
                    tt("dve", rr_[:], cn[:], cbs[0][:], ALU.subtract, [t_cn, t_cb[0]], [t_rr])
                    cp("dve", cbs[1][:], rr_[:], [t_rr], [t_cb[1]])
                    tt("dve", rr_[:], rr_[:], cbs[1][:], ALU.subtract, [t_rr, t_cb[1]], [t_rr])
                    cp("dve", cbs[2][:], rr_[:], [t_rr], [t_cb[2]])
                    for j in range(3):
                        ts("dve", nbs[j][:], cbs[j][:], -1.0, None, ALU.mult, None, [t_cb[j]], [t_nb[j]])
                        dma("sp", ex_d[1, :, 3 + j, :], cbs[j][:], r=[t_cb[j]])
                        dma("sp", ex_d[0, :, j, :], nbs[j][:], r=[t_nb[j]])
                        dma("sp", ex_d[1, :, j, :], onesb[:], r=[t_ob])
                        dma("sp", ex_d[0, :, 3 + j, :], onesb[:], r=[t_ob])
                    S.flush()
                qh = [sb(k, [128, T], BF16) for _ in range(2)]; t_qh = [Tok(), Tok()]; t_qx = [Tok(), Tok()]; t_kx = [Tok(), Tok()]
                kh = [sb(k, [128, T], BF16) for _ in range(2)]; t_kh = [Tok(), Tok()]
                vaug = sb(k, [128, 16, 2, 128], BF16); t_v = Tok()
                ybT = Ring([sb(k, [128, T], BF16) for _ in range(2)])
                PT = Ring([sb(k, [128, 512], BF16) for _ in range(3)])
                rz = Ring([sb(k, [64, 512]) for _ in range(2)])
                pso = Ring([pst(k, [128, 512]) for _ in range(4)])
                for hl_ in range(2):
                    memset("pool", vaug[:, :, hl_, 64:128], 1.0, [t_v])
                def ldb(hp_):
                    return (load_w(wr, win, B0 + hp_ * 128, 128), load_w(wr, win, B0 + 512 + hp_ * 128, 128),
                            load_w(wr, win, B0 + 1024 + hp_ * 128, 128))
                nxtw = ldb(0)
                for hp in range(4):
                    yb, t_yb = ybT.next()
                    for hl in range(2):
                        h = hp * 2 + hl
                        dma("sp", qh[hl][64:70, :], ex_d[0, h], w=[t_qx[hl]])
                        dma("sp", kh[hl][64:70, :], ex_d[1, h], w=[t_kx[hl]])
                    (wq, twq), (wk, twk), (wv, twv) = nxtw
                    if hp + 1 < 4:
                        nxtw = ldb(hp + 1)

                    def evq(tti, p, tp):
                        for hl in range(2):
                            act(qh[hl][0:64, tti * 512:(tti + 1) * 512], p[hl * 64:(hl + 1) * 64, :], AF.Copy, [tp], [t_qh[hl]], scale=0.125)

                    def evk(tti, p, tp):
                        for hl in range(2):
                            cp("dve", kh[hl][0:64, tti * 512:(tti + 1) * 512], p[hl * 64:(hl + 1) * 64, :], [tp], [t_kh[hl]])

                    def evv(tc, p, tp):
                        act(vaug[:, tc, :, 0:64], p[:, 0:128].rearrange("p (h d) -> p h d", h=2), AF.Copy, [tp], [t_v])
                    proj_fm(xnT, xtok, wq, twq, 0, 128, 16, psr, evq)
                    proj_fm(xnT, xtok, wk, twk, 0, 128, 16, psr, evk)
                    proj_tm(xnT, xtok, wv, twv, 128, 16, psr, evv)
                    for hl in range(2):
                        for i in range(4):
                            po, tpo = pso.next()
                            nj = 4 * i + 4

                            def s_stage(j, i=i, hl=hl):
                                t0 = max(i * 512, j * 128)
                                ncol = (i + 1) * 512 - t0
                                c0 = t0 - i * 512
                                p, tp = psr.next()
                                mm(S, p[:, 0:ncol], [(kh[hl][0:70, j * 128:(j + 1) * 128], qh[hl][0:70, t0:t0 + ncol])],
                                   r=[t_kh[hl], t_qh[hl], t_kx[hl], t_qx[hl]], w=[tp])
                                pt_, tpt = PT.next()
                                act(pt_[:, 0:ncol], p[:, 0:ncol], AF.Exp, [tp], [tpt])
                                if j >= 4 * i:
                                    S.op("pool", lambda e, pt_=pt_: e.affine_select(out=pt_[:, 0:128], in_=pt_[:, 0:128], pattern=[[1, 128]],
                                                                                   compare_op=ALU.is_ge, fill=0.0, base=0, channel_multiplier=-1),
                                         r=[tpt], w=[tpt])
                                return (j, pt_, tpt, c0, ncol)

                            def pv_stage(st_, hl=hl, po=po, tpo=tpo, nj=nj):
                                j, pt_, tpt, c0, ncol = st_
                                mm(S, po[:, c0:c0 + ncol], [(vaug[:, j, hl, :], pt_[:, 0:ncol])],
                                   r=[t_v, tpt], w=[tpo], start=(j == 0), stop=(j == nj - 1))
                            prev = None
                            for j in range(nj):
                                cur = s_stage(j)
                                if prev is not None:
                                    pv_stage(prev)
                                prev = cur
                            pv_stage(prev)
                            rz_, trz = rz.next()
                            act(rz_[:], po[64:128, :], AF.Copy, [tpo], [trz])
                            recip(rz_[:], rz_[:], [trz], [trz])
                            tt("dve", yb[hl * 64:(hl + 1) * 64, i * 512:(i + 1) * 512], po[0:64, :], rz_[:], ALU.mult, [tpo, trz], [t_yb])
                    dma("sp", yT_d[512 + hp * 128:512 + (hp + 1) * 128, :], yb[:], r=[t_yb])
                S.flush()

        def mixer_c(l, win, xnT, xtok):
            with ExitStack() as k:
                wr = Ring([sb(k, [128, 16, 128], BF16) for _ in range(8)])
                psr = Ring([pst(k, [128, 512]) for _ in range(2)])
                ptr = Ring([pst(k, [128, 1024], BF16) for _ in range(1)])
                pmy = pst(k, [128, 512])
                psA = Ring([pst(k, [128, 512])[:, 0:128]])
                psU = Ring([pst(k, [128, 512])[:, 0:128] for _ in range(2)])
                psO = Ring([pmy[:, 0:128], pst(k, [128, 512])[:, 0:128]])
                t_pmy = psO.items[0][1]
                lbt = sb(k, [128, 16]); t_lb = Tok()
                gn = sb(k, [128, 1]); t_gn = Tok()
                onesf = sb(k, [128, T]); t_of = Tok()
                qf = sb(k, [128, T]); t_qf = Tok()
                bA = sb(k, [128, T]); t_A = Tok()
                bB = sb(k, [128, T]); t_B = Tok()
                bG = sb(k, [128, T]); t_G = Tok()
                dd = sb(k, [128, 32]); t_dd = Tok()
                qtil = sb(k, [128, T], BF16); t_qt = Tok()
                ktil = sb(k, [128, T], BF16); t_kt = Tok()
                ktok = [sb(k, [128, 16, 128], BF16) for _ in range(2)]; t_ktok = [Tok(), Tok()]
                vtoks = [(sb(k, [128, 16, 128], BF16), Tok()) for _ in range(2)]
                gates = [(sb(k, [128, T], BF16), Tok()) for _ in range(2)]
                oT = sb(k, [128, T]); t_oT = Tok()
                AT = Ring([sb(k, [128, 2, 64], BF16) for _ in range(2)])
                Tst = sb(k, [128, 128]); t_T = Tok()
                Sb = Ring([sb(k, [128, 128], BF16) for _ in range(2)])
                sq = (sb(k, [128, 512], BF16), Tok())
                rs_t = sb(k, [128, 512]); t_rs = Tok()
                t1 = sb(k, [128, 512]); t_t1 = Tok()
                ycT = Ring([sb(k, [128, T], BF16) for _ in range(2)])
                lbin = sb(k, [8, 128]); t_lbin = Tok()
                identf = sb(k, [128, 128]); t_idf = Tok()
                rm = sb(k, [128, 2]); t_rm = Tok()
                tri2 = sb(k, [128, 2, 64]); t_tri = Tok()
                dma("sp", identf[:], c_identf, w=[t_idf])
                dma("sp", rm[:], c_rm, w=[t_rm])
                dma("sp", tri2[:], c_tri2, w=[t_tri])
                dma("sp", lbin[:], hgrn_lb.rearrange("l (c p) -> (l c) p", p=128), w=[t_lbin])
                S.op("pe", lambda e: e.transpose(out=pmy[:, 256:264], in_=lbin[:], identity=identf[0:8, 0:8]), r=[t_lbin, t_idf], w=[t_pmy])
                cp("dve", lbt[:, 0:8], pmy[:, 256:264], [t_pmy], [t_lb])
                dma("sp", gn[:], hgrn_norm_g[l].rearrange("(p o) -> p o", o=1), w=[t_gn])
                memset("pool", onesf[:], 1.0, [t_of])
                if l == 0:
                    memset("dve", lbt[:, 8:12], 0.0, [t_lb])
                else:
                    tt("dve", lbt[:, 8:12], lbt[:, 4:8], lbt[:, 0:4], ALU.subtract, [t_lb], [t_lb])
                    act(lbt[:, 8:12], lbt[:, 8:12], AF.Sigmoid, [t_lb], [t_lb])
                ts("dve", lbt[:, 12:16], lbt[:, 8:12], -1.0, 1.0, ALU.mult, ALU.add, [t_lb], [t_lb])
                def ldc(h_):
                    return (load_w(wr, win, C0 + h_ * 128, 128), load_w(wr, win, C0 + 512 + h_ * 128, 128),
                            load_w(wr, win, C0 + 1024 + h_ * 128, 128), load_w(wr, win, C0 + 1536 + h_ * 128, 128))
                def do_proj(h_, wts):
                    (wq, twq), (wzz, twz), (wi, twi), (wg, twg) = wts
                    vt_, tv_ = vtoks[h_ % 2]
                    gt_, tg_ = gates[h_ % 2]

                    def evq(tti, p, tp):
                        act(qf[:, tti * 512:(tti + 1) * 512], p[:], AF.Copy, [tp], [t_qf], scale=128 ** -0.5)

                    def evz(tti, p, tp):
                        act(bA[:, tti * 512:(tti + 1) * 512], p[:], AF.Sigmoid, [tp], [t_A])

                    def evv(tc, p, tp):
                        cp("dve", vt_[:, tc, :], p[:, 0:128], [tp], [tv_])

                    def evg(tti, p, tp):
                        act(gt_[:, tti * 512:(tti + 1) * 512], p[:], AF.Sigmoid, [tp], [tg_])
                    proj_fm(xnT, xtok, wq, twq, 0, 128, 16, psr, evq)
                    proj_fm(xnT, xtok, wzz, twz, 0, 128, 16, psr, evz)
                    proj_tm(xnT, xtok, wi, twi, 128, 16, psr, evv)
                    proj_fm(xnT, xtok, wg, twg, 0, 128, 16, psr, evg)
                wts_all = {0: ldc(0)}
                do_proj(0, wts_all[0])
                wts_all[1] = ldc(1)
                for h in range(4):
                    yc, t_yc = ycT.next()
                    vtok, t_v = vtoks[h % 2]
                    gate, t_gate = gates[h % 2]
                    ts("dve", bA[:], bA[:], lbt[:, 12 + h:13 + h], lbt[:, 8 + h:9 + h], ALU.mult, ALU.add, [t_A, t_lb], [t_A])
                    act(bB[:], bA[:], AF.Ln, [t_A], [t_B])
                    ts("dve", bA[:], bA[:], -1.0, 1.0, ALU.mult, ALU.add, [t_A], [t_A])
                    S.op("dve", lambda e: e.tensor_tensor_scan(out=bG[:], data0=onesf[:], data1=bB[:], initial=0.0, op0=ALU.mult, op1=ALU.add),
                         r=[t_B, t_of], w=[t_G])
                    G3 = bG[:].rearrange("p (c t) -> p c t", t=64)
                    E3 = bB[:].rearrange("p (c t) -> p c t", t=64)
                    tt("dve", E3, G3, G3[:, :, 31:32].broadcast_to([128, 32, 64]), ALU.subtract, [t_G], [t_B])
                    tt("dve", dd[:, 0:31], G3[:, 1:32, 31], G3[:, 0:31, 31], ALU.subtract, [t_G], [t_dd])
                    act(dd[:, 0:31], dd[:, 0:31], AF.Exp, [t_dd], [t_dd])
                    act(bG[:], bB[:], AF.Exp, [t_B], [t_G])
                    act(bB[:], bB[:], AF.Exp, [t_B], [t_B], scale=-1.0)
                    tt("dve", qtil[:], qf[:], bG[:], ALU.mult, [t_qf, t_G], [t_qt])
                    tt("dve", ktil[:], bA[:], bB[:], ALU.mult, [t_A, t_B], [t_kt])
                    if h + 1 < 4:
                        do_proj(h + 1, wts_all[h + 1])
                        if h + 2 < 4:
                            wts_all[h + 2] = ldc(h + 2)
                    for half in range(2):
                        p, tp = ptr.next()

                        def fn(e, p=p, half=half):
                            ins = None
                            for j in range(8):
                                c = (half * 8 + j) * 128
                                ins = e.transpose(out=p[:, j * 128:(j + 1) * 128], in_=ktil[:, c:c + 128], identity=ident[:])
                            return ins
                        S.op("pe", fn, r=[t_kt, t_ident], w=[tp])
                        for hf in range(2):
                            act(ktok[hf][:, half * 8:(half + 1) * 8, :], p[:].rearrange("p (k t) -> p k t", k=8), AF.Copy, [tp, t_rm], [t_ktok[hf]],
                                scale=rm[:, hf:hf + 1])
                    sbc = None
                    for tc in range(16):
                        pa, tpa = psA.next()
                        at, tat = AT.next()
                        for hf in range(2):
                            c = 2 * tc + hf
                            mm(S, pa[:, hf * 64:(hf + 1) * 64], [(ktil[:, tc * 128:(tc + 1) * 128], qtil[:, c * 64:(c + 1) * 64])],
                               r=[t_kt, t_qt], w=[tpa])
                        tt("dve", at[:], pa[:].rearrange("p (h t) -> p h t", h=2), tri2[:], ALU.mult, [tpa, t_tri], [tat])
                        po, tpo = psO.next()
                        for hf in range(2):
                            c = 2 * tc + hf
                            pu, tpu = psU.next()
                            mm(S, pu[:], [(ktok[hf][:, tc, :], vtok[:, tc, :])], r=[t_ktok[hf], t_v], w=[tpu])
                            mm(S, po[:, hf * 64:(hf + 1) * 64], [(vtok[:, tc, :], at[:, hf, :])], r=[t_v, tat], w=[tpo],
                               start=True, stop=(c == 0))
                            if c > 0:
                                mm(S, po[:, hf * 64:(hf + 1) * 64], [(sbc[0][:], qtil[:, c * 64:(c + 1) * 64])], r=[sbc[1], t_qt], w=[tpo],
                                   start=False, stop=True)
                            if c == 0:
                                cp("dve", Tst[:], pu[:], [tpu], [t_T])
                            else:
                                stt(Tst[:], Tst[:], dd[:, c - 1:c], pu[:], ALU.mult, ALU.add, [t_T, t_dd, tpu], [t_T])
                            if c < 31:
                                sbc = Sb.next()
                                ts("dve", sbc[0][:], Tst[:], dd[:, c:c + 1], None, ALU.mult, None, [t_T, t_dd], [sbc[1]])
                        act(oT[:, tc * 128:(tc + 1) * 128], po[:], AF.Copy, [tpo], [t_oT])
                    for tti in range(4):
                        cs = slice(tti * 512, (tti + 1) * 512)
                        rms_part(sq, oT[:, cs], 512, psr, [t_oT], rs_t, t_rs)
                        stt(t1[:], oT[:, cs], gn[:, 0:1], rs_t[:], ALU.mult, ALU.mult, [t_oT, t_gn, t_rs], [t_t1])
                        tt("dve", yc[:, cs], t1[:], gate[:, cs], ALU.mult, [t_t1, t_gate], [t_yc])
                    dma("sp", yT_d[1024 + h * 128:1024 + (h + 1) * 128, :], yc[:], r=[t_yc])
                S.flush()

        def mixer_d(l, win, xnT, xtok):
            lam_init = 0.8 - 0.6 * float(np.exp(-0.3 * l))
            with ExitStack() as k:
                wr = Ring([sb(k, [128, 16, 128], BF16) for _ in range(6)])
                psr = Ring([pst(k, [128, 512]) for _ in range(4)])
                pacc = [pst(k, [128, 512]) for _ in range(4)]; t_acc = [Tok() for _ in range(4)]
                cosT = sb(k, [128, T]); t_cos = Tok()
                sinT = sb(k, [128, T]); t_sin = Tok()
                mk = sb(k, [128, 2]); t_mk = Tok()
                lamt = sb(k, [128, 256]); t_lam = Tok()
                lw_ = sb(k, [128, 128]); t_lw = Tok()
                ls = sb(k, [128, 8]); t_ls = Tok()
                gD = sb(k, [128, 1]); t_gD = Tok()
                qr = sb(k, [128, T], BF16); t_qr = Tok()
                k1p = sb(k, [128, T], BF16); t_k1 = Tok()
                k2p = sb(k, [128, T], BF16); t_k2 = Tok()
                vtok = sb(k, [128, 16, 128], BF16); t_v = Tok()
                tA = Ring([sb(k, [128, 512]) for _ in range(2)])
                tB = Ring([sb(k, [128, 512]) for _ in range(2)])
                PT = Ring([sb(k, [128, 512], BF16) for _ in range(6)])
                r1r = Ring([sb(k, [128, 512]) for _ in range(2)])
                r2r = Ring([sb(k, [128, 512]) for _ in range(2)])
                oD = sb(k, [128, 512]); t_oD = Tok()
                sq = (sb(k, [128, 512], BF16), Tok())
                rs_t = sb(k, [128, 512]); t_rs = Tok()
                ydT = Ring([sb(k, [128, T], BF16) for _ in range(2)])
                dma("sp", cosT[:], c_cos, w=[t_cos])
                dma("sp", sinT[:], c_sinA, w=[t_sin])
                dma("sp", mk[:], c_mk, w=[t_mk])
                dma("sp", lamt[:], diff_lambda[l].partition_broadcast(128), w=[t_lam])
                dma("sp", gD[:], diff_norm_g[l].rearrange("(p o) -> p o", o=1), w=[t_gD])
                ts("dve", gD[:], gD[:], 1.0 - lam_init, None, ALU.mult, None, [t_gD], [t_gD])
                tt("dve", lw_[:, 0:64], lamt[:, 0:64], lamt[:, 64:128], ALU.mult, [t_lam], [t_lw])
                tt("dve", lw_[:, 64:128], lamt[:, 128:192], lamt[:, 192:256], ALU.mult, [t_lam], [t_lw])
                S.op("dve", lambda e: e.reduce_sum(out=ls[:, 0:1], in_=lw_[:, 0:64], axis=AX.X), r=[t_lw], w=[t_ls])
                S.op("dve", lambda e: e.reduce_sum(out=ls[:, 1:2], in_=lw_[:, 64:128], axis=AX.X), r=[t_lw], w=[t_ls])
                act(ls[:, 2:4], ls[:, 0:2], AF.Exp, [t_ls], [t_ls])
                tt("dve", ls[:, 4:5], ls[:, 3:4], ls[:, 2:3], ALU.subtract, [t_ls], [t_ls])
                ts("dve", ls[:, 5:6], ls[:, 4:5], -lam_init, None, ALU.add, None, [t_ls], [t_ls])
                def ldd(h_):
                    return (load_w(wr, win, D0 + h_ * 128, 128), load_w(wr, win, D0 + 512 + h_ * 128, 128),
                            load_w(wr, win, D0 + 1024 + h_ * 128, 128))
                nxtw = ldd(0)
                for h in range(4):
                    yd, t_yd = ydT.next()
                    (wq, twq), (wk, twk), (wv, twv) = nxtw
                    if h + 1 < 4:
                        nxtw = ldd(h + 1)

                    def rope(tti, p, tp, dst):
                        cs = slice(tti * 512, (tti + 1) * 512)
                        a_, ta_ = tA.next()
                        b_, tb_ = tB.next()
                        tt("dve", a_[:], p[:], cosT[:, cs], ALU.mult, [tp, t_cos], [ta_])
                        tt("dve", b_[0:64, :], p[64:128, :], sinT[64:128, cs], ALU.mult, [tp, t_sin], [tb_])
                        tt("dve", b_[64:128, :], p[0:64, :], sinT[0:64, cs], ALU.mult, [tp, t_sin], [tb_])
                        return a_, ta_, b_, tb_, cs

                    def evq(tti, p, tp):
                        a_, ta_, b_, tb_, cs = rope(tti, p, tp, None)
                        tt("dve", qr[:, cs], a_[:], b_[:], ALU.add, [ta_, tb_], [t_qr])

                    def evk(tti, p, tp):
                        a_, ta_, b_, tb_, cs = rope(tti, p, tp, None)
                        tt("dve", a_[:], a_[:], b_[:], ALU.add, [ta_, tb_], [ta_])
                        act(k1p[:, cs], a_[:], AF.Copy, [ta_, t_mk], [t_k1], scale=mk[:, 0:1])
                        act(k2p[:, cs], a_[:], AF.Copy, [ta_, t_mk], [t_k2], scale=mk[:, 1:2])

                    def evv(tc, p, tp):
                        act(vtok[:, tc, :], p[:, 0:128], AF.Copy, [tp], [t_v])
                    proj_fm(xnT, xtok, wq, twq, 0, 128, 16, psr, evq)
                    proj_fm(xnT, xtok, wk, twk, 0, 128, 16, psr, evk)
                    proj_tm(xnT, xtok, wv, twv, 128, 16, psr, evv)
                    pend = None
                    for i in range(4):
                        nj = 4 * i + 4

                        def s_stage(j, i=i):
                            t0 = max(i * 512, j * 128)
                            ncol = (i + 1) * 512 - t0
                            c0 = t0 - i * 512
                            res_ = []
                            for m, (kp, tkp) in enumerate(((k1p, t_k1), (k2p, t_k2))):
                                p, tp = psr.next()
                                mm(S, p[:, 0:ncol], [(kp[:, j * 128:(j + 1) * 128], qr[:, t0:t0 + ncol])], r=[tkp, t_qr], w=[tp])
                                pt_, tpt = PT.next()
                                act(pt_[:, 0:ncol], p[:, 0:ncol], AF.Exp, [tp], [tpt], scale=0.125)
                                if j >= 4 * i:
                                    memset("pool", pt_[64:128, 0:64], 0.0, [tpt])
                                res_.append((pt_, tpt))
                            return (j, res_, c0, ncol)

                        def pv_stage(st_, nj=nj):
                            j, res_, c0, ncol = st_
                            for m, (pt_, tpt) in enumerate(res_):
                                mm(S, pacc[2 * m][:, c0:c0 + ncol], [(vtok[:, j, :], pt_[:, 0:ncol])], r=[t_v, tpt], w=[t_acc[2 * m]],
                                   start=(j == 0), stop=(j == nj - 1))
                                mm(S, pacc[2 * m + 1][:, c0:c0 + ncol], [(ones_bf[:], pt_[:, 0:ncol])], r=[t_ones, tpt], w=[t_acc[2 * m + 1]],
                                   start=(j == 0), stop=(j == nj - 1))
                        def fin2(i_, r1, t_r1, r2, t_r2, yd=yd, t_yd=t_yd):
                            cs = slice(i_ * 512, (i_ + 1) * 512)
                            stt(oD[:], r2[:], ls[:, 5:6], r1[:], ALU.mult, ALU.add, [t_r1, t_r2, t_ls], [t_oD])
                            rms_part(sq, oD[:], 512, psr, [t_oD], rs_t, t_rs)
                            stt(yd[:, cs], oD[:], gD[:, 0:1], rs_t[:], ALU.mult, ALU.mult, [t_oD, t_gD, t_rs], [t_yd])
                        prev = None
                        for j in range(nj):
                            cur = s_stage(j)
                            if prev is not None:
                                pv_stage(prev)
                            prev = cur
                            if j == 1 and pend is not None:
                                fin2(*pend)
                                pend = None
                        pv_stage(prev)
                        r1, t_r1 = r1r.next()
                        r2, t_r2 = r2r.next()
                        recip(r1[:], pacc[1][:], [t_acc[1]], [t_r1])
                        tt("dve", r1[:], pacc[0][:], r1[:], ALU.mult, [t_acc[0], t_r1], [t_r1])
                        recip(r2[:], pacc[3][:], [t_acc[3]], [t_r2])
                        tt("dve", r2[:], pacc[2][:], r2[:], ALU.mult, [t_acc[2], t_r2], [t_r2])
                        pend = (i, r1, t_r1, r2, t_r2)
                    fin2(*pend)
                    pend = None
                    dma("sp", yT_d[1536 + h * 128:1536 + (h + 1) * 128, :], yd[:], r=[t_yd])
                S.flush()

        def phase_gate(l, xnT, xtok):
            with ExitStack() as k:
                yall = sb(k, [128, 16, T], BF16); t_y = Tok()
                wgr = Ring([sb(k, [128, 16, 128], BF16) for _ in range(3)])
                wbr = Ring([sb(k, [128, 4, 128], BF16) for _ in range(3)])
                bg = sb(k, [128, 4, 16]); t_bg = Tok()
                psg = Ring([pst(k, [128, 512]) for _ in range(4)])
                psb = Ring([pst(k, [128, 512]) for _ in range(4)])
                gt = Ring([sb(k, [128, 512]) for _ in range(3)])
                tmp = Ring([sb(k, [128, 512]) for _ in range(3)])
                accs = [sb(k, [128, 512]) for _ in range(4)]; t_accs = [Tok() for _ in range(4)]
                mo = Ring([sb(k, [128, T], BF16) for _ in range(2)])
                for q4 in range(4):
                    dma("sp", yall[:, q4 * 4:(q4 + 1) * 4, :], yT_d[q4 * 512:(q4 + 1) * 512, :].rearrange("(kc p) t -> p kc t", p=128), w=[t_y])
                bgin = sb(k, [64, 128]); t_bgin = Tok()
                identf = sb(k, [128, 128]); t_idf = Tok()
                dma("sp", identf[:], c_identf, w=[t_idf])
                dma("sp", bgin[:], b_gate[l].rearrange("n (oc p) -> (n oc) p", p=128), w=[t_bgin])
                p0, tp0 = psg.next()
                S.op("pe", lambda e: e.transpose(out=p0[:, 0:64], in_=bgin[:], identity=identf[0:64, 0:64]), r=[t_bgin, t_idf], w=[tp0])
                cp("dve", bg[:], p0[:, 0:64].rearrange("p (n oc) -> p n oc", n=4), [tp0], [t_bg])
                def ldg(i_):
                    oc_, n_ = divmod(i_, 4)
                    return load_w(wgr, w_gate[l, n_], oc_ * 128, 128), load_w(wbr, w_branch[l, n_], oc_ * 128, 128, KC=4)
                nxtw = ldg(0)
                for oc in range(16):
                    m_, tm_ = mo.next()
                    for n in range(4):
                        (wg, twg), (wb_, twb) = nxtw
                        if oc * 4 + n + 1 < 64:
                            nxtw = ldg(oc * 4 + n + 1)
                        for tti in range(4):
                            cs = slice(tti * 512, (tti + 1) * 512)
                            pg, tpg = psg.next()
                            pb, tpb = psb.next()
                            mm(S, pg[:], [(wg[:, kc, :], xnT[:, kc, cs]) for kc in range(16)], r=[xtok, twg], w=[tpg])
                            mm(S, pb[:], [(wb_[:, kc, :], yall[:, n * 4 + kc, cs]) for kc in range(4)], r=[t_y, twb], w=[tpb])
                            g_, tg_ = gt.next()
                            act(g_[:], pg[:], AF.Sigmoid, [tpg, t_bg], [tg_], bias=bg[:, n, oc:oc + 1])
                            if n == 0:
                                tt("dve", accs[tti][:], g_[:], pb[:], ALU.mult, [tg_, tpb], [t_accs[tti]])
                            else:
                                t_, tt_ = tmp.next()
                                tt("dve", t_[:], g_[:], pb[:], ALU.mult, [tg_, tpb], [tt_])
                                if n < 3:
                                    tt("pool", accs[tti][:], accs[tti][:], t_[:], ALU.add, [t_accs[tti], tt_], [t_accs[tti]])
                                else:
                                    tt("pool", m_[:, cs], accs[tti][:], t_[:], ALU.add, [t_accs[tti], tt_], [tm_])
                    dma("sp", mixT_d[oc * 128:(oc + 1) * 128, :], m_[:], r=[tm_])
                S.flush()

        def phase_wout(l, src_rows):
            with ExitStack() as k:
                mT = sb(k, [128, 16, T], BF16); t_m = Tok()
                wo = sb(k, [128, 16, D], BF16); t_wo = [Tok() for _ in range(4)]
                xr = Ring([sb(k, [128, D]) for _ in range(2)])
                xo = Ring([sb(k, [128, D]) for _ in range(2)])
                psr = Ring([pst(k, [128, 512]) for _ in range(4)])
                for q4 in range(4):
                    dma("sp", mT[:, q4 * 4:(q4 + 1) * 4, :], mixT_d[q4 * 512:(q4 + 1) * 512, :].rearrange("(kc p) t -> p kc t", p=128), w=[t_m])
                for ct in range(4):
                    dma("pool", wo[:, :, ct * 512:(ct + 1) * 512], w_out[l][:, ct * 512:(ct + 1) * 512].rearrange("(kc p) n -> p kc n", p=128), w=[t_wo[ct]], ndesc=128)
                for tc in range(16):
                    xi, txi = xr.next()
                    xo_, txo = xo.next()
                    dma("sp", xi[:], src_rows[tc * 128:(tc + 1) * 128, :], w=[txi])
                    for ct in range(4):
                        cs = slice(ct * 512, (ct + 1) * 512)
                        p, tp = psr.next()
                        mm(S, p[:], [(mT[:, kc, tc * 128:(tc + 1) * 128], wo[:, kc, cs]) for kc in range(16)], r=[t_m, t_wo[ct]], w=[tp])
                        tt("dve", xo_[:, cs], p[:], xi[:, cs], ALU.add, [tp, txi], [txo])
                    dma("sp", xa_d[tc * 128:(tc + 1) * 128, :], xo_[:], r=[txo])
                S.flush()

        def phase_ffn_up(l, hnT, htok):
            with ExitStack() as k:
                wr = Ring([sb(k, [128, 16, 128], BF16) for _ in range(4)])
                cwin = sb(k, [88, 4, 128]); t_cwin = Tok()
                cw = sb(k, [128, 4, 88]); t_cw = Tok()
                identf = sb(k, [128, 128]); t_idf = Tok()
                ha = Ring([sb(k, [128, T + 2]) for _ in range(2)])
                hg = Ring([sb(k, [128, T + 2]) for _ in range(2)])
                ca = Ring([sb(k, [128, T]) for _ in range(2)])
                cg = Ring([sb(k, [128, T]) for _ in range(2)])
                ao = Ring([sb(k, [128, T], BF16) for _ in range(2)])
                psa = Ring([pst(k, [128, 512]) for _ in range(4)])
                psg = Ring([pst(k, [128, 512]) for _ in range(4)])
                dma("sp", identf[:], c_identf, w=[t_idf])
                for kk in range(3):
                    dma("sp", cwin[:, kk, :], ffn_conv_w[l, kk].rearrange("(c p) -> c p", p=128), w=[t_cwin])
                dma("sp", cwin[:, 3, :], ffn_conv_b[l].rearrange("(c p) -> c p", p=128), w=[t_cwin])
                p0, tp0 = psa.next()

                def fnT(e):
                    ins = None
                    for kk in range(4):
                        ins = e.transpose(out=p0[:, kk * 88:(kk + 1) * 88], in_=cwin[:, kk, :], identity=identf[0:88, 0:88])
                    return ins
                S.op("pe", fnT, r=[t_cwin, t_idf], w=[tp0])
                cp("dve", cw[:], p0[:, 0:352].rearrange("p (k c) -> p k c", k=4), [tp0], [t_cw])
                for r_ in (ha, hg):
                    for (tb, ttk) in r_.items:
                        memset("pool", tb[:, 0:2], 0.0, [ttk])
                wup = ffn_w_up[l]
                def ldw(j):
                    return load_w(wr, wup, j * 128, 128), load_w(wr, wup, (44 + j) * 128, 128)
                nxtw = ldw(0)
                for j in range(44):
                    (wa, twa), (wg, twg) = nxtw
                    if j + 1 < 44:
                        nxtw = ldw(j + 1)
                    ha_, tha = ha.next()
                    hg_, thg = hg.next()
                    for tti in range(4):
                        cs = slice(tti * 512, (tti + 1) * 512)
                        pa, tpa = psa.next()
                        pg, tpg = psg.next()
                        mm(S, pa[:], [(wa[:, kc, :], hnT[:, kc, cs]) for kc in range(16)], r=[htok, twa], w=[tpa])
                        mm(S, pg[:], [(wg[:, kc, :], hnT[:, kc, cs]) for kc in range(16)], r=[htok, twg], w=[tpg])
                        act(ha_[:, 2 + tti * 512:2 + (tti + 1) * 512], pa[:], AF.Copy, [tpa], [tha])
                        act(hg_[:, 2 + tti * 512:2 + (tti + 1) * 512], pg[:], AF.Copy, [tpg], [thg])
                    ca_, tca = ca.next()
                    cg_, tcg = cg.next()
                    ao_, tao = ao.next()
                    for (hb, thb, c_, tc_, jj) in ((ha_, tha, ca_, tca, j), (hg_, thg, cg_, tcg, 44 + j)):
                        act(c_[:], hb[:, 2:T + 2], AF.Identity, [thb, t_cw], [tc_], scale=cw[:, 2, jj:jj + 1], bias=cw[:, 3, jj:jj + 1])
                        stt(c_[:], hb[:, 1:T + 1], cw[:, 1, jj:jj + 1], c_[:], ALU.mult, ALU.add, [thb, t_cw, tc_], [tc_])
                        stt(c_[:], hb[:, 0:T], cw[:, 0, jj:jj + 1], c_[:], ALU.mult, ALU.add, [thb, t_cw, tc_], [tc_])
                    act(ca_[:], ca_[:], AF.Gelu_apprx_tanh, [tca], [tca])
                    tt("pool", ao_[:], ca_[:], cg_[:], ALU.mult, [tca, tcg], [tao])
                    dma("sp", actT_d[j * 128:(j + 1) * 128, :], ao_[:], r=[tao])
                S.flush()

        def phase_ffn_down(l, dst_rows):
            with ExitStack() as k:
                aT = sb(k, [128, 44, 1024], BF16); t_a = Tok()
                wd = Ring([sb(k, [128, 44, 256], BF16) for _ in range(2)])
                xr = Ring([sb(k, [128, 256]) for _ in range(4)])
                xo = Ring([sb(k, [128, 256]) for _ in range(4)])
                psr = Ring([pst(k, [128, 512]) for _ in range(4)])
                wdn = ffn_w_down[l]
                for half in range(2):
                    for q4 in range(4):
                        dma("sp", aT[:, q4 * 11:(q4 + 1) * 11, :],
                            actT_d[q4 * 1408:(q4 + 1) * 1408, half * 1024:(half + 1) * 1024].rearrange("(kc p) t -> p kc t", p=128), w=[t_a])
                    for ct in range(8):
                        w_, tw_ = load_w(wd, wdn, ct * 256, 256, KC=44)
                        for tc in range(8):
                            r0 = half * 1024 + tc * 128
                            xi, txi = xr.next()
                            xo_, txo = xo.next()
                            dma("sp", xi[:], xa_d[r0:r0 + 128, ct * 256:(ct + 1) * 256], w=[txi])
                            p, tp = psr.next()
                            mm(S, p[:, 0:256], [(aT[:, kc, tc * 128:(tc + 1) * 128], w_[:, kc, :]) for kc in range(44)], r=[t_a, tw_], w=[tp])
                            tt("dve", xo_[:], p[:, 0:256], xi[:], ALU.add, [tp, txi], [txo])
                            dma("sp", dst_rows[r0:r0 + 128, ct * 256:(ct + 1) * 256], xo_[:], r=[txo])
                S.flush()

        def phase_final(src, dst):
            with ExitStack() as k:
                Gt = sb(k, [128, D]); tG = Tok()
                xin = Ring([sb(k, [128, D]) for _ in range(3)])
                xs = Ring([sb(k, [128, D]) for _ in range(3)])
                junk = sb(k, [128, D], BF16); tj = Tok()
                st = Ring([sb(k, [128, 4]) for _ in range(3)])
                dma("sp", Gt[:], norm_final_g.partition_broadcast(128), w=[tG])

                def fa(tc):
                    xi, txi = xin.next()
                    xo, txo = xs.next()
                    sv, tsv = st.next()
                    dma("sp", xi[:], src[tc * 128:(tc + 1) * 128, :], w=[txi])
                    act(junk[:], xi[:], AF.Square, [txi], [tj, tsv], accum_out=sv[:, 0:1])
                    act(sv[:, 1:2], sv[:, 0:1], AF.Sqrt, [tsv, t_eps], [tsv], scale=1.0 / D, bias=epst[:])
                    recip(sv[:, 2:3], sv[:, 1:2], [tsv], [tsv])
                    stt(xo[:], xi[:], sv[:, 2:3], Gt[:], ALU.mult, ALU.mult, [txi, tsv, tG], [txo])
                    return xo, txo
                cur = fa(0)
                for tc in range(16):
                    nxt = fa(tc + 1) if tc + 1 < 16 else None
                    dma("sp", dst[tc * 128:(tc + 1) * 128, :], cur[0][:], r=[cur[1]])
                    cur = nxt
                S.flush()

        scr = [xb_d, xc_d]
        for s in range(NS):
            src = x_in[s]
            for l in range(NLAY):
                dst = scr[l % 2]
                layer(l, src, dst)
                src = dst
            if STOP >= 10: phase_final(src, out[s])
    return nc


def _constants():
    ident = np.eye(128, dtype=np.float32)
    half = 32
    inv_freq = (10000.0 ** (-np.arange(half, dtype=np.float32) / half)).astype(np.float32)
    pos = np.arange(T, dtype=np.float32)
    ang = (pos[None, :] * inv_freq[:, None]).astype(np.float32)
    cos = np.cos(ang).astype(np.float32)
    sin = np.sin(ang).astype(np.float32)
    c_cos = np.tile(cos, (4, 1))
    c_sinA = np.concatenate([sin, sin, -sin, -sin], axis=0)
    tri = (np.arange(64)[None, :] >= (np.arange(128)[:, None] % 64)).astype(np.float32)
    g = (np.arange(128) // 32) % 2
    mk = np.stack([(g == 0), (g == 1)], axis=1).astype(np.float32)
    return {
        "c_ident": ident.astype(ml_dtypes.bfloat16),
        "c_identf": ident,
        "c_cos": np.ascontiguousarray(c_cos),
        "c_sinA": np.ascontiguousarray(c_sinA),
        "c_tri": tri,
        "c_mk": mk,
        "c_rm": np.stack([np.arange(128) < 64, np.arange(128) >= 64], axis=1).astype(np.float32),
        "c_tri2": np.ascontiguousarray(np.stack([tri * (np.arange(128)[:, None] < 64), tri * (np.arange(128)[:, None] >= 64)], axis=1).astype(np.float32)),
    }


def _perm_cols():
    r = np.arange(128)
    comp = (r // 32) % 2
    part = r // 64
    return comp * 64 + part * 32 + (r % 32)


def _prep_shared(inp):
    f = lambda a: np.ascontiguousarray(np.asarray(a, dtype=np.float32))
    w_in = f(inp["w_in"]).copy()
    pc = _perm_cols()
    for base in (D0, D0 + 512):
        for h in range(4):
            c = base + h * 128
            w_in[:, :, c:c + 128] = w_in[:, :, c + pc]
    m = {
        "norm_mix_g": f(inp["norm_mix_g"]), "w_in": w_in, "fox_b_f": f(inp["fox_b_f"]),
        "gmlp_ln_g": f(inp["gmlp_ln_g"]), "gmlp_ln_b": f(inp["gmlp_ln_b"]),
        "gmlp_w_sT": f(np.transpose(np.asarray(inp["gmlp_w_s"]), (0, 1, 3, 2))),
        "gmlp_b_s": f(np.asarray(inp["gmlp_b_s"]).reshape(DEPTH, 512)),
        "hgrn_lb_logits": f(inp["hgrn_lb_logits"]), "hgrn_norm_g": f(inp["hgrn_norm_g"]),
        "diff_lambda": f(np.asarray(inp["diff_lambda"]).reshape(DEPTH, 256)), "diff_norm_g": f(inp["diff_norm_g"]),
        "w_branch": f(inp["w_branch"]), "w_gate": f(inp["w_gate"]), "b_gate": f(inp["b_gate"]),
        "w_out": f(inp["w_out"]), "norm_ffn_g": f(inp["norm_ffn_g"]), "ffn_w_up": f(inp["ffn_w_up"]),
        "ffn_conv_w": f(inp["ffn_conv_w"]), "ffn_conv_b": f(inp["ffn_conv_b"]), "ffn_w_down": f(inp["ffn_w_down"]),
        "norm_final_g": f(inp["norm_final_g"]),
    }
    m.update(_constants())
    return m


def kernel(**inputs):
    x = np.ascontiguousarray(np.asarray(inputs["x"], dtype=np.float32))
    shared = _prep_shared(inputs)
    NS = x.shape[0] // NCORES
    nc = build_nc(NS=NS, NLAY=DEPTH, dbg=False)
    in_maps = []
    for c in range(NCORES):
        m = dict(shared)
        m["x"] = np.ascontiguousarray(x[c * NS:(c + 1) * NS])
        in_maps.append(m)
    res = run_bass_kernel_spmd(nc, in_maps, core_ids=list(range(NCORES)))
    return np.concatenate([np.asarray(r["out"], dtype=np.float32) for r in res.results], axis=0)
```

```python
import numpy as np
import ml_dtypes
import concourse.bass as bass
import concourse.mybir as mybir
from concourse.bass_utils import run_bass_kernel_spmd

F32 = mybir.dt.float32
BF16 = mybir.dt.bfloat16
ALU = mybir.AluOpType
AF = mybir.ActivationFunctionType
AX = mybir.AxisListType

D = 2048
T = 2048
DEPTH = 2
BW = 512
DFF = 5632
EPS = 1e-6
A0 = 0
B0 = 1024
C0 = 1024 + 1544
D0 = C0 + 2048
IN_COLS = 6152
NCORES = 8


class Tok:
    __slots__ = ("lw", "rd", "name")

    def __init__(self, name=""):
        self.lw = None
        self.rd = []
        self.name = name


class Op:
    __slots__ = ("eng", "fn", "deps", "sig", "val", "dma", "sem", "lane_wait")


CE = ("pe", "act", "dve", "pool")
FULLSYNC = True
NLANE = 12


class Sched:
    def __init__(self, nc):
        self.nc = nc
        self.sem = {e: nc.alloc_semaphore("c_" + e) for e in CE}
        self.cnt = {e: 0 for e in CE}
        self.lanes = {q: [[nc.alloc_semaphore("l_%s%d" % (q, i)), 0] for i in range(NLANE)] for q in ("sp", "pool")}
        self.rr = {"sp": 0, "pool": 0}
        self.ops = {e: [] for e in ("pe", "act", "dve", "pool", "sp")}
        self.toks = set()
        self.waited = {}
        self.nphase = 0
        self.pool_out = []

    def _deps(self, o, r, w):
        deps = []
        seen = set()

        def add(d, raw):
            if d is None or d is o or id(d) in seen:
                return
            if (not d.dma) and (not o.dma) and d.eng == o.eng and (d.eng == "pe" or (not raw and not FULLSYNC)):
                return
            seen.add(id(d))
            deps.append(d)
            if not d.dma:
                d.sig = True

        for t in r:
            add(t.lw, True)
        for t in w:
            add(t.lw, False)
            for d in t.rd:
                add(d, False)
        o.deps = deps
        for t in r:
            t.rd.append(o)
            self.toks.add(t)
        for t in w:
            t.lw = o
            t.rd = []
            self.toks.add(t)

    def op(self, eng, fn, r=(), w=()):
        o = Op()
        o.eng = eng
        o.fn = fn
        o.dma = False
        o.sig = False
        o.val = None
        o.sem = None
        o.lane_wait = 0
        self._deps(o, r, w)
        self.ops[eng].append(o)
        return o

    def dma(self, q, fn, r=(), w=(), ndesc=0):
        o = Op()
        o.eng = q
        o.fn = fn
        o.dma = True
        o.sig = True
        extra = []
        if q == "pool" and ndesc:
            while self.pool_out and sum(n for _, n in self.pool_out) + ndesc > 640:
                extra.append(self.pool_out.pop(0)[0])
            self.pool_out.append((o, ndesc))
        lane = self.lanes[q][self.rr[q]]
        self.rr[q] = (self.rr[q] + 1) % NLANE
        o.lane_wait = 16 * lane[1]
        lane[1] += 1
        o.sem = lane[0]
        o.val = 16 * lane[1]
        self._deps(o, r, w)
        for d in extra:
            if d not in o.deps:
                o.deps.append(d)
        self.ops[q].append(o)
        return o

    def flush(self):
        self.pool_out = []
        for e in CE:
            c = self.cnt[e]
            for o in self.ops[e]:
                if (not o.dma) and o.sig:
                    c += 1
                    o.val = c
            self.cnt[e] = c
        ops = self.ops
        self.ops = {e: [] for e in ("pe", "act", "dve", "pool", "sp")}
        waited = self.waited
        sems = self.sem
        lanes = self.lanes

        def emit(e, name):
            def w8(sem, val):
                key = (name, sem.num)
                if waited.get(key, 0) >= val:
                    return
                e.wait_ge(sem, val)
                waited[key] = val

            for o in ops[name]:
                for d in o.deps:
                    if d.dma:
                        w8(d.sem, d.val)
                    else:
                        w8(sems[d.eng], d.val)
                if o.dma and o.lane_wait:
                    w8(o.sem, o.lane_wait)
                ins = o.fn(e)
                if o.dma:
                    ins.then_inc(o.sem, 16)
                elif o.sig:
                    ins.then_inc(sems[o.eng], 1)
            if name in lanes:
                for sem, cnt in lanes[name]:
                    if cnt:
                        w8(sem, 16 * cnt)

        with self.nc.Block() as blk:
            @blk.sync
            def _(e):
                emit(e, "sp")

            @blk.gpsimd
            def _(e):
                emit(e, "pool")

            @blk.tensor
            def _(e):
                emit(e, "pe")

            @blk.scalar
            def _(e):
                emit(e, "act")

            @blk.vector
            def _(e):
                emit(e, "dve")
        for t in self.toks:
            t.lw = None
            t.rd = []
        self.toks = set()
        self.nphase += 1


class Ring:
    def __init__(self, items):
        self.items = [(t, Tok()) for t in items]
        self.i = 0

    def next(self):
        it = self.items[self.i]
        self.i = (self.i + 1) % len(self.items)
        return it


def mm(S, out_ap, pairs, r, w, start=True, stop=True):
    def fn(e, pairs=pairs, out_ap=out_ap, start=start, stop=stop):
        n = len(pairs)
        ins = None
        for i, (l, rh) in enumerate(pairs):
            ins = e.matmul(out_ap, l, rh, start=(start and i == 0), stop=(stop and i == n - 1))
        return ins
    return S.op("pe", fn, r=r, w=w)


from contextlib import ExitStack

_uid = [0]


def build_nc(NS=2, NLAY=2, dbg=False, STOP=99):
    nc = bass.Bass("TRN2", target_bir_lowering=False)

    def din(name, shape, dt=F32):
        return nc.dram_tensor(name, list(shape), dt, kind="ExternalInput").ap()

    x_in = din("x", [NS, T, D])
    norm_mix_g = din("norm_mix_g", [DEPTH, D])
    w_in = din("w_in", [DEPTH, D, IN_COLS])
    fox_b_f = din("fox_b_f", [DEPTH, 8])
    gmlp_ln_g = din("gmlp_ln_g", [DEPTH, BW])
    gmlp_ln_b = din("gmlp_ln_b", [DEPTH, BW])
    gmlp_w_sT = din("gmlp_w_sT", [DEPTH, 4, 128, 128])
    gmlp_b_s = din("gmlp_b_s", [DEPTH, 512])
    hgrn_lb = din("hgrn_lb_logits", [DEPTH, 512])
    hgrn_norm_g = din("hgrn_norm_g", [DEPTH, 128])
    diff_lambda = din("diff_lambda", [DEPTH, 256])
    diff_norm_g = din("diff_norm_g", [DEPTH, 128])
    w_branch = din("w_branch", [DEPTH, 4, BW, D])
    w_gate = din("w_gate", [DEPTH, 4, D, D])
    b_gate = din("b_gate", [DEPTH, 4, D])
    w_out = din("w_out", [DEPTH, D, D])
    norm_ffn_g = din("norm_ffn_g", [DEPTH, D])
    ffn_w_up = din("ffn_w_up", [DEPTH, D, 2 * DFF])
    ffn_conv_w = din("ffn_conv_w", [DEPTH, 3, 2 * DFF])
    ffn_conv_b = din("ffn_conv_b", [DEPTH, 2 * DFF])
    ffn_w_down = din("ffn_w_down", [DEPTH, DFF, D])
    norm_final_g = din("norm_final_g", [D])
    c_ident = din("c_ident", [128, 128], BF16)
    c_cos = din("c_cos", [128, T])
    c_sinA = din("c_sinA", [128, T])
    c_tri = din("c_tri", [128, 64])
    c_mk = din("c_mk", [128, 2])
    c_identf = din("c_identf", [128, 128])
    c_rm = din("c_rm", [128, 2])
    c_tri2 = din("c_tri2", [128, 2, 64])

    out = nc.dram_tensor("out", [NS, T, D], F32, kind="ExternalOutput").ap()
    okind = "ExternalOutput" if dbg else "Internal"

    def dscr(name, shape, dt):
        if dbg:
            return nc.dram_tensor(name, list(shape), dt, kind="ExternalOutput").ap()
        return nc.dram_tensor(name, list(shape), dt).ap()

    yT_d = dscr("yT_d", [D, T], BF16)
    mixT_d = dscr("mixT_d", [D, T], BF16)
    xa_d = dscr("xa_d", [T, D], F32)
    xb_d = dscr("xb_d", [T, D], F32)
    xc_d = dscr("xc_d", [T, D], F32)
    actT_d = dscr("actT_d", [DFF, T], BF16)
    ex_d = dscr("ex_d", [2, 8, 6, T], BF16)

    S = Sched(nc)

    def sb(k, shape, dt=F32):
        _uid[0] += 1
        return k.enter_context(nc.sbuf_tensor("s%d" % _uid[0], list(shape), dt))

    def pst(k, shape, dt=F32):
        _uid[0] += 1
        return k.enter_context(nc.psum_tensor("p%d" % _uid[0], list(shape), dt))

    def dma(q, out_ap, in_ap, r=(), w=(), slow=False, ndesc=0):
        if slow:
            return S.dma(q, lambda e, o=out_ap, i=in_ap: e.dma_start(out=o, in_=i, allow_slow_non_contiguous=True), r=r, w=w, ndesc=ndesc)
        return S.dma(q, lambda e, o=out_ap, i=in_ap: e.dma_start(out=o, in_=i), r=r, w=w, ndesc=ndesc)

    def act(out_ap, in_ap, func, r, w, **kw):
        return S.op("act", lambda e, o=out_ap, i=in_ap, f=func, kw=kw: e.activation(out=o, in_=i, func=f, **kw), r=r, w=w)

    def tt(eng, out_ap, a, b, op, r, w):
        return S.op(eng, lambda e, o=out_ap, a=a, b=b, op=op: e.tensor_tensor(out=o, in0=a, in1=b, op=op), r=r, w=w)

    def ts(eng, out_ap, a, s1, s2, op0, op1, r, w):
        if op1 is None:
            return S.op(eng, lambda e, o=out_ap, a=a, s1=s1, op0=op0: e.tensor_scalar(out=o, in0=a, scalar1=s1, scalar2=None, op0=op0), r=r, w=w)
        return S.op(eng, lambda e, o=out_ap, a=a, s1=s1, s2=s2, op0=op0, op1=op1: e.tensor_scalar(out=o, in0=a, scalar1=s1, scalar2=s2, op0=op0, op1=op1), r=r, w=w)

    def stt(out_ap, a, sc, b, op0, op1, r, w):
        return S.op("dve", lambda e, o=out_ap, a=a, sc=sc, b=b, op0=op0, op1=op1: e.scalar_tensor_tensor(out=o, in0=a, scalar=sc, in1=b, op0=op0, op1=op1), r=r, w=w)

    def recip(out_ap, in_ap, r, w):
        return S.op("dve", lambda e, o=out_ap, i=in_ap: e.reciprocal(out=o, in_=i), r=r, w=w)

    def cp(eng, out_ap, in_ap, r, w):
        return S.op(eng, lambda e, o=out_ap, i=in_ap: e.tensor_copy(out=o, in_=i), r=r, w=w)

    def memset(eng, ap, v, w):
        return S.op(eng, lambda e, ap=ap, v=v: e.memset(ap, v), w=w)

    G = ExitStack()
    with G:
        ident = sb(G, [128, 128], BF16); t_ident = Tok()
        ones_bf = sb(G, [128, 128], BF16); t_ones = Tok()
        epst = sb(G, [128, 1]); t_eps = Tok()
        dma("sp", ident[:], c_ident, w=[t_ident])
        memset("dve", ones_bf[:], 1.0, [t_ones])
        memset("dve", epst[:], EPS, [t_eps])
        CONST_R = [t_ident, t_ones, t_eps]

        def load_w(ring, wap, c0, ncol, KC=16):
            wt, tw = ring.next()
            dma("pool", wt[:, 0:KC, 0:ncol], wap[:, c0:c0 + ncol].rearrange("(kc p) n -> p kc n", p=128), w=[tw], ndesc=KC * 8)
            return wt, tw

        def proj_fm(xT, xtok, wt, tw, col_off, M, KC, psr, evac):
            for tti in range(4):
                p, tp = psr.next()
                pairs = [(wt[:, kc, col_off:col_off + M], xT[:, kc, tti * 512:(tti + 1) * 512]) for kc in range(KC)]
                mm(S, p[0:M, :], pairs, r=[xtok, tw], w=[tp])
                evac(tti, p, tp)

        def proj_tm(xT, xtok, wt, tw, ncol, KC, psr, evac):
            for tc in range(16):
                p, tp = psr.next()
                pairs = [(xT[:, kc, tc * 128:(tc + 1) * 128], wt[:, kc, 0:ncol]) for kc in range(KC)]
                mm(S, p[:, 0:ncol], pairs, r=[xtok, tw], w=[tp])
                evac(tc, p, tp)

        def phase_norm(src, gvec, xnT, xtok):
            with ExitStack() as k:
                Gt = sb(k, [128, D]); tG = Tok()
                xin = Ring([sb(k, [128, D]) for _ in range(3)])
                xs = Ring([sb(k, [128, D], BF16) for _ in range(3)])
                junk = sb(k, [128, D], BF16); tj = Tok()
                st = Ring([sb(k, [128, 4]) for _ in range(3)])
                ptr = Ring([pst(k, [128, 1024], BF16) for _ in range(4)])
                dma("sp", Gt[:], gvec.partition_broadcast(128), w=[tG])
                def stage_a(tc):
                    xi, txi = xin.next()
                    xo, txo = xs.next()
                    sv, tsv = st.next()
                    dma("sp", xi[:], src[tc * 128:(tc + 1) * 128, :], w=[txi])
                    act(junk[:], xi[:], AF.Square, [txi], [tj, tsv], accum_out=sv[:, 0:1])
                    act(sv[:, 1:2], sv[:, 0:1], AF.Sqrt, [tsv, t_eps], [tsv], scale=1.0 / D, bias=epst[:])
                    recip(sv[:, 2:3], sv[:, 1:2], [tsv], [tsv])
                    stt(xo[:], xi[:], sv[:, 2:3], Gt[:], ALU.mult, ALU.mult, [txi, tsv, tG], [txo])
                    return xo, txo

                def stage_b(tc, xo, txo):
                    for half in range(2):
                        p, tp = ptr.next()

                        def fn(e, p=p, xo=xo, half=half):
                            ins = None
                            for j in range(8):
                                c = (half * 8 + j) * 128
                                ins = e.transpose(out=p[:, j * 128:(j + 1) * 128], in_=xo[:, c:c + 128], identity=ident[:])
                            return ins
                        S.op("pe", fn, r=[txo, t_ident], w=[tp])
                        act(xnT[:, half * 8:(half + 1) * 8, tc * 128:(tc + 1) * 128],
                            p[:].rearrange("p (k t) -> p k t", k=8), AF.Copy, [tp], [xtok])
                cur = stage_a(0)
                for tc in range(16):
                    nxt = stage_a(tc + 1) if tc + 1 < 16 else None
                    stage_b(tc, *cur)
                    cur = nxt
                S.flush()

        def rms_part(k_sq, o_ap, ncol, psr, r_o, rs_t, t_rs):
            sq, tsq = k_sq
            act(sq[:, 0:ncol], o_ap, AF.Square, r_o, [tsq])
            p, tp = psr.next()
            mm(S, p[:, 0:ncol], [(ones_bf[:], sq[:, 0:ncol])], r=[tsq, t_ones], w=[tp])
            act(rs_t[:, 0:ncol], p[:, 0:ncol], AF.Sqrt, [tp, t_eps], [t_rs], scale=1.0 / 128, bias=epst[:])
            recip(rs_t[:, 0:ncol], rs_t[:, 0:ncol], [t_rs], [t_rs])

        def layer(l, src_rows, dst_rows):
            win = w_in[l]
            LK = ExitStack()
            with LK:
                xnT = sb(LK, [128, 16, T], BF16); xtok = Tok()
                phase_norm(src_rows, norm_mix_g[l], xnT, xtok)
                if STOP >= 2: mixer_a(l, win, xnT, xtok)
                if STOP >= 3: mixer_b(l, win, xnT, xtok)
                if STOP >= 4: mixer_c(l, win, xnT, xtok)
                if STOP >= 5: mixer_d(l, win, xnT, xtok)
                if STOP >= 6: phase_gate(l, xnT, xtok)
            if STOP >= 7: phase_wout(l, src_rows)
            if STOP >= 8:
                with ExitStack() as k2:
                    hnT = sb(k2, [128, 16, T], BF16); htok = Tok()
                    phase_norm(xa_d, norm_ffn_g[l], hnT, htok)
                    phase_ffn_up(l, hnT, htok)
            if STOP >= 9: phase_ffn_down(l, dst_rows)

        def mixer_a(l, win, xnT, xtok):
            with ExitStack() as k:
                wr = Ring([sb(k, [128, 16, 512], BF16) for _ in range(2)])
                uaT = sb(k, [128, 4, T]); t_ua = Tok()
                yaT = sb(k, [128, 4, T], BF16); t_ya = Tok()
                WsT = sb(k, [128, 4, 128], BF16); t_ws = Tok()
                Wsf = sb(k, [128, 4, 128]); t_wsf = Tok()
                BS = sb(k, [128, 512]); t_bs = Tok()
                Gl = sb(k, [128, 512]); t_gl = Tok()
                Bl = sb(k, [128, 512]); t_bl = Tok()
                vg = Ring([sb(k, [128, 512]) for _ in range(2)])
                vc = Ring([sb(k, [128, 512]) for _ in range(2)])
                vj = sb(k, [128, 512], BF16); t_vj = Tok()
                vn = Ring([sb(k, [128, 512], BF16) for _ in range(4)])
                sv_r = Ring([sb(k, [128, 8]) for _ in range(2)])
                mx = Ring([sb(k, [128, 512]) for _ in range(2)])
                psr = Ring([pst(k, [128, 512]) for _ in range(6)])
                dma("sp", Wsf[:], gmlp_w_sT[l].rearrange("g s t -> s g t"), w=[t_wsf])
                memset("pool", Wsf[64:128, :, 0:64], 0.0, [t_wsf])
                cp("pool", WsT[:], Wsf[:], [t_wsf], [t_ws])
                dma("sp", BS[:], gmlp_b_s[l].partition_broadcast(128), w=[t_bs])
                dma("sp", Gl[:], gmlp_ln_g[l].partition_broadcast(128), w=[t_gl])
                dma("sp", Bl[:], gmlp_ln_b[l].partition_broadcast(128), w=[t_bl])
                wt, tw = load_w(wr, win, A0, 512)
                for oc in range(4):
                    def ev(tti, p, tp, oc=oc):
                        act(uaT[:, oc, tti * 512:(tti + 1) * 512], p[:], AF.Gelu_apprx_tanh, [tp], [t_ua])
                    proj_fm(xnT, xtok, wt, tw, oc * 128, 128, 16, psr, ev)
                wt2, tw2 = load_w(wr, win, A0 + 512, 512)

                def ev2(tc, p, tp):
                    g_, tg_ = vg.next()
                    c_, tc_ = vc.next()
                    n_, tn_ = vn.next()
                    sv, tsv = sv_r.next()
                    act(g_[:], p[:], AF.Gelu_apprx_tanh, [tp], [tg_, tsv], accum_out=sv[:, 0:1])
                    ts("dve", sv[:, 1:2], sv[:, 0:1], 1.0 / 512, None, ALU.mult, None, [tsv], [tsv])
                    ts("dve", c_[:], g_[:], sv[:, 1:2], None, ALU.subtract, None, [tg_, tsv], [tc_])
                    act(vj[:], c_[:], AF.Square, [tc_], [t_vj, tsv], accum_out=sv[:, 2:3])
                    act(sv[:, 3:4], sv[:, 2:3], AF.Sqrt, [tsv, t_eps], [tsv], scale=1.0 / 512, bias=epst[:])
                    recip(sv[:, 4:5], sv[:, 3:4], [tsv], [tsv])
                    stt(c_[:], c_[:], sv[:, 4:5], Gl[:], ALU.mult, ALU.mult, [tc_, tsv, t_gl], [tc_])
                    tt("dve", n_[:], c_[:], Bl[:], ALU.add, [tc_, t_bl], [tn_])

                    def stage_b(tc=tc, n_=n_, tn_=tn_):
                        m_, tm_ = mx.next()
                        p2, tp2 = psr.next()

                        def fn(e, p2=p2, n_=n_):
                            ins = None
                            for g in range(4):
                                ins = e.matmul(p2[:, g * 128:(g + 1) * 128], n_[:, g * 128:(g + 1) * 128], WsT[:, g, :], start=True, stop=True)
                            return ins
                        S.op("pe", fn, r=[tn_, t_ws], w=[tp2])
                        tt("dve", m_[:], p2[:], BS[:], ALU.add, [tp2, t_bs], [tm_])
                        tt("dve", yaT[:, :, tc * 128:(tc + 1) * 128], m_[:].rearrange("p (g t) -> p g t", g=4),
                           uaT[:, :, tc * 128:(tc + 1) * 128], ALU.mult, [tm_, t_ua], [t_ya])
                    pendA.append(stage_b)
                    if len(pendA) > 2:
                        pendA.pop(0)()
                pendA = []
                proj_tm(xnT, xtok, wt2, tw2, 512, 16, psr, ev2)
                while pendA:
                    pendA.pop(0)()
                dma("sp", yT_d[0:512, :].rearrange("(g p) t -> p g t", p=128), yaT[:], r=[t_ya])
                S.flush()

        def mixer_b(l, win, xnT, xtok):
            with ExitStack() as k:
                wr = Ring([sb(k, [128, 16, 128], BF16) for _ in range(6)])
                wz = sb(k, [128, 16, 8], BF16); t_wz = Tok()
                psr = Ring([pst(k, [128, 512]) for _ in range(3)])
                with ExitStack() as k1:
                    cn = sb(k1, [8, T]); t_cn = Tok()
                    ex = sb(k1, [8, T]); t_ex = Tok()
                    rr_ = sb(k1, [8, T]); t_rr = Tok()
                    onesf = sb(k1, [8, T]); t_of = Tok()
                    nbf = sb(k1, [8, 2]); t_nbf = Tok()
                    cbs = [sb(k1, [8, T], BF16) for _ in range(3)]; t_cb = [Tok() for _ in range(3)]
                    nbs = [sb(k1, [8, T], BF16) for _ in range(3)]; t_nb = [Tok() for _ in range(3)]
                    onesb = sb(k1, [8, T], BF16); t_ob = Tok()
                    dma("pool", wz[:], win[:, B0 + 1536:B0 + 1544].rearrange("(kc p) n -> p kc n", p=128), w=[t_wz], ndesc=128)
                    dma("sp", nbf[:, 0:1], fox_b_f[l].rearrange("(h o) -> h o", o=1), w=[t_nbf])
                    ts("dve", nbf[:, 1:2], nbf[:, 0:1], -1.0, None, ALU.mult, None, [t_nbf], [t_nbf])
                    memset("pool", onesf[:], 1.0, [t_of])
                    memset("pool", onesb[:], 1.0, [t_ob])
                    for tti in range(4):
                        p, tp = psr.next()
                        pairs = [(wz[:, kc, :], xnT[:, kc, tti * 512:(tti + 1) * 512]) for kc in range(16)]
                        mm(S, p[0:8, :], pairs, r=[xtok, t_wz], w=[tp])
                        act(ex[:, tti * 512:(tti + 1) * 512], p[0:8, :], AF.Exp, [tp, t_nbf], [t_ex], scale=-1.0, bias=nbf[:, 1:2])
                    act(ex[:], ex[:], AF.Ln, [t_ex], [t_ex], bias=1.0)
                    S.op("dve", lambda e: e.tensor_tensor_scan(out=cn[:], data0=onesf[:], data1=ex[:], initial=0.0, op0=ALU.mult, op1=ALU.add),
                         r=[t_ex, t_of], w=[t_cn])
                    cp("dve", cbs[0][:], cn[:], [t_cn], [t_cb[0]])
                    tt("dve", rr_[:], cn[:], cbs[0][:], ALU.subtract, [t_cn, t_cb[0]], [t_rr])
                    cp("dve", cbs[1][:], rr_[:], [t_rr], [t_cb[1]])
                    tt("dve", rr_[:], rr_[:], cbs[1][:], ALU.subtract, [t_rr, t_cb[1]], [t_rr])
                    cp("dve", cbs[2][:], rr_[:], [t_rr], [t_cb[2]])
                    for j in range(3):
                        ts("dve", nbs[j][:], cbs[j][:], -1.0, None, ALU.mult, None, [t_cb[j]], [t_nb[j]])
                        dma("sp", ex_d[1, :, 3 + j, :], cbs[j][:], r=[t_cb[j]])
                        dma("sp", ex_d[0, :, j, :], nbs[j][:], r=[t_nb[j]])
                        dma("sp", ex_d[1, :, j, :], onesb[:], r=[t_ob])
                        dma("sp", ex_d[0, :, 3 + j, :], onesb[:], r=[t_ob])
                    S.flush()
                qh = [sb(k, [128, T], BF16) for _ in range(2)]; t_qh = [Tok(), Tok()]; t_qx = [Tok(), Tok()]; t_kx = [Tok(), Tok()]
                kh = [sb(k, [128, T], BF16) for _ in range(2)]; t_kh = [Tok(), Tok()]
                vaug = sb(k, [128, 16, 8, 128], BF16); t_v = Tok()
                wv4 = sb(k, [128, 16, 512], BF16); t_wv4 = Tok()
                dma("pool", wv4[:], win[:, B0 + 1024:B0 + 1536].rearrange("(kc p) n -> p kc n", p=128), w=[t_wv4], ndesc=128)
                ybT = Ring([sb(k, [128, T], BF16) for _ in range(2)])
                PT = Ring([sb(k, [128, 512], BF16) for _ in range(3)])
                rz = Ring([sb(k, [64, 512]) for _ in range(2)])
                pso = Ring([pst(k, [128, 512]) for _ in range(4)])
                for hl_ in range(8):
                    memset("pool", vaug[:, :, hl_, 64:128], 1.0, [t_v])

                def evv_all(tc, p, tp):
                    act(vaug[:, tc, :, 0:64], p[:, 0:512].rearrange("p (h d) -> p h d", h=8), AF.Copy, [tp], [t_v])
                proj_tm(xnT, xtok, wv4, t_wv4, 512, 16, psr, evv_all)
                def ldb(hp_):
                    return (load_w(wr, win, B0 + hp_ * 128, 128), load_w(wr, win, B0 + 512 + hp_ * 128, 128))
                nxtw = ldb(0)
                for hp in range(4):
                    yb, t_yb = ybT.next()
                    for hl in range(2):
                        h = hp * 2 + hl
                        dma("sp", qh[hl][64:70, :], ex_d[0, h], w=[t_qx[hl]])
                        dma("sp", kh[hl][64:70, :], ex_d[1, h], w=[t_kx[hl]])
                    (wq, twq), (wk, twk) = nxtw
                    if hp + 1 < 4:
                        nxtw = ldb(hp + 1)

                    def evq(tti, p, tp):
                        for hl in range(2):
                            act(qh[hl][0:64, tti * 512:(tti + 1) * 512], p[hl * 64:(hl + 1) * 64, :], AF.Copy, [tp], [t_qh[hl]], scale=0.125)

                    def evk(tti, p, tp):
                        for hl in range(2):
                            cp("dve", kh[hl][0:64, tti * 512:(tti + 1) * 512], p[hl * 64:(hl + 1) * 64, :], [tp], [t_kh[hl]])

                    proj_fm(xnT, xtok, wq, twq, 0, 128, 16, psr, evq)
                    proj_fm(xnT, xtok, wk, twk, 0, 128, 16, psr, evk)
                    for hl in range(2):
                        for i in range(4):
                            po, tpo = pso.next()
                            nj = 4 * i + 4

                            def s_stage(j, i=i, hl=hl):
                                t0 = max(i * 512, j * 128)
                                ncol = (i + 1) * 512 - t0
                                c0 = t0 - i * 512
                                p, tp = psr.next()
                                mm(S, p[:, 0:ncol], [(kh[hl][0:70, j * 128:(j + 1) * 128], qh[hl][0:70, t0:t0 + ncol])],
                                   r=[t_kh[hl], t_qh[hl], t_kx[hl], t_qx[hl]], w=[tp])
                                pt_, tpt = PT.next()
                                act(pt_[:, 0:ncol], p[:, 0:ncol], AF.Exp, [tp], [tpt])
                                if j >= 4 * i:
                                    S.op("pool", lambda e, pt_=pt_: e.affine_select(out=pt_[:, 0:128], in_=pt_[:, 0:128], pattern=[[1, 128]],
                                                                                   compare_op=ALU.is_ge, fill=0.0, base=0, channel_multiplier=-1),
                                         r=[tpt], w=[tpt])
                                return (j, pt_, tpt, c0, ncol)

                            def pv_stage(st_, hl=hl, po=po, tpo=tpo, nj=nj, hp=hp):
                                j, pt_, tpt, c0, ncol = st_
                                mm(S, po[:, c0:c0 + ncol], [(vaug[:, j, hp * 2 + hl, :], pt_[:, 0:ncol])],
                                   r=[t_v, tpt], w=[tpo], start=(j == 0), stop=(j == nj - 1))
                            prev = None
                            for j in range(nj):
                                cur = s_stage(j)
                                if prev is not None:
                                    pv_stage(prev)
                                prev = cur
                            pv_stage(prev)
                            rz_, trz = rz.next()
                            act(rz_[:], po[64:128, :], AF.Copy, [tpo], [trz])
                            recip(rz_[:], rz_[:], [trz], [trz])
                            tt("dve", yb[hl * 64:(hl + 1) * 64, i * 512:(i + 1) * 512], po[0:64, :], rz_[:], ALU.mult, [tpo, trz], [t_yb])
                    dma("sp", yT_d[512 + hp * 128:512 + (hp + 1) * 128, :], yb[:], r=[t_yb])
                S.flush()

        def mixer_c(l, win, xnT, xtok):
            with ExitStack() as k:
                wr = Ring([sb(k, [128, 16, 128], BF16) for _ in range(8)])
                psr = Ring([pst(k, [128, 512]) for _ in range(2)])
                ptr = Ring([pst(k, [128, 1024], BF16) for _ in range(1)])
                pmy = pst(k, [128, 512])
                psA = Ring([pst(k, [128, 512])[:, 0:128]])
                psU = Ring([pst(k, [128, 512])[:, 0:128] for _ in range(2)])
                psO = Ring([pmy[:, 0:128], pst(k, [128, 512])[:, 0:128]])
                t_pmy = psO.items[0][1]
                lbt = sb(k, [128, 16]); t_lb = Tok()
                gn = sb(k, [128, 1]); t_gn = Tok()
                onesf = sb(k, [128, T]); t_of = Tok()
                qf = sb(k, [128, T]); t_qf = Tok()
                bA = sb(k, [128, T]); t_A = Tok()
                bB = sb(k, [128, T]); t_B = Tok()
                bG = sb(k, [128, T]); t_G = Tok()
                dd = sb(k, [128, 32]); t_dd = Tok()
                qtil = sb(k, [128, T], BF16); t_qt = Tok()
                ktil = sb(k, [128, T], BF16); t_kt = Tok()
                ktok = [sb(k, [128, 16, 128], BF16) for _ in range(2)]; t_ktok = [Tok(), Tok()]
                vtok = sb(k, [128, 16, 128], BF16); t_v = Tok()
                gate = sb(k, [128, T], BF16); t_gate = Tok()
                oT = sb(k, [128, T]); t_oT = Tok()
                AT = Ring([sb(k, [128, 2, 64], BF16) for _ in range(2)])
                Tst = sb(k, [128, 128]); t_T = Tok()
                Sb = Ring([sb(k, [128, 128], BF16) for _ in range(2)])
                sq = (sb(k, [128, 512], BF16), Tok())
                rs_t = sb(k, [128, 512]); t_rs = Tok()
                t1 = sb(k, [128, 512]); t_t1 = Tok()
                ycT = Ring([sb(k, [128, T], BF16) for _ in range(2)])
                lbin = sb(k, [8, 128]); t_lbin = Tok()
                identf = sb(k, [128, 128]); t_idf = Tok()
                rm = sb(k, [128, 2]); t_rm = Tok()
                tri2 = sb(k, [128, 2, 64]); t_tri = Tok()
                dma("sp", identf[:], c_identf, w=[t_idf])
                dma("sp", rm[:], c_rm, w=[t_rm])
                dma("sp", tri2[:], c_tri2, w=[t_tri])
                dma("sp", lbin[:], hgrn_lb.rearrange("l (c p) -> (l c) p", p=128), w=[t_lbin])
                S.op("pe", lambda e: e.transpose(out=pmy[:, 256:264], in_=lbin[:], identity=identf[0:8, 0:8]), r=[t_lbin, t_idf], w=[t_pmy])
                cp("dve", lbt[:, 0:8], pmy[:, 256:264], [t_pmy], [t_lb])
                dma("sp", gn[:], hgrn_norm_g[l].rearrange("(p o) -> p o", o=1), w=[t_gn])
                memset("pool", onesf[:], 1.0, [t_of])
                if l == 0:
                    memset("dve", lbt[:, 8:12], 0.0, [t_lb])
                else:
                    tt("dve", lbt[:, 8:12], lbt[:, 4:8], lbt[:, 0:4], ALU.subtract, [t_lb], [t_lb])
                    act(lbt[:, 8:12], lbt[:, 8:12], AF.Sigmoid, [t_lb], [t_lb])
                ts("dve", lbt[:, 12:16], lbt[:, 8:12], -1.0, 1.0, ALU.mult, ALU.add, [t_lb], [t_lb])
                def ldc(h_):
                    return (load_w(wr, win, C0 + h_ * 128, 128), load_w(wr, win, C0 + 512 + h_ * 128, 128),
                            load_w(wr, win, C0 + 1024 + h_ * 128, 128), load_w(wr, win, C0 + 1536 + h_ * 128, 128))
                nxtw = ldc(0)
                for h in range(4):
                    yc, t_yc = ycT.next()
                    (wq, twq), (wzz, twz), (wi, twi), (wg, twg) = nxtw
                    if h + 1 < 4:
                        nxtw = ldc(h + 1)

                    def evq(tti, p, tp):
                        act(qf[:, tti * 512:(tti + 1) * 512], p[:], AF.Copy, [tp], [t_qf], scale=128 ** -0.5)

                    def evz(tti, p, tp, h=h):
                        act(bA[:, tti * 512:(tti + 1) * 512], p[:], AF.Sigmoid, [tp], [t_A])

                    def evv(tc, p, tp):
                        cp("dve", vtok[:, tc, :], p[:, 0:128], [tp], [t_v])

                    def evg(tti, p, tp):
                        act(gate[:, tti * 512:(tti + 1) * 512], p[:], AF.Sigmoid, [tp], [t_gate])
                    proj_fm(xnT, xtok, wq, twq, 0, 128, 16, psr, evq)
                    proj_fm(xnT, xtok, wzz, twz, 0, 128, 16, psr, evz)
                    proj_tm(xnT, xtok, wi, twi, 128, 16, psr, evv)
                    proj_fm(xnT, xtok, wg, twg, 0, 128, 16, psr, evg)
                    ts("dve", bA[:], bA[:], lbt[:, 12 + h:13 + h], lbt[:, 8 + h:9 + h], ALU.mult, ALU.add, [t_A, t_lb], [t_A])
                    act(bB[:], bA[:], AF.Ln, [t_A], [t_B])
                    ts("dve", bA[:], bA[:], -1.0, 1.0, ALU.mult, ALU.add, [t_A], [t_A])
                    S.op("dve", lambda e: e.tensor_tensor_scan(out=bG[:], data0=onesf[:], data1=bB[:], initial=0.0, op0=ALU.mult, op1=ALU.add),
                         r=[t_B, t_of], w=[t_G])
                    G3 = bG[:].rearrange("p (c t) -> p c t", t=64)
                    E3 = bB[:].rearrange("p (c t) -> p c t", t=64)
                    tt("dve", E3, G3, G3[:, :, 31:32].broadcast_to([128, 32, 64]), ALU.subtract, [t_G], [t_B])
                    tt("dve", dd[:, 0:31], G3[:, 1:32, 31], G3[:, 0:31, 31], ALU.subtract, [t_G], [t_dd])
                    act(dd[:, 0:31], dd[:, 0:31], AF.Exp, [t_dd], [t_dd])
                    act(bG[:], bB[:], AF.Exp, [t_B], [t_G])
                    act(bB[:], bB[:], AF.Exp, [t_B], [t_B], scale=-1.0)
                    tt("dve", qtil[:], qf[:], bG[:], ALU.mult, [t_qf, t_G], [t_qt])
                    tt("dve", ktil[:], bA[:], bB[:], ALU.mult, [t_A, t_B], [t_kt])
                    for half in range(2):
                        p, tp = ptr.next()

                        def fn(e, p=p, half=half):
                            ins = None
                            for j in range(8):
                                c = (half * 8 + j) * 128
                                ins = e.transpose(out=p[:, j * 128:(j + 1) * 128], in_=ktil[:, c:c + 128], identity=ident[:])
                            return ins
                        S.op("pe", fn, r=[t_kt, t_ident], w=[tp])
                        for hf in range(2):
                            act(ktok[hf][:, half * 8:(half + 1) * 8, :], p[:].rearrange("p (k t) -> p k t", k=8), AF.Copy, [tp, t_rm], [t_ktok[hf]],
                                scale=rm[:, hf:hf + 1])
                    sbc = None
                    for tc in range(16):
                        pa, tpa = psA.next()
                        at, tat = AT.next()
                        for hf in range(2):
                            c = 2 * tc + hf
                            mm(S, pa[:, hf * 64:(hf + 1) * 64], [(ktil[:, tc * 128:(tc + 1) * 128], qtil[:, c * 64:(c + 1) * 64])],
                               r=[t_kt, t_qt], w=[tpa])
                        tt("dve", at[:], pa[:].rearrange("p (h t) -> p h t", h=2), tri2[:], ALU.mult, [tpa, t_tri], [tat])
                        po, tpo = psO.next()
                        for hf in range(2):
                            c = 2 * tc + hf
                            pu, tpu = psU.next()
                            mm(S, pu[:], [(ktok[hf][:, tc, :], vtok[:, tc, :])], r=[t_ktok[hf], t_v], w=[tpu])
                            mm(S, po[:, hf * 64:(hf + 1) * 64], [(vtok[:, tc, :], at[:, hf, :])], r=[t_v, tat], w=[tpo],
                               start=True, stop=(c == 0))
                            if c > 0:
                                mm(S, po[:, hf * 64:(hf + 1) * 64], [(sbc[0][:], qtil[:, c * 64:(c + 1) * 64])], r=[sbc[1], t_qt], w=[tpo],
                                   start=False, stop=True)
                            if c == 0:
                                cp("dve", Tst[:], pu[:], [tpu], [t_T])
                            else:
                                stt(Tst[:], Tst[:], dd[:, c - 1:c], pu[:], ALU.mult, ALU.add, [t_T, t_dd, tpu], [t_T])
                            if c < 31:
                                sbc = Sb.next()
                                ts("dve", sbc[0][:], Tst[:], dd[:, c:c + 1], None, ALU.mult, None, [t_T, t_dd], [sbc[1]])
                        act(oT[:, tc * 128:(tc + 1) * 128], po[:], AF.Copy, [tpo], [t_oT])
                    for tti in range(4):
                        cs = slice(tti * 512, (tti + 1) * 512)
                        rms_part(sq, oT[:, cs], 512, psr, [t_oT], rs_t, t_rs)
                        stt(t1[:], oT[:, cs], gn[:, 0:1], rs_t[:], ALU.mult, ALU.mult, [t_oT, t_gn, t_rs], [t_t1])
                        tt("dve", yc[:, cs], t1[:], gate[:, cs], ALU.mult, [t_t1, t_gate], [t_yc])
                    dma("sp", yT_d[1024 + h * 128:1024 + (h + 1) * 128, :], yc[:], r=[t_yc])
                S.flush()

        def mixer_d(l, win, xnT, xtok):
            lam_init = 0.8 - 0.6 * float(np.exp(-0.3 * l))
            with ExitStack() as k:
                wr = Ring([sb(k, [128, 16, 128], BF16) for _ in range(6)])
                psr = Ring([pst(k, [128, 512]) for _ in range(4)])
                pacc = [pst(k, [128, 512]) for _ in range(4)]; t_acc = [Tok() for _ in range(4)]
                cosT = sb(k, [128, T]); t_cos = Tok()
                sinT = sb(k, [128, T]); t_sin = Tok()
                mk = sb(k, [128, 2]); t_mk = Tok()
                lamt = sb(k, [128, 256]); t_lam = Tok()
                lw_ = sb(k, [128, 128]); t_lw = Tok()
                ls = sb(k, [128, 8]); t_ls = Tok()
                gD = sb(k, [128, 1]); t_gD = Tok()
                qr = sb(k, [128, T], BF16); t_qr = Tok()
                k1p = sb(k, [128, T], BF16); t_k1 = Tok()
                k2p = sb(k, [128, T], BF16); t_k2 = Tok()
                vtok = sb(k, [128, 16, 512], BF16); t_v = Tok()
                wv4 = sb(k, [128, 16, 512], BF16); t_wv4 = Tok()
                dma("pool", wv4[:], win[:, D0 + 1024:D0 + 1536].rearrange("(kc p) n -> p kc n", p=128), w=[t_wv4], ndesc=128)
                tA = Ring([sb(k, [128, 512]) for _ in range(2)])
                tB = Ring([sb(k, [128, 512]) for _ in range(2)])
                PT = Ring([sb(k, [128, 512], BF16) for _ in range(6)])
                r1r = Ring([sb(k, [128, 512]) for _ in range(2)])
                r2r = Ring([sb(k, [128, 512]) for _ in range(2)])
                oD = sb(k, [128, 512]); t_oD = Tok()
                sq = (sb(k, [128, 512], BF16), Tok())
                rs_t = sb(k, [128, 512]); t_rs = Tok()
                ydT = Ring([sb(k, [128, T], BF16) for _ in range(2)])
                dma("sp", cosT[:], c_cos, w=[t_cos])
                dma("sp", sinT[:], c_sinA, w=[t_sin])
                dma("sp", mk[:], c_mk, w=[t_mk])
                dma("sp", lamt[:], diff_lambda[l].partition_broadcast(128), w=[t_lam])
                dma("sp", gD[:], diff_norm_g[l].rearrange("(p o) -> p o", o=1), w=[t_gD])
                ts("dve", gD[:], gD[:], 1.0 - lam_init, None, ALU.mult, None, [t_gD], [t_gD])
                tt("dve", lw_[:, 0:64], lamt[:, 0:64], lamt[:, 64:128], ALU.mult, [t_lam], [t_lw])
                tt("dve", lw_[:, 64:128], lamt[:, 128:192], lamt[:, 192:256], ALU.mult, [t_lam], [t_lw])
                S.op("dve", lambda e: e.reduce_sum(out=ls[:, 0:1], in_=lw_[:, 0:64], axis=AX.X), r=[t_lw], w=[t_ls])
                S.op("dve", lambda e: e.reduce_sum(out=ls[:, 1:2], in_=lw_[:, 64:128], axis=AX.X), r=[t_lw], w=[t_ls])
                act(ls[:, 2:4], ls[:, 0:2], AF.Exp, [t_ls], [t_ls])
                tt("dve", ls[:, 4:5], ls[:, 3:4], ls[:, 2:3], ALU.subtract, [t_ls], [t_ls])
                ts("dve", ls[:, 5:6], ls[:, 4:5], -lam_init, None, ALU.add, None, [t_ls], [t_ls])
                def ldd(h_):
                    return (load_w(wr, win, D0 + h_ * 128, 128), load_w(wr, win, D0 + 512 + h_ * 128, 128))

                def evv_all(tc, p, tp):
                    act(vtok[:, tc, :], p[:, 0:512], AF.Copy, [tp], [t_v])
                proj_tm(xnT, xtok, wv4, t_wv4, 512, 16, psr, evv_all)
                nxtw = ldd(0)
                for h in range(4):
                    yd, t_yd = ydT.next()
                    (wq, twq), (wk, twk) = nxtw
                    if h + 1 < 4:
                        nxtw = ldd(h + 1)

                    def rope(tti, p, tp, dst):
                        cs = slice(tti * 512, (tti + 1) * 512)
                        a_, ta_ = tA.next()
                        b_, tb_ = tB.next()
                        tt("dve", a_[:], p[:], cosT[:, cs], ALU.mult, [tp, t_cos], [ta_])
                        tt("dve", b_[0:64, :], p[64:128, :], sinT[64:128, cs], ALU.mult, [tp, t_sin], [tb_])
                        tt("dve", b_[64:128, :], p[0:64, :], sinT[0:64, cs], ALU.mult, [tp, t_sin], [tb_])
                        return a_, ta_, b_, tb_, cs

                    def evq(tti, p, tp):
                        a_, ta_, b_, tb_, cs = rope(tti, p, tp, None)
                        tt("dve", qr[:, cs], a_[:], b_[:], ALU.add, [ta_, tb_], [t_qr])

                    def evk(tti, p, tp):
                        a_, ta_, b_, tb_, cs = rope(tti, p, tp, None)
                        tt("dve", a_[:], a_[:], b_[:], ALU.add, [ta_, tb_], [ta_])
                        act(k1p[:, cs], a_[:], AF.Copy, [ta_, t_mk], [t_k1], scale=mk[:, 0:1])
                        act(k2p[:, cs], a_[:], AF.Copy, [ta_, t_mk], [t_k2], scale=mk[:, 1:2])

                    proj_fm(xnT, xtok, wq, twq, 0, 128, 16, psr, evq)
                    proj_fm(xnT, xtok, wk, twk, 0, 128, 16, psr, evk)
                    pend = None
                    for i in range(4):
                        nj = 4 * i + 4

                        def s_stage(j, i=i):
                            t0 = max(i * 512, j * 128)
                            ncol = (i + 1) * 512 - t0
                            c0 = t0 - i * 512
                            res_ = []
                            for m, (kp, tkp) in enumerate(((k1p, t_k1), (k2p, t_k2))):
                                p, tp = psr.next()
                                mm(S, p[:, 0:ncol], [(kp[:, j * 128:(j + 1) * 128], qr[:, t0:t0 + ncol])], r=[tkp, t_qr], w=[tp])
                                pt_, tpt = PT.next()
                                act(pt_[:, 0:ncol], p[:, 0:ncol], AF.Exp, [tp], [tpt], scale=0.125)
                                if j >= 4 * i:
                                    memset("pool", pt_[64:128, 0:64], 0.0, [tpt])
                                res_.append((pt_, tpt))
                            return (j, res_, c0, ncol)

                        def pv_stage(st_, nj=nj, h=h):
                            j, res_, c0, ncol = st_
                            for m, (pt_, tpt) in enumerate(res_):
                                mm(S, pacc[2 * m][:, c0:c0 + ncol], [(vtok[:, j, h * 128:(h + 1) * 128], pt_[:, 0:ncol])], r=[t_v, tpt], w=[t_acc[2 * m]],
                                   start=(j == 0), stop=(j == nj - 1))
                                mm(S, pacc[2 * m + 1][:, c0:c0 + ncol], [(ones_bf[:], pt_[:, 0:ncol])], r=[t_ones, tpt], w=[t_acc[2 * m + 1]],
                                   start=(j == 0), stop=(j == nj - 1))
                        def fin2(i_, r1, t_r1, r2, t_r2, yd=yd, t_yd=t_yd):
                            cs = slice(i_ * 512, (i_ + 1) * 512)
                            stt(oD[:], r2[:], ls[:, 5:6], r1[:], ALU.mult, ALU.add, [t_r1, t_r2, t_ls], [t_oD])
                            rms_part(sq, oD[:], 512, psr, [t_oD], rs_t, t_rs)
                            stt(yd[:, cs], oD[:], gD[:, 0:1], rs_t[:], ALU.mult, ALU.mult, [t_oD, t_gD, t_rs], [t_yd])
                        prev = None
                        for j in range(nj):
                            cur = s_stage(j)
                            if prev is not None:
                                pv_stage(prev)
                            prev = cur
                            if j == 1 and pend is not None:
                                fin2(*pend)
                                pend = None
                        pv_stage(prev)
                        r1, t_r1 = r1r.next()
                        r2, t_r2 = r2r.next()
                        recip(r1[:], pacc[1][:], [t_acc[1]], [t_r1])
                        tt("dve", r1[:], pacc[0][:], r1[:], ALU.mult, [t_acc[0], t_r1], [t_r1])
                        recip(r2[:], pacc[3][:], [t_acc[3]], [t_r2])
                        tt("dve", r2[:], pacc[2][:], r2[:], ALU.mult, [t_acc[2], t_r2], [t_r2])
                        pend = (i, r1, t_r1, r2, t_r2)
                    fin2(*pend)
                    pend = None
                    dma("sp", yT_d[1536 + h * 128:1536 + (h + 1) * 128, :], yd[:], r=[t_yd])
                S.flush()

        def phase_gate(l, xnT, xtok):
            with ExitStack() as k:
                yall = sb(k, [128, 16, T], BF16); t_y = Tok()
                wgr = Ring([sb(k, [128, 16, 128], BF16) for _ in range(3)])
                wbr = Ring([sb(k, [128, 4, 128], BF16) for _ in range(3)])
                bg = sb(k, [128, 4, 16]); t_bg = Tok()
                psg = Ring([pst(k, [128, 512]) for _ in range(4)])
                psb = Ring([pst(k, [128, 512]) for _ in range(4)])
                gt = Ring([sb(k, [128, 512]) for _ in range(3)])
                tmp = Ring([sb(k, [128, 512]) for _ in range(3)])
                accs = [sb(k, [128, 512]) for _ in range(4)]; t_accs = [Tok() for _ in range(4)]
                mo = Ring([sb(k, [128, T], BF16) for _ in range(2)])
                for q4 in range(4):
                    dma("sp", yall[:, q4 * 4:(q4 + 1) * 4, :], yT_d[q4 * 512:(q4 + 1) * 512, :].rearrange("(kc p) t -> p kc t", p=128), w=[t_y])
                bgin = sb(k, [64, 128]); t_bgin = Tok()
                identf = sb(k, [128, 128]); t_idf = Tok()
                dma("sp", identf[:], c_identf, w=[t_idf])
                dma("sp", bgin[:], b_gate[l].rearrange("n (oc p) -> (n oc) p", p=128), w=[t_bgin])
                p0, tp0 = psg.next()
                S.op("pe", lambda e: e.transpose(out=p0[:, 0:64], in_=bgin[:], identity=identf[0:64, 0:64]), r=[t_bgin, t_idf], w=[tp0])
                cp("dve", bg[:], p0[:, 0:64].rearrange("p (n oc) -> p n oc", n=4), [tp0], [t_bg])
                def ldg(i_):
                    oc_, n_ = divmod(i_, 4)
                    return load_w(wgr, w_gate[l, n_], oc_ * 128, 128), load_w(wbr, w_branch[l, n_], oc_ * 128, 128, KC=4)
                nxtw = ldg(0)
                for oc in range(16):
                    m_, tm_ = mo.next()
                    for n in range(4):
                        (wg, twg), (wb_, twb) = nxtw
                        if oc * 4 + n + 1 < 64:
                            nxtw = ldg(oc * 4 + n + 1)
                        for tti in range(4):
                            cs = slice(tti * 512, (tti + 1) * 512)
                            pg, tpg = psg.next()
                            pb, tpb = psb.next()
                            mm(S, pg[:], [(wg[:, kc, :], xnT[:, kc, cs]) for kc in range(16)], r=[xtok, twg], w=[tpg])
                            mm(S, pb[:], [(wb_[:, kc, :], yall[:, n * 4 + kc, cs]) for kc in range(4)], r=[t_y, twb], w=[tpb])
                            g_, tg_ = gt.next()
                            act(g_[:], pg[:], AF.Sigmoid, [tpg, t_bg], [tg_], bias=bg[:, n, oc:oc + 1])
                            if n == 0:
                                tt("dve", accs[tti][:], g_[:], pb[:], ALU.mult, [tg_, tpb], [t_accs[tti]])
                            else:
                                t_, tt_ = tmp.next()
                                tt("dve", t_[:], g_[:], pb[:], ALU.mult, [tg_, tpb], [tt_])
                                if n < 3:
                                    tt("pool", accs[tti][:], accs[tti][:], t_[:], ALU.add, [t_accs[tti], tt_], [t_accs[tti]])
                                else:
                                    tt("pool", m_[:, cs], accs[tti][:], t_[:], ALU.add, [t_accs[tti], tt_], [tm_])
                    dma("sp", mixT_d[oc * 128:(oc + 1) * 128, :], m_[:], r=[tm_])
                S.flush()

        def phase_wout(l, src_rows):
            with ExitStack() as k:
                mT = sb(k, [128, 16, T], BF16); t_m = Tok()
                wo = sb(k, [128, 16, D], BF16); t_wo = [Tok() for _ in range(4)]
                xr = Ring([sb(k, [128, D]) for _ in range(2)])
                xo = Ring([sb(k, [128, D]) for _ in range(2)])
                psr = Ring([pst(k, [128, 512]) for _ in range(4)])
                for q4 in range(4):
                    dma("sp", mT[:, q4 * 4:(q4 + 1) * 4, :], mixT_d[q4 * 512:(q4 + 1) * 512, :].rearrange("(kc p) t -> p kc t", p=128), w=[t_m])
                for ct in range(4):
                    dma("pool", wo[:, :, ct * 512:(ct + 1) * 512], w_out[l][:, ct * 512:(ct + 1) * 512].rearrange("(kc p) n -> p kc n", p=128), w=[t_wo[ct]], ndesc=128)
                for tc in range(16):
                    xi, txi = xr.next()
                    xo_, txo = xo.next()
                    dma("sp", xi[:], src_rows[tc * 128:(tc + 1) * 128, :], w=[txi])
                    for ct in range(4):
                        cs = slice(ct * 512, (ct + 1) * 512)
                        p, tp = psr.next()
                        mm(S, p[:], [(mT[:, kc, tc * 128:(tc + 1) * 128], wo[:, kc, cs]) for kc in range(16)], r=[t_m, t_wo[ct]], w=[tp])
                        tt("dve", xo_[:, cs], p[:], xi[:, cs], ALU.add, [tp, txi], [txo])
                    dma("sp", xa_d[tc * 128:(tc + 1) * 128, :], xo_[:], r=[txo])
                S.flush()

        def phase_ffn_up(l, hnT, htok):
            with ExitStack() as k:
                wr = Ring([sb(k, [128, 16, 128], BF16) for _ in range(4)])
                cwin = sb(k, [88, 4, 128]); t_cwin = Tok()
                cw = sb(k, [128, 4, 88]); t_cw = Tok()
                identf = sb(k, [128, 128]); t_idf = Tok()
                ha = Ring([sb(k, [128, T + 2]) for _ in range(2)])
                hg = Ring([sb(k, [128, T + 2]) for _ in range(2)])
                ca = Ring([sb(k, [128, T]) for _ in range(2)])
                cg = Ring([sb(k, [128, T]) for _ in range(2)])
                ao = Ring([sb(k, [128, T], BF16) for _ in range(2)])
                psa = Ring([pst(k, [128, 512]) for _ in range(4)])
                psg = Ring([pst(k, [128, 512]) for _ in range(4)])
                dma("sp", identf[:], c_identf, w=[t_idf])
                for kk in range(3):
                    dma("sp", cwin[:, kk, :], ffn_conv_w[l, kk].rearrange("(c p) -> c p", p=128), w=[t_cwin])
                dma("sp", cwin[:, 3, :], ffn_conv_b[l].rearrange("(c p) -> c p", p=128), w=[t_cwin])
                p0, tp0 = psa.next()

                def fnT(e):
                    ins = None
                    for kk in range(4):
                        ins = e.transpose(out=p0[:, kk * 88:(kk + 1) * 88], in_=cwin[:, kk, :], identity=identf[0:88, 0:88])
                    return ins
                S.op("pe", fnT, r=[t_cwin, t_idf], w=[tp0])
                cp("dve", cw[:], p0[:, 0:352].rearrange("p (k c) -> p k c", k=4), [tp0], [t_cw])
                for r_ in (ha, hg):
                    for (tb, ttk) in r_.items:
                        memset("pool", tb[:, 0:2], 0.0, [ttk])
                wup = ffn_w_up[l]
                def ldw(j):
                    return load_w(wr, wup, j * 128, 128), load_w(wr, wup, (44 + j) * 128, 128)
                nxtw = ldw(0)
                for j in range(44):
                    (wa, twa), (wg, twg) = nxtw
                    if j + 1 < 44:
                        nxtw = ldw(j + 1)
                    ha_, tha = ha.next()
                    hg_, thg = hg.next()
                    for tti in range(4):
                        cs = slice(tti * 512, (tti + 1) * 512)
                        pa, tpa = psa.next()
                        pg, tpg = psg.next()
                        mm(S, pa[:], [(wa[:, kc, :], hnT[:, kc, cs]) for kc in range(16)], r=[htok, twa], w=[tpa])
                        mm(S, pg[:], [(wg[:, kc, :], hnT[:, kc, cs]) for kc in range(16)], r=[htok, twg], w=[tpg])
                        act(ha_[:, 2 + tti * 512:2 + (tti + 1) * 512], pa[:], AF.Copy, [tpa], [tha])
                        act(hg_[:, 2 + tti * 512:2 + (tti + 1) * 512], pg[:], AF.Copy, [tpg], [thg])
                    ca_, tca = ca.next()
                    cg_, tcg = cg.next()
                    ao_, tao = ao.next()
                    for (hb, thb, c_, tc_, jj) in ((ha_, tha, ca_, tca, j), (hg_, thg, cg_, tcg, 44 + j)):
                        act(c_[:], hb[:, 2:T + 2], AF.Identity, [thb, t_cw], [tc_], scale=cw[:, 2, jj:jj + 1], bias=cw[:, 3, jj:jj + 1])
                        stt(c_[:], hb[:, 1:T + 1], cw[:, 1, jj:jj + 1], c_[:], ALU.mult, ALU.add, [thb, t_cw, tc_], [tc_])
                        stt(c_[:], hb[:, 0:T], cw[:, 0, jj:jj + 1], c_[:], ALU.mult, ALU.add, [thb, t_cw, tc_], [tc_])
                    act(ca_[:], ca_[:], AF.Gelu_apprx_tanh, [tca], [tca])
                    tt("pool", ao_[:], ca_[:], cg_[:], ALU.mult, [tca, tcg], [tao])
                    dma("sp", actT_d[j * 128:(j + 1) * 128, :], ao_[:], r=[tao])
                S.flush()

        def phase_ffn_down(l, dst_rows):
            with ExitStack() as k:
                aT = sb(k, [128, 44, 1024], BF16); t_a = Tok()
                wd = Ring([sb(k, [128, 44, 256], BF16) for _ in range(2)])
                xr = Ring([sb(k, [128, 256]) for _ in range(4)])
                xo = Ring([sb(k, [128, 256]) for _ in range(4)])
                psr = Ring([pst(k, [128, 512]) for _ in range(4)])
                wdn = ffn_w_down[l]
                for half in range(2):
                    for q4 in range(4):
                        dma("sp", aT[:, q4 * 11:(q4 + 1) * 11, :],
                            actT_d[q4 * 1408:(q4 + 1) * 1408, half * 1024:(half + 1) * 1024].rearrange("(kc p) t -> p kc t", p=128), w=[t_a])
                    for ct in range(8):
                        w_, tw_ = load_w(wd, wdn, ct * 256, 256, KC=44)
                        for tc in range(8):
                            r0 = half * 1024 + tc * 128
                            xi, txi = xr.next()
                            xo_, txo = xo.next()
                            dma("sp", xi[:], xa_d[r0:r0 + 128, ct * 256:(ct + 1) * 256], w=[txi])
                            p, tp = psr.next()
                            mm(S, p[:, 0:256], [(aT[:, kc, tc * 128:(tc + 1) * 128], w_[:, kc, :]) for kc in range(44)], r=[t_a, tw_], w=[tp])
                            tt("dve", xo_[:], p[:, 0:256], xi[:], ALU.add, [tp, txi], [txo])
                            dma("sp", dst_rows[r0:r0 + 128, ct * 256:(ct + 1) * 256], xo_[:], r=[txo])
                S.flush()

        def phase_final(src, dst):
            with ExitStack() as k:
                Gt = sb(k, [128, D]); tG = Tok()
                xin = Ring([sb(k, [128, D]) for _ in range(3)])
                xs = Ring([sb(k, [128, D]) for _ in range(3)])
                junk = sb(k, [128, D], BF16); tj = Tok()
                st = Ring([sb(k, [128, 4]) for _ in range(3)])
                dma("sp", Gt[:], norm_final_g.partition_broadcast(128), w=[tG])

                def fa(tc):
                    xi, txi = xin.next()
                    xo, txo = xs.next()
                    sv, tsv = st.next()
                    dma("sp", xi[:], src[tc * 128:(tc + 1) * 128, :], w=[txi])
                    act(junk[:], xi[:], AF.Square, [txi], [tj, tsv], accum_out=sv[:, 0:1])
                    act(sv[:, 1:2], sv[:, 0:1], AF.Sqrt, [tsv, t_eps], [tsv], scale=1.0 / D, bias=epst[:])
                    recip(sv[:, 2:3], sv[:, 1:2], [tsv], [tsv])
                    stt(xo[:], xi[:], sv[:, 2:3], Gt[:], ALU.mult, ALU.mult, [txi, tsv, tG], [txo])
                    return xo, txo
                cur = fa(0)
                for tc in range(16):
                    nxt = fa(tc + 1) if tc + 1 < 16 else None
                    dma("sp", dst[tc * 128:(tc + 1) * 128, :], cur[0][:], r=[cur[1]])
                    cur = nxt
                S.flush()

        scr = [xb_d, xc_d]
        for s in range(NS):
            src = x_in[s]
            for l in range(NLAY):
                dst = scr[l % 2]
                layer(l, src, dst)
                src = dst
            if STOP >= 10: phase_final(src, out[s])
    return nc


def _constants():
    ident = np.eye(128, dtype=np.float32)
    half = 32
    inv_freq = (10000.0 ** (-np.arange(half, dtype=np.float32) / half)).astype(np.float32)
    pos = np.arange(T, dtype=np.float32)
    ang = (pos[None, :] * inv_freq[:, None]).astype(np.float32)
    cos = np.cos(ang).astype(np.float32)
    sin = np.sin(ang).astype(np.float32)
    c_cos = np.tile(cos, (4, 1))
    c_sinA = np.concatenate([sin, sin, -sin, -sin], axis=0)
    tri = (np.arange(64)[None, :] >= (np.arange(128)[:, None] % 64)).astype(np.float32)
    g = (np.arange(128) // 32) % 2
    mk = np.stack([(g == 0), (g == 1)], axis=1).astype(np.float32)
    return {
        "c_ident": ident.astype(ml_dtypes.bfloat16),
        "c_identf": ident,
        "c_cos": np.ascontiguousarray(c_cos),
        "c_sinA": np.ascontiguousarray(c_sinA),
        "c_tri": tri,
        "c_mk": mk,
        "c_rm": np.stack([np.arange(128) < 64, np.arange(128) >= 64], axis=1).astype(np.float32),
        "c_tri2": np.ascontiguousarray(np.stack([tri * (np.arange(128)[:, None] < 64), tri * (np.arange(128)[:, None] >= 64)], axis=1).astype(np.float32)),
    }


def _perm_cols():
    r = np.arange(128)
    comp = (r // 32) % 2
    part = r // 64
    return comp * 64 + part * 32 + (r % 32)


def _prep_shared(inp):
    f = lambda a: np.ascontiguousarray(np.asarray(a, dtype=np.float32))
    w_in = f(inp["w_in"]).copy()
    pc = _perm_cols()
    for base in (D0, D0 + 512):
        for h in range(4):
            c = base + h * 128
            w_in[:, :, c:c + 128] = w_in[:, :, c + pc]
    m = {
        "norm_mix_g": f(inp["norm_mix_g"]), "w_in": w_in, "fox_b_f": f(inp["fox_b_f"]),
        "gmlp_ln_g": f(inp["gmlp_ln_g"]), "gmlp_ln_b": f(inp["gmlp_ln_b"]),
        "gmlp_w_sT": f(np.transpose(np.asarray(inp["gmlp_w_s"]), (0, 1, 3, 2))),
        "gmlp_b_s": f(np.asarray(inp["gmlp_b_s"]).reshape(DEPTH, 512)),
        "hgrn_lb_logits": f(inp["hgrn_lb_logits"]), "hgrn_norm_g": f(inp["hgrn_norm_g"]),
        "diff_lambda": f(np.asarray(inp["diff_lambda"]).reshape(DEPTH, 256)), "diff_norm_g": f(inp["diff_norm_g"]),
        "w_branch": f(inp["w_branch"]), "w_gate": f(inp["w_gate"]), "b_gate": f(inp["b_gate"]),
        "w_out": f(inp["w_out"]), "norm_ffn_g": f(inp["norm_ffn_g"]), "ffn_w_up": f(inp["ffn_w_up"]),
        "ffn_conv_w": f(inp["ffn_conv_w"]), "ffn_conv_b": f(inp["ffn_conv_b"]), "ffn_w_down": f(inp["ffn_w_down"]),
        "norm_final_g": f(inp["norm_final_g"]),
    }
    m.update(_constants())
    return m


def kernel(**inputs):
    x = np.ascontiguousarray(np.asarray(inputs["x"], dtype=np.float32))
    shared = _prep_shared(inputs)
    NS = x.shape[0] // NCORES
    nc = build_nc(NS=NS, NLAY=DEPTH, dbg=False)
    in_maps = []
    for c in range(NCORES):
        m = dict(shared)
        m["x"] = np.ascontiguousarray(x[c * NS:(c + 1) * NS])
        in_maps.append(m)
    res = run_bass_kernel_spmd(nc, in_maps, core_ids=list(range(NCORES)))
    return np.concatenate([np.asarray(r["out"], dtype=np.float32) for r in res.results], axis=0)
```

```python
import numpy as np
import ml_dtypes
import concourse.bass as bass
import concourse.mybir as mybir
from concourse.bass_utils import run_bass_kernel_spmd

F32 = mybir.dt.float32
BF16 = mybir.dt.bfloat16
ALU = mybir.AluOpType
AF = mybir.ActivationFunctionType
AX = mybir.AxisListType

D = 2048
T = 2048
DEPTH = 2
BW = 512
DFF = 5632
EPS = 1e-6
A0 = 0
B0 = 1024
C0 = 1024 + 1544
D0 = C0 + 2048
IN_COLS = 6152
NCORES = 8


class Tok:
    __slots__ = ("lw", "rd", "name")

    def __init__(self, name=""):
        self.lw = None
        self.rd = []
        self.name = name


class Op:
    __slots__ = ("eng", "fn", "deps", "sig", "val", "dma", "sem", "lane_wait")


CE = ("pe", "act", "dve", "pool")
FULLSYNC = True
NLANE = 12


class Sched:
    def __init__(self, nc):
        self.nc = nc
        self.sem = {e: nc.alloc_semaphore("c_" + e) for e in CE}
        self.cnt = {e: 0 for e in CE}
        self.lanes = {q: [[nc.alloc_semaphore("l_%s%d" % (q, i)), 0] for i in range(NLANE)] for q in ("sp", "pool")}
        self.rr = {"sp": 0, "pool": 0}
        self.ops = {e: [] for e in ("pe", "act", "dve", "pool", "sp")}
        self.toks = set()
        self.waited = {}
        self.nphase = 0
        self.pool_out = []

    def _deps(self, o, r, w):
        deps = []
        seen = set()

        def add(d, raw):
            if d is None or d is o or id(d) in seen:
                return
            if (not d.dma) and (not o.dma) and d.eng == o.eng and (d.eng == "pe" or (not raw and not FULLSYNC)):
                return
            seen.add(id(d))
            deps.append(d)
            if not d.dma:
                d.sig = True

        for t in r:
            add(t.lw, True)
        for t in w:
            add(t.lw, False)
            for d in t.rd:
                add(d, False)
        o.deps = deps
        for t in r:
            t.rd.append(o)
            self.toks.add(t)
        for t in w:
            t.lw = o
            t.rd = []
            self.toks.add(t)

    def op(self, eng, fn, r=(), w=()):
        o = Op()
        o.eng = eng
        o.fn = fn
        o.dma = False
        o.sig = False
        o.val = None
        o.sem = None
        o.lane_wait = 0
        self._deps(o, r, w)
        self.ops[eng].append(o)
        return o

    def dma(self, q, fn, r=(), w=(), ndesc=0):
        o = Op()
        o.eng = q
        o.fn = fn
        o.dma = True
        o.sig = True
        extra = []
        if q == "pool" and ndesc:
            while self.pool_out and sum(n for _, n in self.pool_out) + ndesc > 640:
                extra.append(self.pool_out.pop(0)[0])
            self.pool_out.append((o, ndesc))
        lane = self.lanes[q][self.rr[q]]
        self.rr[q] = (self.rr[q] + 1) % NLANE
        o.lane_wait = 16 * lane[1]
        lane[1] += 1
        o.sem = lane[0]
        o.val = 16 * lane[1]
        self._deps(o, r, w)
        for d in extra:
            if d not in o.deps:
                o.deps.append(d)
        self.ops[q].append(o)
        return o

    def flush(self):
        self.pool_out = []
        for e in CE:
            c = self.cnt[e]
            for o in self.ops[e]:
                if (not o.dma) and o.sig:
                    c += 1
                    o.val = c
            self.cnt[e] = c
        ops = self.ops
        self.ops = {e: [] for e in ("pe", "act", "dve", "pool", "sp")}
        waited = self.waited
        sems = self.sem
        lanes = self.lanes

        def emit(e, name):
            def w8(sem, val):
                key = (name, sem.num)
                if waited.get(key, 0) >= val:
                    return
                e.wait_ge(sem, val)
                waited[key] = val

            for o in ops[name]:
                for d in o.deps:
                    if d.dma:
                        w8(d.sem, d.val)
                    else:
                        w8(sems[d.eng], d.val)
                if o.dma and o.lane_wait:
                    w8(o.sem, o.lane_wait)
                ins = o.fn(e)
                if o.dma:
                    ins.then_inc(o.sem, 16)
                elif o.sig:
                    ins.then_inc(sems[o.eng], 1)
            if name in lanes:
                for sem, cnt in lanes[name]:
                    if cnt:
                        w8(sem, 16 * cnt)

        with self.nc.Block() as blk:
            @blk.sync
            def _(e):
                emit(e, "sp")

            @blk.gpsimd
            def _(e):
                emit(e, "pool")

            @blk.tensor
            def _(e):
                emit(e, "pe")

            @blk.scalar
            def _(e):
                emit(e, "act")

            @blk.vector
            def _(e):
                emit(e, "dve")
        for t in self.toks:
            t.lw = None
            t.rd = []
        self.toks = set()
        self.nphase += 1


class Ring:
    def __init__(self, items):
        self.items = [(t, Tok()) for t in items]
        self.i = 0

    def next(self):
        it = self.items[self.i]
        self.i = (self.i + 1) % len(self.items)
        return it


def mm(S, out_ap, pairs, r, w, start=True, stop=True):
    def fn(e, pairs=pairs, out_ap=out_ap, start=start, stop=stop):
        n = len(pairs)
        ins = None
        for i, (l, rh) in enumerate(pairs):
            ins = e.matmul(out_ap, l, rh, start=(start and i == 0), stop=(stop and i == n - 1))
        return ins
    return S.op("pe", fn, r=r, w=w)


from contextlib import ExitStack

_uid = [0]


def build_nc(NS=2, NLAY=2, dbg=False, STOP=99):
    nc = bass.Bass("TRN2", target_bir_lowering=False)

    def din(name, shape, dt=F32):
        return nc.dram_tensor(name, list(shape), dt, kind="ExternalInput").ap()

    x_in = din("x", [NS, T, D])
    norm_mix_g = din("norm_mix_g", [DEPTH, D])
    w_in = din("w_in", [DEPTH, D, IN_COLS])
    fox_b_f = din("fox_b_f", [DEPTH, 8])
    gmlp_ln_g = din("gmlp_ln_g", [DEPTH, BW])
    gmlp_ln_b = din("gmlp_ln_b", [DEPTH, BW])
    gmlp_w_sT = din("gmlp_w_sT", [DEPTH, 4, 128, 128])
    gmlp_b_s = din("gmlp_b_s", [DEPTH, 512])
    hgrn_lb = din("hgrn_lb_logits", [DEPTH, 512])
    hgrn_norm_g = din("hgrn_norm_g", [DEPTH, 128])
    diff_lambda = din("diff_lambda", [DEPTH, 256])
    diff_norm_g = din("diff_norm_g", [DEPTH, 128])
    w_branch = din("w_branch", [DEPTH, 4, BW, D])
    w_gate = din("w_gate", [DEPTH, 4, D, D])
    b_gate = din("b_gate", [DEPTH, 4, D])
    w_out = din("w_out", [DEPTH, D, D])
    norm_ffn_g = din("norm_ffn_g", [DEPTH, D])
    ffn_w_up = din("ffn_w_up", [DEPTH, D, 2 * DFF])
    ffn_conv_w = din("ffn_conv_w", [DEPTH, 3, 2 * DFF])
    ffn_conv_b = din("ffn_conv_b", [DEPTH, 2 * DFF])
    ffn_w_down = din("ffn_w_down", [DEPTH, DFF, D])
    norm_final_g = din("norm_final_g", [D])
    c_ident = din("c_ident", [128, 128], BF16)
    c_cos = din("c_cos", [128, T])
    c_sinA = din("c_sinA", [128, T])
    c_tri = din("c_tri", [128, 64])
    c_mk = din("c_mk", [128, 2])
    c_identf = din("c_identf", [128, 128])
    c_rm = din("c_rm", [128, 2])
    c_tri2 = din("c_tri2", [128, 2, 64])

    out = nc.dram_tensor("out", [NS, T, D], F32, kind="ExternalOutput").ap()
    okind = "ExternalOutput" if dbg else "Internal"

    def dscr(name, shape, dt):
        if dbg:
            return nc.dram_tensor(name, list(shape), dt, kind="ExternalOutput").ap()
        return nc.dram_tensor(name, list(shape), dt).ap()

    yT_d = dscr("yT_d", [D, T], BF16)
    mixT_d = dscr("mixT_d", [D, T], BF16)
    xa_d = dscr("xa_d", [T, D], F32)
    xb_d = dscr("xb_d", [T, D], F32)
    xc_d = dscr("xc_d", [T, D], F32)
    actT_d = dscr("actT_d", [DFF, T], BF16)
    ex_d = dscr("ex_d", [2, 8, 6, T], BF16)

    S = Sched(nc)

    def sb(k, shape, dt=F32):
        _uid[0] += 1
        return k.enter_context(nc.sbuf_tensor("s%d" % _uid[0], list(shape), dt))

    def pst(k, shape, dt=F32):
        _uid[0] += 1
        return k.enter_context(nc.psum_tensor("p%d" % _uid[0], list(shape), dt))

    def dma(q, out_ap, in_ap, r=(), w=(), slow=False, ndesc=0):
        if slow:
            return S.dma(q, lambda e, o=out_ap, i=in_ap: e.dma_start(out=o, in_=i, allow_slow_non_contiguous=True), r=r, w=w, ndesc=ndesc)
        return S.dma(q, lambda e, o=out_ap, i=in_ap: e.dma_start(out=o, in_=i), r=r, w=w, ndesc=ndesc)

    def act(out_ap, in_ap, func, r, w, **kw):
        return S.op("act", lambda e, o=out_ap, i=in_ap, f=func, kw=kw: e.activation(out=o, in_=i, func=f, **kw), r=r, w=w)

    def tt(eng, out_ap, a, b, op, r, w):
        return S.op(eng, lambda e, o=out_ap, a=a, b=b, op=op: e.tensor_tensor(out=o, in0=a, in1=b, op=op), r=r, w=w)

    def ts(eng, out_ap, a, s1, s2, op0, op1, r, w):
        if op1 is None:
            return S.op(eng, lambda e, o=out_ap, a=a, s1=s1, op0=op0: e.tensor_scalar(out=o, in0=a, scalar1=s1, scalar2=None, op0=op0), r=r, w=w)
        return S.op(eng, lambda e, o=out_ap, a=a, s1=s1, s2=s2, op0=op0, op1=op1: e.tensor_scalar(out=o, in0=a, scalar1=s1, scalar2=s2, op0=op0, op1=op1), r=r, w=w)

    def stt(out_ap, a, sc, b, op0, op1, r, w):
        return S.op("dve", lambda e, o=out_ap, a=a, sc=sc, b=b, op0=op0, op1=op1: e.scalar_tensor_tensor(out=o, in0=a, scalar=sc, in1=b, op0=op0, op1=op1), r=r, w=w)

    def recip(out_ap, in_ap, r, w):
        return S.op("dve", lambda e, o=out_ap, i=in_ap: e.reciprocal(out=o, in_=i), r=r, w=w)

    def cp(eng, out_ap, in_ap, r, w):
        return S.op(eng, lambda e, o=out_ap, i=in_ap: e.tensor_copy(out=o, in_=i), r=r, w=w)

    def memset(eng, ap, v, w):
        return S.op(eng, lambda e, ap=ap, v=v: e.memset(ap, v), w=w)

    G = ExitStack()
    with G:
        ident = sb(G, [128, 128], BF16); t_ident = Tok()
        ones_bf = sb(G, [128, 128], BF16); t_ones = Tok()
        epst = sb(G, [128, 1]); t_eps = Tok()
        dma("sp", ident[:], c_ident, w=[t_ident])
        memset("dve", ones_bf[:], 1.0, [t_ones])
        memset("dve", epst[:], EPS, [t_eps])
        CONST_R = [t_ident, t_ones, t_eps]

        def load_w(ring, wap, c0, ncol, KC=16):
            wt, tw = ring.next()
            dma("pool", wt[:, 0:KC, 0:ncol], wap[:, c0:c0 + ncol].rearrange("(kc p) n -> p kc n", p=128), w=[tw], ndesc=KC * 8)
            return wt, tw

        def proj_fm(xT, xtok, wt, tw, col_off, M, KC, psr, evac):
            for tti in range(4):
                p, tp = psr.next()
                pairs = [(wt[:, kc, col_off:col_off + M], xT[:, kc, tti * 512:(tti + 1) * 512]) for kc in range(KC)]
                mm(S, p[0:M, :], pairs, r=[xtok, tw], w=[tp])
                evac(tti, p, tp)

        def proj_tm(xT, xtok, wt, tw, ncol, KC, psr, evac):
            for tc in range(16):
                p, tp = psr.next()
                pairs = [(xT[:, kc, tc * 128:(tc + 1) * 128], wt[:, kc, 0:ncol]) for kc in range(KC)]
                mm(S, p[:, 0:ncol], pairs, r=[xtok, tw], w=[tp])
                evac(tc, p, tp)

        def phase_norm(src, gvec, xnT, xtok):
            with ExitStack() as k:
                Gt = sb(k, [128, D]); tG = Tok()
                xin = Ring([sb(k, [128, D]) for _ in range(3)])
                xs = Ring([sb(k, [128, D], BF16) for _ in range(3)])
                junk = sb(k, [128, D], BF16); tj = Tok()
                st = Ring([sb(k, [128, 4]) for _ in range(3)])
                ptr = Ring([pst(k, [128, 1024], BF16) for _ in range(4)])
                dma("sp", Gt[:], gvec.partition_broadcast(128), w=[tG])
                def stage_a(tc):
                    xi, txi = xin.next()
                    xo, txo = xs.next()
                    sv, tsv = st.next()
                    dma("sp", xi[:], src[tc * 128:(tc + 1) * 128, :], w=[txi])
                    act(junk[:], xi[:], AF.Square, [txi], [tj, tsv], accum_out=sv[:, 0:1])
                    act(sv[:, 1:2], sv[:, 0:1], AF.Sqrt, [tsv, t_eps], [tsv], scale=1.0 / D, bias=epst[:])
                    recip(sv[:, 2:3], sv[:, 1:2], [tsv], [tsv])
                    stt(xo[:], xi[:], sv[:, 2:3], Gt[:], ALU.mult, ALU.mult, [txi, tsv, tG], [txo])
                    return xo, txo

                def stage_b(tc, xo, txo):
                    for half in range(2):
                        p, tp = ptr.next()

                        def fn(e, p=p, xo=xo, half=half):
                            ins = None
                            for j in range(8):
                                c = (half * 8 + j) * 128
                                ins = e.transpose(out=p[:, j * 128:(j + 1) * 128], in_=xo[:, c:c + 128], identity=ident[:])
                            return ins
                        S.op("pe", fn, r=[txo, t_ident], w=[tp])
                        act(xnT[:, half * 8:(half + 1) * 8, tc * 128:(tc + 1) * 128],
                            p[:].rearrange("p (k t) -> p k t", k=8), AF.Copy, [tp], [xtok])
                cur = stage_a(0)
                for tc in range(16):
                    nxt = stage_a(tc + 1) if tc + 1 < 16 else None
                    stage_b(tc, *cur)
                    cur = nxt
                S.flush()

        def rms_part(k_sq, o_ap, ncol, psr, r_o, rs_t, t_rs):
            sq, tsq = k_sq
            act(sq[:, 0:ncol], o_ap, AF.Square, r_o, [tsq])
            p, tp = psr.next()
            mm(S, p[:, 0:ncol], [(ones_bf[:], sq[:, 0:ncol])], r=[tsq, t_ones], w=[tp])
            act(rs_t[:, 0:ncol], p[:, 0:ncol], AF.Sqrt, [tp, t_eps], [t_rs], scale=1.0 / 128, bias=epst[:])
            recip(rs_t[:, 0:ncol], rs_t[:, 0:ncol], [t_rs], [t_rs])

        def layer(l, src_rows, dst_rows):
            win = w_in[l]
            LK = ExitStack()
            with LK:
                xnT = sb(LK, [128, 16, T], BF16); xtok = Tok()
                phase_norm(src_rows, norm_mix_g[l], xnT, xtok)
                if STOP >= 2: mixer_a(l, win, xnT, xtok)
                if STOP >= 3: mixer_b(l, win, xnT, xtok)
                if STOP >= 4: mixer_c(l, win, xnT, xtok)
                if STOP >= 5: mixer_d(l, win, xnT, xtok)
                if STOP >= 6: phase_gate(l, xnT, xtok)
            if STOP >= 7: phase_wout(l, src_rows)
            if STOP >= 8:
                with ExitStack() as k2:
                    hnT = sb(k2, [128, 16, T], BF16); htok = Tok()
                    phase_norm(xa_d, norm_ffn_g[l], hnT, htok)
                    phase_ffn_up(l, hnT, htok)
            if STOP >= 9: phase_ffn_down(l, dst_rows)

        def mixer_a(l, win, xnT, xtok):
            with ExitStack() as k:
                wr = Ring([sb(k, [128, 16, 512], BF16) for _ in range(2)])
                uaT = sb(k, [128, 4, T]); t_ua = Tok()
                yaT = sb(k, [128, 4, T], BF16); t_ya = Tok()
                WsT = sb(k, [128, 4, 128], BF16); t_ws = Tok()
                Wsf = sb(k, [128, 4, 128]); t_wsf = Tok()
                BS = sb(k, [128, 512]); t_bs = Tok()
                Gl = sb(k, [128, 512]); t_gl = Tok()
                Bl = sb(k, [128, 512]); t_bl = Tok()
                vg = Ring([sb(k, [128, 512]) for _ in range(2)])
                vc = Ring([sb(k, [128, 512]) for _ in range(2)])
                vj = sb(k, [128, 512], BF16); t_vj = Tok()
                vn = Ring([sb(k, [128, 512], BF16) for _ in range(4)])
                sv_r = Ring([sb(k, [128, 8]) for _ in range(2)])
                mx = Ring([sb(k, [128, 512]) for _ in range(2)])
                psr = Ring([pst(k, [128, 512]) for _ in range(6)])
                dma("sp", Wsf[:], gmlp_w_sT[l].rearrange("g s t -> s g t"), w=[t_wsf])
                memset("pool", Wsf[64:128, :, 0:64], 0.0, [t_wsf])
                cp("pool", WsT[:], Wsf[:], [t_wsf], [t_ws])
                dma("sp", BS[:], gmlp_b_s[l].partition_broadcast(128), w=[t_bs])
                dma("sp", Gl[:], gmlp_ln_g[l].partition_broadcast(128), w=[t_gl])
                dma("sp", Bl[:], gmlp_ln_b[l].partition_broadcast(128), w=[t_bl])
                wt, tw = load_w(wr, win, A0, 512)
                for oc in range(4):
                    def ev(tti, p, tp, oc=oc):
                        act(uaT[:, oc, tti * 512:(tti + 1) * 512], p[:], AF.Gelu_apprx_tanh, [tp], [t_ua])
                    proj_fm(xnT, xtok, wt, tw, oc * 128, 128, 16, psr, ev)
                wt2, tw2 = load_w(wr, win, A0 + 512, 512)

                def ev2(tc, p, tp):
                    g_, tg_ = vg.next()
                    c_, tc_ = vc.next()
                    n_, tn_ = vn.next()
                    sv, tsv = sv_r.next()
                    act(g_[:], p[:], AF.Gelu_apprx_tanh, [tp], [tg_, tsv], accum_out=sv[:, 0:1])
                    ts("dve", sv[:, 1:2], sv[:, 0:1], 1.0 / 512, None, ALU.mult, None, [tsv], [tsv])
                    ts("dve", c_[:], g_[:], sv[:, 1:2], None, ALU.subtract, None, [tg_, tsv], [tc_])
                    act(vj[:], c_[:], AF.Square, [tc_], [t_vj, tsv], accum_out=sv[:, 2:3])
                    act(sv[:, 3:4], sv[:, 2:3], AF.Sqrt, [tsv, t_eps], [tsv], scale=1.0 / 512, bias=epst[:])
                    recip(sv[:, 4:5], sv[:, 3:4], [tsv], [tsv])
                    stt(c_[:], c_[:], sv[:, 4:5], Gl[:], ALU.mult, ALU.mult, [tc_, tsv, t_gl], [tc_])
                    tt("dve", n_[:], c_[:], Bl[:], ALU.add, [tc_, t_bl], [tn_])

                    def stage_b(tc=tc, n_=n_, tn_=tn_):
                        m_, tm_ = mx.next()
                        p2, tp2 = psr.next()

                        def fn(e, p2=p2, n_=n_):
                            ins = None
                            for g in range(4):
                                ins = e.matmul(p2[:, g * 128:(g + 1) * 128], n_[:, g * 128:(g + 1) * 128], WsT[:, g, :], start=True, stop=True)
                            return ins
                        S.op("pe", fn, r=[tn_, t_ws], w=[tp2])
                        tt("dve", m_[:], p2[:], BS[:], ALU.add, [tp2, t_bs], [tm_])
                        tt("dve", yaT[:, :, tc * 128:(tc + 1) * 128], m_[:].rearrange("p (g t) -> p g t", g=4),
                           uaT[:, :, tc * 128:(tc + 1) * 128], ALU.mult, [tm_, t_ua], [t_ya])
                    pendA.append(stage_b)
                    if len(pendA) > 2:
                        pendA.pop(0)()
                pendA = []
                proj_tm(xnT, xtok, wt2, tw2, 512, 16, psr, ev2)
                while pendA:
                    pendA.pop(0)()
                dma("sp", yT_d[0:512, :].rearrange("(g p) t -> p g t", p=128), yaT[:], r=[t_ya])
                S.flush()

        def mixer_b(l, win, xnT, xtok):
            with ExitStack() as k:
                wr = Ring([sb(k, [128, 16, 128], BF16) for _ in range(6)])
                wz = sb(k, [128, 16, 8], BF16); t_wz = Tok()
                psr = Ring([pst(k, [128, 512]) for _ in range(4)])
                with ExitStack() as k1:
                    cn = sb(k1, [8, T]); t_cn = Tok()
                    ex = sb(k1, [8, T]); t_ex = Tok()
                    rr_ = sb(k1, [8, T]); t_rr = Tok()
                    onesf = sb(k1, [8, T]); t_of = Tok()
                    nbf = sb(k1, [8, 2]); t_nbf = Tok()
                    cbs = [sb(k1, [8, T], BF16) for _ in range(3)]; t_cb = [Tok() for _ in range(3)]
                    nbs = [sb(k1, [8, T], BF16) for _ in range(3)]; t_nb = [Tok() for _ in range(3)]
                    onesb = sb(k1, [8, T], BF16); t_ob = Tok()
                    dma("pool", wz[:], win[:, B0 + 1536:B0 + 1544].rearrange("(kc p) n -> p kc n", p=128), w=[t_wz], ndesc=128)
                    dma("sp", nbf[:, 0:1], fox_b_f[l].rearrange("(h o) -> h o", o=1), w=[t_nbf])
                    ts("dve", nbf[:, 1:2], nbf[:, 0:1], -1.0, None, ALU.mult, None, [t_nbf], [t_nbf])
                    memset("pool", onesf[:], 1.0, [t_of])
                    memset("pool", onesb[:], 1.0, [t_ob])
                    for tti in range(4):
                        p, tp = psr.next()
                        pairs = [(wz[:, kc, :], xnT[:, kc, tti * 512:(tti + 1) * 512]) for kc in range(16)]
                        mm(S, p[0:8, :], pairs, r=[xtok, t_wz], w=[tp])
                        act(ex[:, tti * 512:(tti + 1) * 512], p[0:8, :], AF.Exp, [tp, t_nbf], [t_ex], scale=-1.0, bias=nbf[:, 1:2])
                    act(ex[:], ex[:], AF.Ln, [t_ex], [t_ex], bias=1.0)
                    S.op("dve", lambda e: e.tensor_tensor_scan(out=cn[:], data0=onesf[:], data1=ex[:], initial=0.0, op0=ALU.mult, op1=ALU.add),
                         r=[t_ex, t_of], w=[t_cn])
                    cp("dve", cbs[0][:], cn[:], [t_cn], [t_cb[0]])
                    tt("dve", rr_[:], cn[:], cbs[0][:], ALU.subtract, [t_cn, t_cb[0]], [t_rr])
                    cp("dve", cbs[1][:], rr_[:], [t_rr], [t_cb[1]])
                    tt("dve", rr_[:], rr_[:], cbs[1][:], ALU.subtract, [t_rr, t_cb[1]], [t_rr])
                    cp("dve", cbs[2][:], rr_[:], [t_rr], [t_cb[2]])
                    for j in range(3):
                        ts("dve", nbs[j][:], cbs[j][:], -1.0, None, ALU.mult, None, [t_cb[j]], [t_nb[j]])
                        dma("sp", ex_d[1, :, 3 + j, :], cbs[j][:], r=[t_cb[j]])
                        dma("sp", ex_d[0, :, j, :], nbs[j][:], r=[t_nb[j]])
                        dma("sp", ex_d[1, :, j, :], onesb[:], r=[t_ob])
                        dma("sp", ex_d[0, :, 3 + j, :], onesb[:], r=[t_ob])
                    S.flush()
                qh = [sb(k, [128, T], BF16) for _ in range(2)]; t_qh = [Tok(), Tok()]; t_qx = [Tok(), Tok()]; t_kx = [Tok(), Tok()]
                kh = [sb(k, [128, T], BF16) for _ in range(2)]; t_kh = [Tok(), Tok()]
                vaug = sb(k, [128, 16, 2, 128], BF16); t_v = Tok()
                ybT = Ring([sb(k, [128, T], BF16) for _ in range(2)])
                PT = Ring([sb(k, [128, 512], BF16) for _ in range(4)])
                rz = Ring([sb(k, [64, 512]) for _ in range(2)])
                pso = Ring([pst(k, [128, 512]) for _ in range(4)])
                for hl_ in range(2):
                    memset("pool", vaug[:, :, hl_, 64:128], 1.0, [t_v])
                def ldb(hp_):
                    return (load_w(wr, win, B0 + hp_ * 128, 128), load_w(wr, win, B0 + 512 + hp_ * 128, 128),
                            load_w(wr, win, B0 + 1024 + hp_ * 128, 128))
                nxtw = ldb(0)
                for hp in range(4):
                    yb, t_yb = ybT.next()
                    for hl in range(2):
                        h = hp * 2 + hl
                        dma("sp", qh[hl][64:70, :], ex_d[0, h], w=[t_qx[hl]])
                        dma("sp", kh[hl][64:70, :], ex_d[1, h], w=[t_kx[hl]])
                    (wq, twq), (wk, twk), (wv, twv) = nxtw
                    if hp + 1 < 4:
                        nxtw = ldb(hp + 1)

                    def evq(tti, p, tp):
                        for hl in range(2):
                            act(qh[hl][0:64, tti * 512:(tti + 1) * 512], p[hl * 64:(hl + 1) * 64, :], AF.Copy, [tp], [t_qh[hl]], scale=0.125)

                    def evk(tti, p, tp):
                        for hl in range(2):
                            cp("dve", kh[hl][0:64, tti * 512:(tti + 1) * 512], p[hl * 64:(hl + 1) * 64, :], [tp], [t_kh[hl]])

                    def evv(tc, p, tp):
                        act(vaug[:, tc, :, 0:64], p[:, 0:128].rearrange("p (h d) -> p h d", h=2), AF.Copy, [tp], [t_v])
                    proj_fm(xnT, xtok, wq, twq, 0, 128, 16, psr, evq)
                    proj_fm(xnT, xtok, wk, twk, 0, 128, 16, psr, evk)
                    proj_tm(xnT, xtok, wv, twv, 128, 16, psr, evv)
                    for hl in range(2):
                        for i in range(4):
                            po, tpo = pso.next()
                            nj = 4 * i + 4

                            def s_stage(j, i=i, hl=hl):
                                t0 = max(i * 512, j * 128)
                                ncol = (i + 1) * 512 - t0
                                c0 = t0 - i * 512
                                p, tp = psr.next()
                                mm(S, p[:, 0:ncol], [(kh[hl][0:70, j * 128:(j + 1) * 128], qh[hl][0:70, t0:t0 + ncol])],
                                   r=[t_kh[hl], t_qh[hl], t_kx[hl], t_qx[hl]], w=[tp])
                                pt_, tpt = PT.next()
                                act(pt_[:, 0:ncol], p[:, 0:ncol], AF.Exp, [tp], [tpt])
                                if j >= 4 * i:
                                    S.op("pool", lambda e, pt_=pt_: e.affine_select(out=pt_[:, 0:128], in_=pt_[:, 0:128], pattern=[[1, 128]],
                                                                                   compare_op=ALU.is_ge, fill=0.0, base=0, channel_multiplier=-1),
                                         r=[tpt], w=[tpt])
                                return (j, pt_, tpt, c0, ncol)

                            def pv_stage(st_, hl=hl, po=po, tpo=tpo, nj=nj):
                                j, pt_, tpt, c0, ncol = st_
                                mm(S, po[:, c0:c0 + ncol], [(vaug[:, j, hl, :], pt_[:, 0:ncol])],
                                   r=[t_v, tpt], w=[tpo], start=(j == 0), stop=(j == nj - 1))
                            inflight = []
                            for j in range(nj):
                                inflight.append(s_stage(j))
                                if len(inflight) > 2:
                                    pv_stage(inflight.pop(0))
                            while inflight:
                                pv_stage(inflight.pop(0))
                            rz_, trz = rz.next()
                            act(rz_[:], po[64:128, :], AF.Copy, [tpo], [trz])
                            recip(rz_[:], rz_[:], [trz], [trz])
                            tt("dve", yb[hl * 64:(hl + 1) * 64, i * 512:(i + 1) * 512], po[0:64, :], rz_[:], ALU.mult, [tpo, trz], [t_yb])
                    dma("sp", yT_d[512 + hp * 128:512 + (hp + 1) * 128, :], yb[:], r=[t_yb])
                S.flush()

        def mixer_c(l, win, xnT, xtok):
            with ExitStack() as k:
                wr = Ring([sb(k, [128, 16, 128], BF16) for _ in range(8)])
                psr = Ring([pst(k, [128, 512]) for _ in range(2)])
                ptr = Ring([pst(k, [128, 1024], BF16) for _ in range(1)])
                pmy = pst(k, [128, 512])
                psA = Ring([pst(k, [128, 512])[:, 0:128]])
                psU = Ring([pst(k, [128, 512])[:, 0:128] for _ in range(2)])
                psO = Ring([pmy[:, 0:128], pst(k, [128, 512])[:, 0:128]])
                t_pmy = psO.items[0][1]
                lbt = sb(k, [128, 16]); t_lb = Tok()
                gn = sb(k, [128, 1]); t_gn = Tok()
                onesf = sb(k, [128, T]); t_of = Tok()
                qf = sb(k, [128, T]); t_qf = Tok()
                bA = sb(k, [128, T]); t_A = Tok()
                bB = sb(k, [128, T]); t_B = Tok()
                bG = sb(k, [128, T]); t_G = Tok()
                dd = sb(k, [128, 32]); t_dd = Tok()
                qtil = sb(k, [128, T], BF16); t_qt = Tok()
                ktil = sb(k, [128, T], BF16); t_kt = Tok()
                ktok = [sb(k, [128, 16, 128], BF16) for _ in range(2)]; t_ktok = [Tok(), Tok()]
                vtok = sb(k, [128, 16, 128], BF16); t_v = Tok()
                gate = sb(k, [128, T], BF16); t_gate = Tok()
                oT = sb(k, [128, T]); t_oT = Tok()
                AT = Ring([sb(k, [128, 2, 64], BF16) for _ in range(2)])
                Tst = sb(k, [128, 128]); t_T = Tok()
                Sb = Ring([sb(k, [128, 128], BF16) for _ in range(2)])
                sq = (sb(k, [128, 512], BF16), Tok())
                rs_t = sb(k, [128, 512]); t_rs = Tok()
                t1 = sb(k, [128, 512]); t_t1 = Tok()
                ycT = Ring([sb(k, [128, T], BF16) for _ in range(2)])
                lbin = sb(k, [8, 128]); t_lbin = Tok()
                identf = sb(k, [128, 128]); t_idf = Tok()
                rm = sb(k, [128, 2]); t_rm = Tok()
                tri2 = sb(k, [128, 2, 64]); t_tri = Tok()
                dma("sp", identf[:], c_identf, w=[t_idf])
                dma("sp", rm[:], c_rm, w=[t_rm])
                dma("sp", tri2[:], c_tri2, w=[t_tri])
                dma("sp", lbin[:], hgrn_lb.rearrange("l (c p) -> (l c) p", p=128), w=[t_lbin])
                S.op("pe", lambda e: e.transpose(out=pmy[:, 256:264], in_=lbin[:], identity=identf[0:8, 0:8]), r=[t_lbin, t_idf], w=[t_pmy])
                cp("dve", lbt[:, 0:8], pmy[:, 256:264], [t_pmy], [t_lb])
                dma("sp", gn[:], hgrn_norm_g[l].rearrange("(p o) -> p o", o=1), w=[t_gn])
                memset("pool", onesf[:], 1.0, [t_of])
                if l == 0:
                    memset("dve", lbt[:, 8:12], 0.0, [t_lb])
                else:
                    tt("dve", lbt[:, 8:12], lbt[:, 4:8], lbt[:, 0:4], ALU.subtract, [t_lb], [t_lb])
                    act(lbt[:, 8:12], lbt[:, 8:12], AF.Sigmoid, [t_lb], [t_lb])
                ts("dve", lbt[:, 12:16], lbt[:, 8:12], -1.0, 1.0, ALU.mult, ALU.add, [t_lb], [t_lb])
                def ldc(h_):
                    return (load_w(wr, win, C0 + h_ * 128, 128), load_w(wr, win, C0 + 512 + h_ * 128, 128),
                            load_w(wr, win, C0 + 1024 + h_ * 128, 128), load_w(wr, win, C0 + 1536 + h_ * 128, 128))
                nxtw = ldc(0)
                for h in range(4):
                    yc, t_yc = ycT.next()
                    (wq, twq), (wzz, twz), (wi, twi), (wg, twg) = nxtw
                    if h + 1 < 4:
                        nxtw = ldc(h + 1)

                    def evq(tti, p, tp):
                        act(qf[:, tti * 512:(tti + 1) * 512], p[:], AF.Copy, [tp], [t_qf], scale=128 ** -0.5)

                    def evz(tti, p, tp, h=h):
                        act(bA[:, tti * 512:(tti + 1) * 512], p[:], AF.Sigmoid, [tp], [t_A])

                    def evv(tc, p, tp):
                        cp("dve", vtok[:, tc, :], p[:, 0:128], [tp], [t_v])

                    def evg(tti, p, tp):
                        act(gate[:, tti * 512:(tti + 1) * 512], p[:], AF.Sigmoid, [tp], [t_gate])
                    proj_fm(xnT, xtok, wq, twq, 0, 128, 16, psr, evq)
                    proj_fm(xnT, xtok, wzz, twz, 0, 128, 16, psr, evz)
                    proj_tm(xnT, xtok, wi, twi, 128, 16, psr, evv)
                    proj_fm(xnT, xtok, wg, twg, 0, 128, 16, psr, evg)
                    ts("dve", bA[:], bA[:], lbt[:, 12 + h:13 + h], lbt[:, 8 + h:9 + h], ALU.mult, ALU.add, [t_A, t_lb], [t_A])
                    act(bB[:], bA[:], AF.Ln, [t_A], [t_B])
                    ts("dve", bA[:], bA[:], -1.0, 1.0, ALU.mult, ALU.add, [t_A], [t_A])
                    S.op("dve", lambda e: e.tensor_tensor_scan(out=bG[:], data0=onesf[:], data1=bB[:], initial=0.0, op0=ALU.mult, op1=ALU.add),
                         r=[t_B, t_of], w=[t_G])
                    G3 = bG[:].rearrange("p (c t) -> p c t", t=64)
                    E3 = bB[:].rearrange("p (c t) -> p c t", t=64)
                    tt("dve", E3, G3, G3[:, :, 31:32].broadcast_to([128, 32, 64]), ALU.subtract, [t_G], [t_B])
                    tt("dve", dd[:, 0:31], G3[:, 1:32, 31], G3[:, 0:31, 31], ALU.subtract, [t_G], [t_dd])
                    act(dd[:, 0:31], dd[:, 0:31], AF.Exp, [t_dd], [t_dd])
                    act(bG[:], bB[:], AF.Exp, [t_B], [t_G])
                    act(bB[:], bB[:], AF.Exp, [t_B], [t_B], scale=-1.0)
                    tt("dve", qtil[:], qf[:], bG[:], ALU.mult, [t_qf, t_G], [t_qt])
                    tt("dve", ktil[:], bA[:], bB[:], ALU.mult, [t_A, t_B], [t_kt])
                    for half in range(2):
                        p, tp = ptr.next()

                        def fn(e, p=p, half=half):
                            ins = None
                            for j in range(8):
                                c = (half * 8 + j) * 128
                                ins = e.transpose(out=p[:, j * 128:(j + 1) * 128], in_=ktil[:, c:c + 128], identity=ident[:])
                            return ins
                        S.op("pe", fn, r=[t_kt, t_ident], w=[tp])
                        for hf in range(2):
                            act(ktok[hf][:, half * 8:(half + 1) * 8, :], p[:].rearrange("p (k t) -> p k t", k=8), AF.Copy, [tp, t_rm], [t_ktok[hf]],
                                scale=rm[:, hf:hf + 1])
                    sbc = None
                    for tc in range(16):
                        pa, tpa = psA.next()
                        at, tat = AT.next()
                        for hf in range(2):
                            c = 2 * tc + hf
                            mm(S, pa[:, hf * 64:(hf + 1) * 64], [(ktil[:, tc * 128:(tc + 1) * 128], qtil[:, c * 64:(c + 1) * 64])],
                               r=[t_kt, t_qt], w=[tpa])
                        tt("dve", at[:], pa[:].rearrange("p (h t) -> p h t", h=2), tri2[:], ALU.mult, [tpa, t_tri], [tat])
                        po, tpo = psO.next()
                        for hf in range(2):
                            c = 2 * tc + hf
                            pu, tpu = psU.next()
                            mm(S, pu[:], [(ktok[hf][:, tc, :], vtok[:, tc, :])], r=[t_ktok[hf], t_v], w=[tpu])
                            mm(S, po[:, hf * 64:(hf + 1) * 64], [(vtok[:, tc, :], at[:, hf, :])], r=[t_v, tat], w=[tpo],
                               start=True, stop=(c == 0))
                            if c > 0:
                                mm(S, po[:, hf * 64:(hf + 1) * 64], [(sbc[0][:], qtil[:, c * 64:(c + 1) * 64])], r=[sbc[1], t_qt], w=[tpo],
                                   start=False, stop=True)
                            if c == 0:
                                cp("dve", Tst[:], pu[:], [tpu], [t_T])
                            else:
                                stt(Tst[:], Tst[:], dd[:, c - 1:c], pu[:], ALU.mult, ALU.add, [t_T, t_dd, tpu], [t_T])
                            if c < 31:
                                sbc = Sb.next()
                                ts("dve", sbc[0][:], Tst[:], dd[:, c:c + 1], None, ALU.mult, None, [t_T, t_dd], [sbc[1]])
                        act(oT[:, tc * 128:(tc + 1) * 128], po[:], AF.Copy, [tpo], [t_oT])
                    for tti in range(4):
                        cs = slice(tti * 512, (tti + 1) * 512)
                        rms_part(sq, oT[:, cs], 512, psr, [t_oT], rs_t, t_rs)
                        stt(t1[:], oT[:, cs], gn[:, 0:1], rs_t[:], ALU.mult, ALU.mult, [t_oT, t_gn, t_rs], [t_t1])
                        tt("dve", yc[:, cs], t1[:], gate[:, cs], ALU.mult, [t_t1, t_gate], [t_yc])
                    dma("sp", yT_d[1024 + h * 128:1024 + (h + 1) * 128, :], yc[:], r=[t_yc])
                S.flush()

        def mixer_d(l, win, xnT, xtok):
            lam_init = 0.8 - 0.6 * float(np.exp(-0.3 * l))
            with ExitStack() as k:
                wr = Ring([sb(k, [128, 16, 128], BF16) for _ in range(6)])
                psr = Ring([pst(k, [128, 512]) for _ in range(4)])
                pacc = [pst(k, [128, 512]) for _ in range(4)]; t_acc = [Tok() for _ in range(4)]
                cosT = sb(k, [128, T]); t_cos = Tok()
                sinT = sb(k, [128, T]); t_sin = Tok()
                mk = sb(k, [128, 2]); t_mk = Tok()
                lamt = sb(k, [128, 256]); t_lam = Tok()
                lw_ = sb(k, [128, 128]); t_lw = Tok()
                ls = sb(k, [128, 8]); t_ls = Tok()
                gD = sb(k, [128, 1]); t_gD = Tok()
                qr = sb(k, [128, T], BF16); t_qr = Tok()
                k1p = sb(k, [128, T], BF16); t_k1 = Tok()
                k2p = sb(k, [128, T], BF16); t_k2 = Tok()
                vtok = sb(k, [128, 16, 128], BF16); t_v = Tok()
                tA = Ring([sb(k, [128, 512]) for _ in range(2)])
                tB = Ring([sb(k, [128, 512]) for _ in range(2)])
                PT = Ring([sb(k, [128, 512], BF16) for _ in range(6)])
                r1r = Ring([sb(k, [128, 512]) for _ in range(2)])
                r2r = Ring([sb(k, [128, 512]) for _ in range(2)])
                oD = sb(k, [128, 512]); t_oD = Tok()
                sq = (sb(k, [128, 512], BF16), Tok())
                rs_t = sb(k, [128, 512]); t_rs = Tok()
                ydT = Ring([sb(k, [128, T], BF16) for _ in range(2)])
                dma("sp", cosT[:], c_cos, w=[t_cos])
                dma("sp", sinT[:], c_sinA, w=[t_sin])
                dma("sp", mk[:], c_mk, w=[t_mk])
                dma("sp", lamt[:], diff_lambda[l].partition_broadcast(128), w=[t_lam])
                dma("sp", gD[:], diff_norm_g[l].rearrange("(p o) -> p o", o=1), w=[t_gD])
                ts("dve", gD[:], gD[:], 1.0 - lam_init, None, ALU.mult, None, [t_gD], [t_gD])
                tt("dve", lw_[:, 0:64], lamt[:, 0:64], lamt[:, 64:128], ALU.mult, [t_lam], [t_lw])
                tt("dve", lw_[:, 64:128], lamt[:, 128:192], lamt[:, 192:256], ALU.mult, [t_lam], [t_lw])
                S.op("dve", lambda e: e.reduce_sum(out=ls[:, 0:1], in_=lw_[:, 0:64], axis=AX.X), r=[t_lw], w=[t_ls])
                S.op("dve", lambda e: e.reduce_sum(out=ls[:, 1:2], in_=lw_[:, 64:128], axis=AX.X), r=[t_lw], w=[t_ls])
                act(ls[:, 2:4], ls[:, 0:2], AF.Exp, [t_ls], [t_ls])
                tt("dve", ls[:, 4:5], ls[:, 3:4], ls[:, 2:3], ALU.subtract, [t_ls], [t_ls])
                ts("dve", ls[:, 5:6], ls[:, 4:5], -lam_init, None, ALU.add, None, [t_ls], [t_ls])
                def ldd(h_):
                    return (load_w(wr, win, D0 + h_ * 128, 128), load_w(wr, win, D0 + 512 + h_ * 128, 128),
                            load_w(wr, win, D0 + 1024 + h_ * 128, 128))
                nxtw = ldd(0)
                for h in range(4):
                    yd, t_yd = ydT.next()
                    (wq, twq), (wk, twk), (wv, twv) = nxtw
                    if h + 1 < 4:
                        nxtw = ldd(h + 1)

                    def rope(tti, p, tp, dst):
                        cs = slice(tti * 512, (tti + 1) * 512)
                        a_, ta_ = tA.next()
                        b_, tb_ = tB.next()
                        tt("dve", a_[:], p[:], cosT[:, cs], ALU.mult, [tp, t_cos], [ta_])
                        tt("dve", b_[0:64, :], p[64:128, :], sinT[64:128, cs], ALU.mult, [tp, t_sin], [tb_])
                        tt("dve", b_[64:128, :], p[0:64, :], sinT[0:64, cs], ALU.mult, [tp, t_sin], [tb_])
                        return a_, ta_, b_, tb_, cs

                    def evq(tti, p, tp):
                        a_, ta_, b_, tb_, cs = rope(tti, p, tp, None)
                        tt("dve", qr[:, cs], a_[:], b_[:], ALU.add, [ta_, tb_], [t_qr])

                    def evk(tti, p, tp):
                        a_, ta_, b_, tb_, cs = rope(tti, p, tp, None)
                        tt("dve", a_[:], a_[:], b_[:], ALU.add, [ta_, tb_], [ta_])
                        act(k1p[:, cs], a_[:], AF.Copy, [ta_, t_mk], [t_k1], scale=mk[:, 0:1])
                        act(k2p[:, cs], a_[:], AF.Copy, [ta_, t_mk], [t_k2], scale=mk[:, 1:2])

                    def evv(tc, p, tp):
                        act(vtok[:, tc, :], p[:, 0:128], AF.Copy, [tp], [t_v])
                    proj_fm(xnT, xtok, wq, twq, 0, 128, 16, psr, evq)
                    proj_fm(xnT, xtok, wk, twk, 0, 128, 16, psr, evk)
                    proj_tm(xnT, xtok, wv, twv, 128, 16, psr, evv)
                    pend = None
                    for i in range(4):
                        nj = 4 * i + 4

                        def s_stage(j, i=i):
                            t0 = max(i * 512, j * 128)
                            ncol = (i + 1) * 512 - t0
                            c0 = t0 - i * 512
                            res_ = []
                            for m, (kp, tkp) in enumerate(((k1p, t_k1), (k2p, t_k2))):
                                p, tp = psr.next()
                                mm(S, p[:, 0:ncol], [(kp[:, j * 128:(j + 1) * 128], qr[:, t0:t0 + ncol])], r=[tkp, t_qr], w=[tp])
                                pt_, tpt = PT.next()
                                act(pt_[:, 0:ncol], p[:, 0:ncol], AF.Exp, [tp], [tpt], scale=0.125)
                                if j >= 4 * i:
                                    memset("pool", pt_[64:128, 0:64], 0.0, [tpt])
                                res_.append((pt_, tpt))
                            return (j, res_, c0, ncol)

                        def pv_stage(st_, nj=nj):
                            j, res_, c0, ncol = st_
                            for m, (pt_, tpt) in enumerate(res_):
                                mm(S, pacc[2 * m][:, c0:c0 + ncol], [(vtok[:, j, :], pt_[:, 0:ncol])], r=[t_v, tpt], w=[t_acc[2 * m]],
                                   start=(j == 0), stop=(j == nj - 1))
                                mm(S, pacc[2 * m + 1][:, c0:c0 + ncol], [(ones_bf[:], pt_[:, 0:ncol])], r=[t_ones, tpt], w=[t_acc[2 * m + 1]],
                                   start=(j == 0), stop=(j == nj - 1))
                        def fin2(i_, r1, t_r1, r2, t_r2, yd=yd, t_yd=t_yd):
                            cs = slice(i_ * 512, (i_ + 1) * 512)
                            stt(oD[:], r2[:], ls[:, 5:6], r1[:], ALU.mult, ALU.add, [t_r1, t_r2, t_ls], [t_oD])
                            rms_part(sq, oD[:], 512, psr, [t_oD], rs_t, t_rs)
                            stt(yd[:, cs], oD[:], gD[:, 0:1], rs_t[:], ALU.mult, ALU.mult, [t_oD, t_gD, t_rs], [t_yd])
                        prev = None
                        for j in range(nj):
                            cur = s_stage(j)
                            if prev is not None:
                                pv_stage(prev)
                            prev = cur
                            if j == 1 and pend is not None:
                                fin2(*pend)
                                pend = None
                        pv_stage(prev)
                        r1, t_r1 = r1r.next()
                        r2, t_r2 = r2r.next()
                        recip(r1[:], pacc[1][:], [t_acc[1]], [t_r1])
                        tt("dve", r1[:], pacc[0][:], r1[:], ALU.mult, [t_acc[0], t_r1], [t_r1])
                        recip(r2[:], pacc[3][:], [t_acc[3]], [t_r2])
                        tt("dve", r2[:], pacc[2][:], r2[:], ALU.mult, [t_acc[2], t_r2], [t_r2])
                        pend = (i, r1, t_r1, r2, t_r2)
                    fin2(*pend)
                    pend = None
                    dma("sp", yT_d[1536 + h * 128:1536 + (h + 1) * 128, :], yd[:], r=[t_yd])
                S.flush()

        def phase_gate(l, xnT, xtok):
            with ExitStack() as k:
                yall = sb(k, [128, 16, T], BF16); t_y = Tok()
                wgr = Ring([sb(k, [128, 16, 128], BF16) for _ in range(3)])
                wbr = Ring([sb(k, [128, 4, 128], BF16) for _ in range(3)])
                bg = sb(k, [128, 4, 16]); t_bg = Tok()
                psg = Ring([pst(k, [128, 512]) for _ in range(4)])
                psb = Ring([pst(k, [128, 512]) for _ in range(4)])
                gt = Ring([sb(k, [128, 512]) for _ in range(3)])
                tmp = Ring([sb(k, [128, 512]) for _ in range(3)])
                accs = [sb(k, [128, 512]) for _ in range(4)]; t_accs = [Tok() for _ in range(4)]
                mo = Ring([sb(k, [128, T], BF16) for _ in range(2)])
                for q4 in range(4):
                    dma("sp", yall[:, q4 * 4:(q4 + 1) * 4, :], yT_d[q4 * 512:(q4 + 1) * 512, :].rearrange("(kc p) t -> p kc t", p=128), w=[t_y])
                bgin = sb(k, [64, 128]); t_bgin = Tok()
                identf = sb(k, [128, 128]); t_idf = Tok()
                dma("sp", identf[:], c_identf, w=[t_idf])
                dma("sp", bgin[:], b_gate[l].rearrange("n (oc p) -> (n oc) p", p=128), w=[t_bgin])
                p0, tp0 = psg.next()
                S.op("pe", lambda e: e.transpose(out=p0[:, 0:64], in_=bgin[:], identity=identf[0:64, 0:64]), r=[t_bgin, t_idf], w=[tp0])
                cp("dve", bg[:], p0[:, 0:64].rearrange("p (n oc) -> p n oc", n=4), [tp0], [t_bg])
                def ldg(i_):
                    oc_, n_ = divmod(i_, 4)
                    return load_w(wgr, w_gate[l, n_], oc_ * 128, 128), load_w(wbr, w_branch[l, n_], oc_ * 128, 128, KC=4)
                nxtw = ldg(0)
                for oc in range(16):
                    m_, tm_ = mo.next()
                    for n in range(4):
                        (wg, twg), (wb_, twb) = nxtw
                        if oc * 4 + n + 1 < 64:
                            nxtw = ldg(oc * 4 + n + 1)
                        for tti in range(4):
                            cs = slice(tti * 512, (tti + 1) * 512)
                            pg, tpg = psg.next()
                            pb, tpb = psb.next()
                            mm(S, pg[:], [(wg[:, kc, :], xnT[:, kc, cs]) for kc in range(16)], r=[xtok, twg], w=[tpg])
                            mm(S, pb[:], [(wb_[:, kc, :], yall[:, n * 4 + kc, cs]) for kc in range(4)], r=[t_y, twb], w=[tpb])
                            g_, tg_ = gt.next()
                            act(g_[:], pg[:], AF.Sigmoid, [tpg, t_bg], [tg_], bias=bg[:, n, oc:oc + 1])
                            if n == 0:
                                tt("dve", accs[tti][:], g_[:], pb[:], ALU.mult, [tg_, tpb], [t_accs[tti]])
                            else:
                                t_, tt_ = tmp.next()
                                tt("dve", t_[:], g_[:], pb[:], ALU.mult, [tg_, tpb], [tt_])
                                if n < 3:
                                    tt("pool", accs[tti][:], accs[tti][:], t_[:], ALU.add, [t_accs[tti], tt_], [t_accs[tti]])
                                else:
                                    tt("pool", m_[:, cs], accs[tti][:], t_[:], ALU.add, [t_accs[tti], tt_], [tm_])
                    dma("sp", mixT_d[oc * 128:(oc + 1) * 128, :], m_[:], r=[tm_])
                S.flush()

        def phase_wout(l, src_rows):
            with ExitStack() as k:
                mT = sb(k, [128, 16, T], BF16); t_m = Tok()
                wo = sb(k, [128, 16, D], BF16); t_wo = [Tok() for _ in range(4)]
                xr = Ring([sb(k, [128, D]) for _ in range(2)])
                xo = Ring([sb(k, [128, D]) for _ in range(2)])
                psr = Ring([pst(k, [128, 512]) for _ in range(4)])
                for q4 in range(4):
                    dma("sp", mT[:, q4 * 4:(q4 + 1) * 4, :], mixT_d[q4 * 512:(q4 + 1) * 512, :].rearrange("(kc p) t -> p kc t", p=128), w=[t_m])
                for ct in range(4):
                    dma("pool", wo[:, :, ct * 512:(ct + 1) * 512], w_out[l][:, ct * 512:(ct + 1) * 512].rearrange("(kc p) n -> p kc n", p=128), w=[t_wo[ct]], ndesc=128)
                for tc in range(16):
                    xi, txi = xr.next()
                    xo_, txo = xo.next()
                    dma("sp", xi[:], src_rows[tc * 128:(tc + 1) * 128, :], w=[txi])
                    for ct in range(4):
                        cs = slice(ct * 512, (ct + 1) * 512)
                        p, tp = psr.next()
                        mm(S, p[:], [(mT[:, kc, tc * 128:(tc + 1) * 128], wo[:, kc, cs]) for kc in range(16)], r=[t_m, t_wo[ct]], w=[tp])
                        tt("dve", xo_[:, cs], p[:], xi[:, cs], ALU.add, [tp, txi], [txo])
                    dma("sp", xa_d[tc * 128:(tc + 1) * 128, :], xo_[:], r=[txo])
                S.flush()

        def phase_ffn_up(l, hnT, htok):
            with ExitStack() as k:
                wr = Ring([sb(k, [128, 16, 128], BF16) for _ in range(4)])
                cwin = sb(k, [88, 4, 128]); t_cwin = Tok()
                cw = sb(k, [128, 4, 88]); t_cw = Tok()
                identf = sb(k, [128, 128]); t_idf = Tok()
                ha = Ring([sb(k, [128, T + 2]) for _ in range(2)])
                hg = Ring([sb(k, [128, T + 2]) for _ in range(2)])
                ca = Ring([sb(k, [128, T]) for _ in range(2)])
                cg = Ring([sb(k, [128, T]) for _ in range(2)])
                ao = Ring([sb(k, [128, T], BF16) for _ in range(2)])
                psa = Ring([pst(k, [128, 512]) for _ in range(4)])
                psg = Ring([pst(k, [128, 512]) for _ in range(4)])
                dma("sp", identf[:], c_identf, w=[t_idf])
                for kk in range(3):
                    dma("sp", cwin[:, kk, :], ffn_conv_w[l, kk].rearrange("(c p) -> c p", p=128), w=[t_cwin])
                dma("sp", cwin[:, 3, :], ffn_conv_b[l].rearrange("(c p) -> c p", p=128), w=[t_cwin])
                p0, tp0 = psa.next()

                def fnT(e):
                    ins = None
                    for kk in range(4):
                        ins = e.transpose(out=p0[:, kk * 88:(kk + 1) * 88], in_=cwin[:, kk, :], identity=identf[0:88, 0:88])
                    return ins
                S.op("pe", fnT, r=[t_cwin, t_idf], w=[tp0])
                cp("dve", cw[:], p0[:, 0:352].rearrange("p (k c) -> p k c", k=4), [tp0], [t_cw])
                for r_ in (ha, hg):
                    for (tb, ttk) in r_.items:
                        memset("pool", tb[:, 0:2], 0.0, [ttk])
                wup = ffn_w_up[l]
                def ldw(j):
                    return load_w(wr, wup, j * 128, 128), load_w(wr, wup, (44 + j) * 128, 128)
                nxtw = ldw(0)
                for j in range(44):
                    (wa, twa), (wg, twg) = nxtw
                    if j + 1 < 44:
                        nxtw = ldw(j + 1)
                    ha_, tha = ha.next()
                    hg_, thg = hg.next()
                    for tti in range(4):
                        cs = slice(tti * 512, (tti + 1) * 512)
                        pa, tpa = psa.next()
                        pg, tpg = psg.next()
                        mm(S, pa[:], [(wa[:, kc, :], hnT[:, kc, cs]) for kc in range(16)], r=[htok, twa], w=[tpa])
                        mm(S, pg[:], [(wg[:, kc, :], hnT[:, kc, cs]) for kc in range(16)], r=[htok, twg], w=[tpg])
                        act(ha_[:, 2 + tti * 512:2 + (tti + 1) * 512], pa[:], AF.Copy, [tpa], [tha])
                        act(hg_[:, 2 + tti * 512:2 + (tti + 1) * 512], pg[:], AF.Copy, [tpg], [thg])
                    ca_, tca = ca.next()
                    cg_, tcg = cg.next()
                    ao_, tao = ao.next()
                    for (hb, thb, c_, tc_, jj) in ((ha_, tha, ca_, tca, j), (hg_, thg, cg_, tcg, 44 + j)):
                        act(c_[:], hb[:, 2:T + 2], AF.Identity, [thb, t_cw], [tc_], scale=cw[:, 2, jj:jj + 1], bias=cw[:, 3, jj:jj + 1])
                        stt(c_[:], hb[:, 1:T + 1], cw[:, 1, jj:jj + 1], c_[:], ALU.mult, ALU.add, [thb, t_cw, tc_], [tc_])
                        stt(c_[:], hb[:, 0:T], cw[:, 0, jj:jj + 1], c_[:], ALU.mult, ALU.add, [thb, t_cw, tc_], [tc_])
                    act(ca_[:], ca_[:], AF.Gelu_apprx_tanh, [tca], [tca])
                    tt("pool", ao_[:], ca_[:], cg_[:], ALU.mult, [tca, tcg], [tao])
                    dma("sp", actT_d[j * 128:(j + 1) * 128, :], ao_[:], r=[tao])
                S.flush()

        def phase_ffn_down(l, dst_rows):
            with ExitStack() as k:
                aT = sb(k, [128, 44, 1024], BF16); t_a = Tok()
                wd = Ring([sb(k, [128, 44, 256], BF16) for _ in range(2)])
                xr = Ring([sb(k, [128, 256]) for _ in range(4)])
                xo = Ring([sb(k, [128, 256]) for _ in range(4)])
                psr = Ring([pst(k, [128, 512]) for _ in range(4)])
                wdn = ffn_w_down[l]
                for half in range(2):
                    for q4 in range(4):
                        dma("sp", aT[:, q4 * 11:(q4 + 1) * 11, :],
                            actT_d[q4 * 1408:(q4 + 1) * 1408, half * 1024:(half + 1) * 1024].rearrange("(kc p) t -> p kc t", p=128), w=[t_a])
                    for ct in range(8):
                        w_, tw_ = load_w(wd, wdn, ct * 256, 256, KC=44)
                        for tc in range(8):
                            r0 = half * 1024 + tc * 128
                            xi, txi = xr.next()
                            xo_, txo = xo.next()
                            dma("sp", xi[:], xa_d[r0:r0 + 128, ct * 256:(ct + 1) * 256], w=[txi])
                            p, tp = psr.next()
                            mm(S, p[:, 0:256], [(aT[:, kc, tc * 128:(tc + 1) * 128], w_[:, kc, :]) for kc in range(44)], r=[t_a, tw_], w=[tp])
                            tt("dve", xo_[:], p[:, 0:256], xi[:], ALU.add, [tp, txi], [txo])
                            dma("sp", dst_rows[r0:r0 + 128, ct * 256:(ct + 1) * 256], xo_[:], r=[txo])
                S.flush()

        def phase_final(src, dst):
            with ExitStack() as k:
                Gt = sb(k, [128, D]); tG = Tok()
                xin = Ring([sb(k, [128, D]) for _ in range(3)])
                xs = Ring([sb(k, [128, D]) for _ in range(3)])
                junk = sb(k, [128, D], BF16); tj = Tok()
                st = Ring([sb(k, [128, 4]) for _ in range(3)])
                dma("sp", Gt[:], norm_final_g.partition_broadcast(128), w=[tG])

                def fa(tc):
                    xi, txi = xin.next()
                    xo, txo = xs.next()
                    sv, tsv = st.next()
                    dma("sp", xi[:], src[tc * 128:(tc + 1) * 128, :], w=[txi])
                    act(junk[:], xi[:], AF.Square, [txi], [tj, tsv], accum_out=sv[:, 0:1])
                    act(sv[:, 1:2], sv[:, 0:1], AF.Sqrt, [tsv, t_eps], [tsv], scale=1.0 / D, bias=epst[:])
                    recip(sv[:, 2:3], sv[:, 1:2], [tsv], [tsv])
                    stt(xo[:], xi[:], sv[:, 2:3], Gt[:], ALU.mult, ALU.mult, [txi, tsv, tG], [txo])
                    return xo, txo
                cur = fa(0)
                for tc in range(16):
                    nxt = fa(tc + 1) if tc + 1 < 16 else None
                    dma("sp", dst[tc * 128:(tc + 1) * 128, :], cur[0][:], r=[cur[1]])
                    cur = nxt
                S.flush()

        scr = [xb_d, xc_d]
        for s in range(NS):
            src = x_in[s]
            for l in range(NLAY):
                dst = scr[l % 2]
                layer(l, src, dst)
                src = dst
            if STOP >= 10: phase_final(src, out[s])
    return nc


def _constants():
    ident = np.eye(128, dtype=np.float32)
    half = 32
    inv_freq = (10000.0 ** (-np.arange(half, dtype=np.float32) / half)).astype(np.float32)
    pos = np.arange(T, dtype=np.float32)
    ang = (pos[None, :] * inv_freq[:, None]).astype(np.float32)
    cos = np.cos(ang).astype(np.float32)
    sin = np.sin(ang).astype(np.float32)
    c_cos = np.tile(cos, (4, 1))
    c_sinA = np.concatenate([sin, sin, -sin, -sin], axis=0)
    tri = (np.arange(64)[None, :] >= (np.arange(128)[:, None] % 64)).astype(np.float32)
    g = (np.arange(128) // 32) % 2
    mk = np.stack([(g == 0), (g == 1)], axis=1).astype(np.float32)
    return {
        "c_ident": ident.astype(ml_dtypes.bfloat16),
        "c_identf": ident,
        "c_cos": np.ascontiguousarray(c_cos),
        "c_sinA": np.ascontiguousarray(c_sinA),
        "c_tri": tri,
        "c_mk": mk,
        "c_rm": np.stack([np.arange(128) < 64, np.arange(128) >= 64], axis=1).astype(np.float32),
        "c_tri2": np.ascontiguousarray(np.stack([tri * (np.arange(128)[:, None] < 64), tri * (np.arange(128)[:, None] >= 64)], axis=1).astype(np.float32)),
    }


def _perm_cols():
    r = np.arange(128)
    comp = (r // 32) % 2
    part = r // 64
    return comp * 64 + part * 32 + (r % 32)


def _prep_shared(inp):
    f = lambda a: np.ascontiguousarray(np.asarray(a, dtype=np.float32))
    w_in = f(inp["w_in"]).copy()
    pc = _perm_cols()
    for base in (D0, D0 + 512):
        for h in range(4):
            c = base + h * 128
            w_in[:, :, c:c + 128] = w_in[:, :, c + pc]
    m = {
        "norm_mix_g": f(inp["norm_mix_g"]), "w_in": w_in, "fox_b_f": f(inp["fox_b_f"]),
        "gmlp_ln_g": f(inp["gmlp_ln_g"]), "gmlp_ln_b": f(inp["gmlp_ln_b"]),
        "gmlp_w_sT": f(np.transpose(np.asarray(inp["gmlp_w_s"]), (0, 1, 3, 2))),
        "gmlp_b_s": f(np.asarray(inp["gmlp_b_s"]).reshape(DEPTH, 512)),
        "hgrn_lb_logits": f(inp["hgrn_lb_logits"]), "hgrn_norm_g": f(inp["hgrn_norm_g"]),
        "diff_lambda": f(np.asarray(inp["diff_lambda"]).reshape(DEPTH, 256)), "diff_norm_g": f(inp["diff_norm_g"]),
        "w_branch": f(inp["w_branch"]), "w_gate": f(inp["w_gate"]), "b_gate": f(inp["b_gate"]),
        "w_out": f(inp["w_out"]), "norm_ffn_g": f(inp["norm_ffn_g"]), "ffn_w_up": f(inp["ffn_w_up"]),
        "ffn_conv_w": f(inp["ffn_conv_w"]), "ffn_conv_b": f(inp["ffn_conv_b"]), "ffn_w_down": f(inp["ffn_w_down"]),
        "norm_final_g": f(inp["norm_final_g"]),
    }
    m.update(_constants())
    return m


def kernel(**inputs):
    x = np.ascontiguousarray(np.asarray(inputs["x"], dtype=np.float32))
    shared = _prep_shared(inputs)
    NS = x.shape[0] // NCORES
    nc = build_nc(NS=NS, NLAY=DEPTH, dbg=False)
    in_maps = []
    for c in range(NCORES):
        m = dict(shared)
        m["x"] = np.ascontiguousarray(x[c * NS:(c + 1) * NS])
        in_maps.append(m)
    res = run_bass_kernel_spmd(nc, in_maps, core_ids=list(range(NCORES)))
    return np.concatenate([np.asarray(r["out"], dtype=np.float32) for r in res.results], axis=0)
```

```python
import numpy as np
import ml_dtypes
import concourse.bass as bass
import concourse.mybir as mybir
from concourse.bass_utils import run_bass_kernel_spmd

F32 = mybir.dt.float32
BF16 = mybir.dt.bfloat16
ALU = mybir.AluOpType
AF = mybir.ActivationFunctionType
AX = mybir.AxisListType

D = 2048
T = 2048
DEPTH = 2
BW = 512
DFF = 5632
EPS = 1e-6
A0 = 0
B0 = 1024
C0 = 1024 + 1544
D0 = C0 + 2048
IN_COLS = 6152
NCORES = 8


class Tok:
    __slots__ = ("lw", "rd", "name")

    def __init__(self, name=""):
        self.lw = None
        self.rd = []
        self.name = name


class Op:
    __slots__ = ("eng", "fn", "deps", "sig", "val", "dma", "sem", "lane_wait")


CE = ("pe", "act", "dve", "pool")
FULLSYNC = True
NLANE = 12


class Sched:
    def __init__(self, nc):
        self.nc = nc
        self.sem = {e: nc.alloc_semaphore("c_" + e) for e in CE}
        self.cnt = {e: 0 for e in CE}
        self.lanes = {q: [[nc.alloc_semaphore("l_%s%d" % (q, i)), 0] for i in range(NLANE)] for q in ("sp", "pool")}
        self.rr = {"sp": 0, "pool": 0}
        self.ops = {e: [] for e in ("pe", "act", "dve", "pool", "sp")}
        self.toks = set()
        self.waited = {}
        self.nphase = 0
        self.pool_out = []

    def _deps(self, o, r, w):
        deps = []
        seen = set()

        def add(d, raw):
            if d is None or d is o or id(d) in seen:
                return
            if (not d.dma) and (not o.dma) and d.eng == o.eng and (d.eng == "pe" or (not raw and not FULLSYNC)):
                return
            seen.add(id(d))
            deps.append(d)
            if not d.dma:
                d.sig = True

        for t in r:
            add(t.lw, True)
        for t in w:
            add(t.lw, False)
            for d in t.rd:
                add(d, False)
        o.deps = deps
        for t in r:
            t.rd.append(o)
            self.toks.add(t)
        for t in w:
            t.lw = o
            t.rd = []
            self.toks.add(t)

    def op(self, eng, fn, r=(), w=()):
        o = Op()
        o.eng = eng
        o.fn = fn
        o.dma = False
        o.sig = False
        o.val = None
        o.sem = None
        o.lane_wait = 0
        self._deps(o, r, w)
        self.ops[eng].append(o)
        return o

    def dma(self, q, fn, r=(), w=(), ndesc=0):
        o = Op()
        o.eng = q
        o.fn = fn
        o.dma = True
        o.sig = True
        extra = []
        if q == "pool" and ndesc:
            while self.pool_out and sum(n for _, n in self.pool_out) + ndesc > 640:
                extra.append(self.pool_out.pop(0)[0])
            self.pool_out.append((o, ndesc))
        lane = self.lanes[q][self.rr[q]]
        self.rr[q] = (self.rr[q] + 1) % NLANE
        o.lane_wait = 16 * lane[1]
        lane[1] += 1
        o.sem = lane[0]
        o.val = 16 * lane[1]
        self._deps(o, r, w)
        for d in extra:
            if d not in o.deps:
                o.deps.append(d)
        self.ops[q].append(o)
        return o

    def flush(self):
        self.pool_out = []
        for e in CE:
            c = self.cnt[e]
            for o in self.ops[e]:
                if (not o.dma) and o.sig:
                    c += 1
                    o.val = c
            self.cnt[e] = c
        ops = self.ops
        self.ops = {e: [] for e in ("pe", "act", "dve", "pool", "sp")}
        waited = self.waited
        sems = self.sem
        lanes = self.lanes

        def emit(e, name):
            def w8(sem, val):
                key = (name, sem.num)
                if waited.get(key, 0) >= val:
                    return
                e.wait_ge(sem, val)
                waited[key] = val

            for o in ops[name]:
                for d in o.deps:
                    if d.dma:
                        w8(d.sem, d.val)
                    else:
                        w8(sems[d.eng], d.val)
                if o.dma and o.lane_wait:
                    w8(o.sem, o.lane_wait)
                ins = o.fn(e)
                if o.dma:
                    ins.then_inc(o.sem, 16)
                elif o.sig:
                    ins.then_inc(sems[o.eng], 1)
            if name in lanes:
                for sem, cnt in lanes[name]:
                    if cnt:
                        w8(sem, 16 * cnt)

        with self.nc.Block() as blk:
            @blk.sync
            def _(e):
                emit(e, "sp")

            @blk.gpsimd
            def _(e):
                emit(e, "pool")

            @blk.tensor
            def _(e):
                emit(e, "pe")

            @blk.scalar
            def _(e):
                emit(e, "act")

            @blk.vector
            def _(e):
                emit(e, "dve")
        for t in self.toks:
            t.lw = None
            t.rd = []
        self.toks = set()
        self.nphase += 1


class Ring:
    def __init__(self, items):
        self.items = [(t, Tok()) for t in items]
        self.i = 0

    def next(self):
        it = self.items[self.i]
        self.i = (self.i + 1) % len(self.items)
        return it


def mm(S, out_ap, pairs, r, w, start=True, stop=True):
    def fn(e, pairs=pairs, out_ap=out_ap, start=start, stop=stop):
        n = len(pairs)
        ins = None
        for i, (l, rh) in enumerate(pairs):
            ins = e.matmul(out_ap, l, rh, start=(start and i == 0), stop=(stop and i == n - 1))
        return ins
    return S.op("pe", fn, r=r, w=w)


from contextlib import ExitStack

_uid = [0]


def build_nc(NS=2, NLAY=2, dbg=False, STOP=99):
    nc = bass.Bass("TRN2", target_bir_lowering=False)

    def din(name, shape, dt=F32):
        return nc.dram_tensor(name, list(shape), dt, kind="ExternalInput").ap()

    x_in = din("x", [NS, T, D])
    norm_mix_g = din("norm_mix_g", [DEPTH, D])
    w_in = din("w_in", [DEPTH, D, IN_COLS])
    fox_b_f = din("fox_b_f", [DEPTH, 8])
    gmlp_ln_g = din("gmlp_ln_g", [DEPTH, BW])
    gmlp_ln_b = din("gmlp_ln_b", [DEPTH, BW])
    gmlp_w_sT = din("gmlp_w_sT", [DEPTH, 4, 128, 128])
    gmlp_b_s = din("gmlp_b_s", [DEPTH, 512])
    hgrn_lb = din("hgrn_lb_logits", [DEPTH, 512])
    hgrn_norm_g = din("hgrn_norm_g", [DEPTH, 128])
    diff_lambda = din("diff_lambda", [DEPTH, 256])
    diff_norm_g = din("diff_norm_g", [DEPTH, 128])
    w_branch = din("w_branch", [DEPTH, 4, BW, D])
    w_gate = din("w_gate", [DEPTH, 4, D, D])
    b_gate = din("b_gate", [DEPTH, 4, D])
    w_out = din("w_out", [DEPTH, D, D])
    norm_ffn_g = din("norm_ffn_g", [DEPTH, D])
    ffn_w_up = din("ffn_w_up", [DEPTH, D, 2 * DFF])
    ffn_conv_w = din("ffn_conv_w", [DEPTH, 3, 2 * DFF])
    ffn_conv_b = din("ffn_conv_b", [DEPTH, 2 * DFF])
    ffn_w_down = din("ffn_w_down", [DEPTH, DFF, D])
    norm_final_g = din("norm_final_g", [D])
    c_ident = din("c_ident", [128, 128], BF16)
    c_cos = din("c_cos", [128, T])
    c_sinA = din("c_sinA", [128, T])
    c_tri = din("c_tri", [128, 64])
    c_mk = din("c_mk", [128, 2])
    c_identf = din("c_identf", [128, 128])
    c_rm = din("c_rm", [128, 2])
    c_tri2 = din("c_tri2", [128, 2, 64])

    out = nc.dram_tensor("out", [NS, T, D], F32, kind="ExternalOutput").ap()
    okind = "ExternalOutput" if dbg else "Internal"

    def dscr(name, shape, dt):
        if dbg:
            return nc.dram_tensor(name, list(shape), dt, kind="ExternalOutput").ap()
        return nc.dram_tensor(name, list(shape), dt).ap()

    yT_d = dscr("yT_d", [D, T], BF16)
    mixT_d = dscr("mixT_d", [D, T], BF16)
    xa_d = dscr("xa_d", [T, D], F32)
    xb_d = dscr("xb_d", [T, D], F32)
    xc_d = dscr("xc_d", [T, D], F32)
    actT_d = dscr("actT_d", [DFF, T], BF16)
    ex_d = dscr("ex_d", [2, 8, 6, T], BF16)

    S = Sched(nc)

    def sb(k, shape, dt=F32):
        _uid[0] += 1
        return k.enter_context(nc.sbuf_tensor("s%d" % _uid[0], list(shape), dt))

    def pst(k, shape, dt=F32):
        _uid[0] += 1
        return k.enter_context(nc.psum_tensor("p%d" % _uid[0], list(shape), dt))

    def dma(q, out_ap, in_ap, r=(), w=(), slow=False, ndesc=0):
        if slow:
            return S.dma(q, lambda e, o=out_ap, i=in_ap: e.dma_start(out=o, in_=i, allow_slow_non_contiguous=True), r=r, w=w, ndesc=ndesc)
        return S.dma(q, lambda e, o=out_ap, i=in_ap: e.dma_start(out=o, in_=i), r=r, w=w, ndesc=ndesc)

    def act(out_ap, in_ap, func, r, w, **kw):
        return S.op("act", lambda e, o=out_ap, i=in_ap, f=func, kw=kw: e.activation(out=o, in_=i, func=f, **kw), r=r, w=w)

    def tt(eng, out_ap, a, b, op, r, w):
        return S.op(eng, lambda e, o=out_ap, a=a, b=b, op=op: e.tensor_tensor(out=o, in0=a, in1=b, op=op), r=r, w=w)

    def ts(eng, out_ap, a, s1, s2, op0, op1, r, w):
        if op1 is None:
            return S.op(eng, lambda e, o=out_ap, a=a, s1=s1, op0=op0: e.tensor_scalar(out=o, in0=a, scalar1=s1, scalar2=None, op0=op0), r=r, w=w)
        return S.op(eng, lambda e, o=out_ap, a=a, s1=s1, s2=s2, op0=op0, op1=op1: e.tensor_scalar(out=o, in0=a, scalar1=s1, scalar2=s2, op0=op0, op1=op1), r=r, w=w)

    def stt(out_ap, a, sc, b, op0, op1, r, w):
        return S.op("dve", lambda e, o=out_ap, a=a, sc=sc, b=b, op0=op0, op1=op1: e.scalar_tensor_tensor(out=o, in0=a, scalar=sc, in1=b, op0=op0, op1=op1), r=r, w=w)

    def recip(out_ap, in_ap, r, w):
        return S.op("dve", lambda e, o=out_ap, i=in_ap: e.reciprocal(out=o, in_=i), r=r, w=w)

    def cp(eng, out_ap, in_ap, r, w):
        return S.op(eng, lambda e, o=out_ap, i=in_ap: e.tensor_copy(out=o, in_=i), r=r, w=w)

    def memset(eng, ap, v, w):
        return S.op(eng, lambda e, ap=ap, v=v: e.memset(ap, v), w=w)

    G = ExitStack()
    with G:
        ident = sb(G, [128, 128], BF16); t_ident = Tok()
        ones_bf = sb(G, [128, 128], BF16); t_ones = Tok()
        epst = sb(G, [128, 1]); t_eps = Tok()
        dma("sp", ident[:], c_ident, w=[t_ident])
        memset("dve", ones_bf[:], 1.0, [t_ones])
        memset("dve", epst[:], EPS, [t_eps])
        CONST_R = [t_ident, t_ones, t_eps]

        def load_w(ring, wap, c0, ncol, KC=16):
            wt, tw = ring.next()
            dma("pool", wt[:, 0:KC, 0:ncol], wap[:, c0:c0 + ncol].rearrange("(kc p) n -> p kc n", p=128), w=[tw], ndesc=KC * 8)
            return wt, tw

        def proj_fm(xT, xtok, wt, tw, col_off, M, KC, psr, evac):
            for tti in range(4):
                p, tp = psr.next()
                pairs = [(wt[:, kc, col_off:col_off + M], xT[:, kc, tti * 512:(tti + 1) * 512]) for kc in range(KC)]
                mm(S, p[0:M, :], pairs, r=[xtok, tw], w=[tp])
                evac(tti, p, tp)

        def proj_tm(xT, xtok, wt, tw, ncol, KC, psr, evac):
            for tc in range(16):
                p, tp = psr.next()
                pairs = [(xT[:, kc, tc * 128:(tc + 1) * 128], wt[:, kc, 0:ncol]) for kc in range(KC)]
                mm(S, p[:, 0:ncol], pairs, r=[xtok, tw], w=[tp])
                evac(tc, p, tp)

        def phase_norm(src, gvec, xnT, xtok):
            with ExitStack() as k:
                Gt = sb(k, [128, D]); tG = Tok()
                xin = Ring([sb(k, [128, D]) for _ in range(3)])
                xs = Ring([sb(k, [128, D], BF16) for _ in range(3)])
                junk = sb(k, [128, D], BF16); tj = Tok()
                st = Ring([sb(k, [128, 4]) for _ in range(3)])
                ptr = Ring([pst(k, [128, 1024], BF16) for _ in range(4)])
                dma("sp", Gt[:], gvec.partition_broadcast(128), w=[tG])
                def stage_a(tc):
                    xi, txi = xin.next()
                    xo, txo = xs.next()
                    sv, tsv = st.next()
                    dma("sp", xi[:], src[tc * 128:(tc + 1) * 128, :], w=[txi])
                    act(junk[:], xi[:], AF.Square, [txi], [tj, tsv], accum_out=sv[:, 0:1])
                    act(sv[:, 1:2], sv[:, 0:1], AF.Sqrt, [tsv, t_eps], [tsv], scale=1.0 / D, bias=epst[:])
                    recip(sv[:, 2:3], sv[:, 1:2], [tsv], [tsv])
                    stt(xo[:], xi[:], sv[:, 2:3], Gt[:], ALU.mult, ALU.mult, [txi, tsv, tG], [txo])
                    return xo, txo

                def stage_b(tc, xo, txo):
                    for half in range(2):
                        p, tp = ptr.next()

                        def fn(e, p=p, xo=xo, half=half):
                            ins = None
                            for j in range(8):
                                c = (half * 8 + j) * 128
                                ins = e.transpose(out=p[:, j * 128:(j + 1) * 128], in_=xo[:, c:c + 128], identity=ident[:])
                            return ins
                        S.op("pe", fn, r=[txo, t_ident], w=[tp])
                        act(xnT[:, half * 8:(half + 1) * 8, tc * 128:(tc + 1) * 128],
                            p[:].rearrange("p (k t) -> p k t", k=8), AF.Copy, [tp], [xtok])
                cur = stage_a(0)
                for tc in range(16):
                    nxt = stage_a(tc + 1) if tc + 1 < 16 else None
                    stage_b(tc, *cur)
                    cur = nxt
                S.flush()

        def rms_part(k_sq, o_ap, ncol, psr, r_o, rs_t, t_rs):
            sq, tsq = k_sq
            act(sq[:, 0:ncol], o_ap, AF.Square, r_o, [tsq])
            p, tp = psr.next()
            mm(S, p[:, 0:ncol], [(ones_bf[:], sq[:, 0:ncol])], r=[tsq, t_ones], w=[tp])
            act(rs_t[:, 0:ncol], p[:, 0:ncol], AF.Sqrt, [tp, t_eps], [t_rs], scale=1.0 / 128, bias=epst[:])
            recip(rs_t[:, 0:ncol], rs_t[:, 0:ncol], [t_rs], [t_rs])

        def layer(l, src_rows, dst_rows):
            win = w_in[l]
            LK = ExitStack()
            with LK:
                xnT = sb(LK, [128, 16, T], BF16); xtok = Tok()
                phase_norm(src_rows, norm_mix_g[l], xnT, xtok)
                if STOP >= 2: mixer_a(l, win, xnT, xtok)
                if STOP >= 3: mixer_b(l, win, xnT, xtok)
                if STOP >= 4: mixer_c(l, win, xnT, xtok)
                if STOP >= 5: mixer_d(l, win, xnT, xtok)
                if STOP >= 6: phase_gate(l, xnT, xtok)
            if STOP >= 7: phase_wout(l, src_rows)
            if STOP >= 8:
                with ExitStack() as k2:
                    hnT = sb(k2, [128, 16, T], BF16); htok = Tok()
                    phase_norm(xa_d, norm_ffn_g[l], hnT, htok)
                    phase_ffn_up(l, hnT, htok)
            if STOP >= 9: phase_ffn_down(l, dst_rows)

        def mixer_a(l, win, xnT, xtok):
            with ExitStack() as k:
                wr = Ring([sb(k, [128, 16, 512], BF16) for _ in range(2)])
                uaT = sb(k, [128, 4, T]); t_ua = Tok()
                yaT = sb(k, [128, 4, T], BF16); t_ya = Tok()
                WsT = sb(k, [128, 4, 128], BF16); t_ws = Tok()
                Wsf = sb(k, [128, 4, 128]); t_wsf = Tok()
                BS = sb(k, [128, 512]); t_bs = Tok()
                Gl = sb(k, [128, 512]); t_gl = Tok()
                Bl = sb(k, [128, 512]); t_bl = Tok()
                vg = Ring([sb(k, [128, 512]) for _ in range(2)])
                vc = Ring([sb(k, [128, 512]) for _ in range(2)])
                vj = sb(k, [128, 512], BF16); t_vj = Tok()
                vn = Ring([sb(k, [128, 512], BF16) for _ in range(4)])
                sv_r = Ring([sb(k, [128, 8]) for _ in range(2)])
                mx = Ring([sb(k, [128, 512]) for _ in range(2)])
                psr = Ring([pst(k, [128, 512]) for _ in range(6)])
                dma("sp", Wsf[:], gmlp_w_sT[l].rearrange("g s t -> s g t"), w=[t_wsf])
                memset("pool", Wsf[64:128, :, 0:64], 0.0, [t_wsf])
                cp("pool", WsT[:], Wsf[:], [t_wsf], [t_ws])
                dma("sp", BS[:], gmlp_b_s[l].partition_broadcast(128), w=[t_bs])
                dma("sp", Gl[:], gmlp_ln_g[l].partition_broadcast(128), w=[t_gl])
                dma("sp", Bl[:], gmlp_ln_b[l].partition_broadcast(128), w=[t_bl])
                wt, tw = load_w(wr, win, A0, 512)
                for oc in range(4):
                    def ev(tti, p, tp, oc=oc):
                        act(uaT[:, oc, tti * 512:(tti + 1) * 512], p[:], AF.Gelu_apprx_tanh, [tp], [t_ua])
                    proj_fm(xnT, xtok, wt, tw, oc * 128, 128, 16, psr, ev)
                wt2, tw2 = load_w(wr, win, A0 + 512, 512)

                def ev2(tc, p, tp):
                    g_, tg_ = vg.next()
                    c_, tc_ = vc.next()
                    n_, tn_ = vn.next()
                    sv, tsv = sv_r.next()
                    act(g_[:], p[:], AF.Gelu_apprx_tanh, [tp], [tg_, tsv], accum_out=sv[:, 0:1])
                    ts("dve", sv[:, 1:2], sv[:, 0:1], 1.0 / 512, None, ALU.mult, None, [tsv], [tsv])
                    ts("dve", c_[:], g_[:], sv[:, 1:2], None, ALU.subtract, None, [tg_, tsv], [tc_])
                    act(vj[:], c_[:], AF.Square, [tc_], [t_vj, tsv], accum_out=sv[:, 2:3])
                    act(sv[:, 3:4], sv[:, 2:3], AF.Sqrt, [tsv, t_eps], [tsv], scale=1.0 / 512, bias=epst[:])
                    recip(sv[:, 4:5], sv[:, 3:4], [tsv], [tsv])
                    stt(c_[:], c_[:], sv[:, 4:5], Gl[:], ALU.mult, ALU.mult, [tc_, tsv, t_gl], [tc_])
                    tt("dve", n_[:], c_[:], Bl[:], ALU.add, [tc_, t_bl], [tn_])

                    def stage_b(tc=tc, n_=n_, tn_=tn_):
                        m_, tm_ = mx.next()
                        p2, tp2 = psr.next()

                        def fn(e, p2=p2, n_=n_):
                            ins = None
                            for g in range(4):
                                ins = e.matmul(p2[:, g * 128:(g + 1) * 128], n_[:, g * 128:(g + 1) * 128], WsT[:, g, :], start=True, stop=True)
                            return ins
                        S.op("pe", fn, r=[tn_, t_ws], w=[tp2])
                        tt("dve", m_[:], p2[:], BS[:], ALU.add, [tp2, t_bs], [tm_])
                        tt("dve", yaT[:, :, tc * 128:(tc + 1) * 128], m_[:].rearrange("p (g t) -> p g t", g=4),
                           uaT[:, :, tc * 128:(tc + 1) * 128], ALU.mult, [tm_, t_ua], [t_ya])
                    pendA.append(stage_b)
                    if len(pendA) > 2:
                        pendA.pop(0)()
                pendA = []
                proj_tm(xnT, xtok, wt2, tw2, 512, 16, psr, ev2)
                while pendA:
                    pendA.pop(0)()
                dma("sp", yT_d[0:512, :].rearrange("(g p) t -> p g t", p=128), yaT[:], r=[t_ya])
                S.flush()

        def mixer_b(l, win, xnT, xtok):
            with ExitStack() as k:
                wr = Ring([sb(k, [128, 16, 128], BF16) for _ in range(6)])
                wz = sb(k, [128, 16, 8], BF16); t_wz = Tok()
                psr = Ring([pst(k, [128, 512]) for _ in range(4)])
                k1 = k
                t_exd = [[Tok() for _ in range(6)] for _ in range(2)]
                if True:
                    cn = sb(k1, [8, T]); t_cn = Tok()
                    ex = sb(k1, [8, T]); t_ex = Tok()
                    rr_ = sb(k1, [8, T]); t_rr = Tok()
                    onesf = sb(k1, [8, T]); t_of = Tok()
                    nbf = sb(k1, [8, 2]); t_nbf = Tok()
                    cbs = [sb(k1, [8, T], BF16) for _ in range(3)]; t_cb = [Tok() for _ in range(3)]
                    nbs = [sb(k1, [8, T], BF16) for _ in range(3)]; t_nb = [Tok() for _ in range(3)]
                    onesb = sb(k1, [8, T], BF16); t_ob = Tok()
                    dma("pool", wz[:], win[:, B0 + 1536:B0 + 1544].rearrange("(kc p) n -> p kc n", p=128), w=[t_wz], ndesc=128)
                    dma("sp", nbf[:, 0:1], fox_b_f[l].rearrange("(h o) -> h o", o=1), w=[t_nbf])
                    ts("dve", nbf[:, 1:2], nbf[:, 0:1], -1.0, None, ALU.mult, None, [t_nbf], [t_nbf])
                    memset("pool", onesf[:], 1.0, [t_of])
                    memset("pool", onesb[:], 1.0, [t_ob])
                    for tti in range(4):
                        p, tp = psr.next()
                        pairs = [(wz[:, kc, :], xnT[:, kc, tti * 512:(tti + 1) * 512]) for kc in range(16)]
                        mm(S, p[0:8, :], pairs, r=[xtok, t_wz], w=[tp])
                        act(ex[:, tti * 512:(tti + 1) * 512], p[0:8, :], AF.Exp, [tp, t_nbf], [t_ex], scale=-1.0, bias=nbf[:, 1:2])
                    act(ex[:], ex[:], AF.Ln, [t_ex], [t_ex], bias=1.0)
                    S.op("dve", lambda e: e.tensor_tensor_scan(out=cn[:], data0=onesf[:], data1=ex[:], initial=0.0, op0=ALU.mult, op1=ALU.add),
                         r=[t_ex, t_of], w=[t_cn])
                    cp("dve", cbs[0][:], cn[:], [t_cn], [t_cb[0]])
                    tt("dve", rr_[:], cn[:], cbs[0][:], ALU.subtract, [t_cn, t_cb[0]], [t_rr])
                    cp("dve", cbs[1][:], rr_[:], [t_rr], [t_cb[1]])
                    tt("dve", rr_[:], rr_[:], cbs[1][:], ALU.subtract, [t_rr, t_cb[1]], [t_rr])
                    cp("dve", cbs[2][:], rr_[:], [t_rr], [t_cb[2]])
                    for j in range(3):
                        ts("dve", nbs[j][:], cbs[j][:], -1.0, None, ALU.mult, None, [t_cb[j]], [t_nb[j]])
                        dma("sp", ex_d[1, :, 3 + j, :], cbs[j][:], r=[t_cb[j]], w=[t_exd[1][3 + j]])
                        dma("sp", ex_d[0, :, j, :], nbs[j][:], r=[t_nb[j]], w=[t_exd[0][j]])
                        dma("sp", ex_d[1, :, j, :], onesb[:], r=[t_ob], w=[t_exd[1][j]])
                        dma("sp", ex_d[0, :, 3 + j, :], onesb[:], r=[t_ob], w=[t_exd[0][3 + j]])
                qh = [sb(k, [128, T], BF16) for _ in range(2)]; t_qh = [Tok(), Tok()]; t_qx = [Tok(), Tok()]; t_kx = [Tok(), Tok()]
                kh = [sb(k, [128, T], BF16) for _ in range(2)]; t_kh = [Tok(), Tok()]
                vaug = sb(k, [128, 16, 2, 128], BF16); t_v = Tok()
                ybT = Ring([sb(k, [128, T], BF16) for _ in range(2)])
                PT = Ring([sb(k, [128, 512], BF16) for _ in range(4)])
                rz = Ring([sb(k, [64, 512]) for _ in range(2)])
                pso = Ring([pst(k, [128, 512]) for _ in range(4)])
                for hl_ in range(2):
                    memset("pool", vaug[:, :, hl_, 64:128], 1.0, [t_v])
                def ldb(hp_):
                    return (load_w(wr, win, B0 + hp_ * 128, 128), load_w(wr, win, B0 + 512 + hp_ * 128, 128),
                            load_w(wr, win, B0 + 1024 + hp_ * 128, 128))
                nxtw = ldb(0)
                for hp in range(4):
                    yb, t_yb = ybT.next()
                    for hl in range(2):
                        h = hp * 2 + hl
                        dma("sp", qh[hl][64:70, :], ex_d[0, h], r=t_exd[0], w=[t_qx[hl]])
                        dma("sp", kh[hl][64:70, :], ex_d[1, h], r=t_exd[1], w=[t_kx[hl]])
                    (wq, twq), (wk, twk), (wv, twv) = nxtw
                    if hp + 1 < 4:
                        nxtw = ldb(hp + 1)

                    def evq(tti, p, tp):
                        for hl in range(2):
                            act(qh[hl][0:64, tti * 512:(tti + 1) * 512], p[hl * 64:(hl + 1) * 64, :], AF.Copy, [tp], [t_qh[hl]], scale=0.125)

                    def evk(tti, p, tp):
                        for hl in range(2):
                            cp("dve", kh[hl][0:64, tti * 512:(tti + 1) * 512], p[hl * 64:(hl + 1) * 64, :], [tp], [t_kh[hl]])

                    def evv(tc, p, tp):
                        act(vaug[:, tc, :, 0:64], p[:, 0:128].rearrange("p (h d) -> p h d", h=2), AF.Copy, [tp], [t_v])
                    proj_fm(xnT, xtok, wq, twq, 0, 128, 16, psr, evq)
                    proj_fm(xnT, xtok, wk, twk, 0, 128, 16, psr, evk)
                    proj_tm(xnT, xtok, wv, twv, 128, 16, psr, evv)
                    for hl in range(2):
                        for i in range(4):
                            po, tpo = pso.next()
                            nj = 4 * i + 4

                            def s_stage(j, i=i, hl=hl):
                                t0 = max(i * 512, j * 128)
                                ncol = (i + 1) * 512 - t0
                                c0 = t0 - i * 512
                                p, tp = psr.next()
                                mm(S, p[:, 0:ncol], [(kh[hl][0:70, j * 128:(j + 1) * 128], qh[hl][0:70, t0:t0 + ncol])],
                                   r=[t_kh[hl], t_qh[hl], t_kx[hl], t_qx[hl]], w=[tp])
                                pt_, tpt = PT.next()
                                act(pt_[:, 0:ncol], p[:, 0:ncol], AF.Exp, [tp], [tpt])
                                if j >= 4 * i:
                                    S.op("pool", lambda e, pt_=pt_: e.affine_select(out=pt_[:, 0:128], in_=pt_[:, 0:128], pattern=[[1, 128]],
                                                                                   compare_op=ALU.is_ge, fill=0.0, base=0, channel_multiplier=-1),
                                         r=[tpt], w=[tpt])
                                return (j, pt_, tpt, c0, ncol)

                            def pv_stage(st_, hl=hl, po=po, tpo=tpo, nj=nj):
                                j, pt_, tpt, c0, ncol = st_
                                mm(S, po[:, c0:c0 + ncol], [(vaug[:, j, hl, :], pt_[:, 0:ncol])],
                                   r=[t_v, tpt], w=[tpo], start=(j == 0), stop=(j == nj - 1))
                            inflight = []
                            for j in range(nj):
                                inflight.append(s_stage(j))
                                if len(inflight) > 2:
                                    pv_stage(inflight.pop(0))
                            while inflight:
                                pv_stage(inflight.pop(0))
                            rz_, trz = rz.next()
                            act(rz_[:], po[64:128, :], AF.Copy, [tpo], [trz])
                            recip(rz_[:], rz_[:], [trz], [trz])
                            tt("dve", yb[hl * 64:(hl + 1) * 64, i * 512:(i + 1) * 512], po[0:64, :], rz_[:], ALU.mult, [tpo, trz], [t_yb])
                    dma("sp", yT_d[512 + hp * 128:512 + (hp + 1) * 128, :], yb[:], r=[t_yb])
                S.flush()

        def mixer_c(l, win, xnT, xtok):
            with ExitStack() as k:
                wr = Ring([sb(k, [128, 16, 128], BF16) for _ in range(8)])
                psr = Ring([pst(k, [128, 512]) for _ in range(2)])
                ptr = Ring([pst(k, [128, 1024], BF16) for _ in range(1)])
                pmy = pst(k, [128, 512])
                psA = Ring([pst(k, [128, 512])[:, 0:128]])
                psU = Ring([pst(k, [128, 512])[:, 0:128] for _ in range(2)])
                psO = Ring([pmy[:, 0:128], pst(k, [128, 512])[:, 0:128]])
                t_pmy = psO.items[0][1]
                lbt = sb(k, [128, 16]); t_lb = Tok()
                gn = sb(k, [128, 1]); t_gn = Tok()
                onesf = sb(k, [128, T]); t_of = Tok()
                qf = sb(k, [128, T]); t_qf = Tok()
                bA = sb(k, [128, T]); t_A = Tok()
                bB = sb(k, [128, T]); t_B = Tok()
                bG = sb(k, [128, T]); t_G = Tok()
                dd = sb(k, [128, 32]); t_dd = Tok()
                qtil = sb(k, [128, T], BF16); t_qt = Tok()
                ktil = sb(k, [128, T], BF16); t_kt = Tok()
                ktok = [sb(k, [128, 16, 128], BF16) for _ in range(2)]; t_ktok = [Tok(), Tok()]
                vtok = sb(k, [128, 16, 128], BF16); t_v = Tok()
                gate = sb(k, [128, T], BF16); t_gate = Tok()
                oT = sb(k, [128, T]); t_oT = Tok()
                AT = Ring([sb(k, [128, 2, 64], BF16) for _ in range(2)])
                Tst = sb(k, [128, 128]); t_T = Tok()
                Sb = Ring([sb(k, [128, 128], BF16) for _ in range(2)])
                sq = (sb(k, [128, 512], BF16), Tok())
                rs_t = sb(k, [128, 512]); t_rs = Tok()
                t1 = sb(k, [128, 512]); t_t1 = Tok()
                ycT = Ring([sb(k, [128, T], BF16) for _ in range(2)])
                lbin = sb(k, [8, 128]); t_lbin = Tok()
                identf = sb(k, [128, 128]); t_idf = Tok()
                rm = sb(k, [128, 2]); t_rm = Tok()
                tri2 = sb(k, [128, 2, 64]); t_tri = Tok()
                dma("sp", identf[:], c_identf, w=[t_idf])
                dma("sp", rm[:], c_rm, w=[t_rm])
                dma("sp", tri2[:], c_tri2, w=[t_tri])
                dma("sp", lbin[:], hgrn_lb.rearrange("l (c p) -> (l c) p", p=128), w=[t_lbin])
                S.op("pe", lambda e: e.transpose(out=pmy[:, 256:264], in_=lbin[:], identity=identf[0:8, 0:8]), r=[t_lbin, t_idf], w=[t_pmy])
                cp("dve", lbt[:, 0:8], pmy[:, 256:264], [t_pmy], [t_lb])
                dma("sp", gn[:], hgrn_norm_g[l].rearrange("(p o) -> p o", o=1), w=[t_gn])
                memset("pool", onesf[:], 1.0, [t_of])
                if l == 0:
                    memset("dve", lbt[:, 8:12], 0.0, [t_lb])
                else:
                    tt("dve", lbt[:, 8:12], lbt[:, 4:8], lbt[:, 0:4], ALU.subtract, [t_lb], [t_lb])
                    act(lbt[:, 8:12], lbt[:, 8:12], AF.Sigmoid, [t_lb], [t_lb])
                ts("dve", lbt[:, 12:16], lbt[:, 8:12], -1.0, 1.0, ALU.mult, ALU.add, [t_lb], [t_lb])
                def ldc(h_):
                    return (load_w(wr, win, C0 + h_ * 128, 128), load_w(wr, win, C0 + 512 + h_ * 128, 128),
                            load_w(wr, win, C0 + 1024 + h_ * 128, 128), load_w(wr, win, C0 + 1536 + h_ * 128, 128))
                nxtw = ldc(0)
                for h in range(4):
                    yc, t_yc = ycT.next()
                    (wq, twq), (wzz, twz), (wi, twi), (wg, twg) = nxtw
                    if h + 1 < 4:
                        nxtw = ldc(h + 1)

                    def evq(tti, p, tp):
                        act(qf[:, tti * 512:(tti + 1) * 512], p[:], AF.Copy, [tp], [t_qf], scale=128 ** -0.5)

                    def evz(tti, p, tp, h=h):
                        act(bA[:, tti * 512:(tti + 1) * 512], p[:], AF.Sigmoid, [tp], [t_A])

                    def evv(tc, p, tp):
                        cp("dve", vtok[:, tc, :], p[:, 0:128], [tp], [t_v])

                    def evg(tti, p, tp):
                        act(gate[:, tti * 512:(tti + 1) * 512], p[:], AF.Sigmoid, [tp], [t_gate])
                    proj_fm(xnT, xtok, wq, twq, 0, 128, 16, psr, evq)
                    proj_fm(xnT, xtok, wzz, twz, 0, 128, 16, psr, evz)
                    proj_tm(xnT, xtok, wi, twi, 128, 16, psr, evv)
                    proj_fm(xnT, xtok, wg, twg, 0, 128, 16, psr, evg)
                    ts("dve", bA[:], bA[:], lbt[:, 12 + h:13 + h], lbt[:, 8 + h:9 + h], ALU.mult, ALU.add, [t_A, t_lb], [t_A])
                    act(bB[:], bA[:], AF.Ln, [t_A], [t_B])
                    ts("dve", bA[:], bA[:], -1.0, 1.0, ALU.mult, ALU.add, [t_A], [t_A])
                    S.op("dve", lambda e: e.tensor_tensor_scan(out=bG[:], data0=onesf[:], data1=bB[:], initial=0.0, op0=ALU.mult, op1=ALU.add),
                         r=[t_B, t_of], w=[t_G])
                    G3 = bG[:].rearrange("p (c t) -> p c t", t=64)
                    E3 = bB[:].rearrange("p (c t) -> p c t", t=64)
                    tt("dve", E3, G3, G3[:, :, 31:32].broadcast_to([128, 32, 64]), ALU.subtract, [t_G], [t_B])
                    tt("dve", dd[:, 0:31], G3[:, 1:32, 31], G3[:, 0:31, 31], ALU.subtract, [t_G], [t_dd])
                    act(dd[:, 0:31], dd[:, 0:31], AF.Exp, [t_dd], [t_dd])
                    act(bG[:], bB[:], AF.Exp, [t_B], [t_G])
                    act(bB[:], bB[:], AF.Exp, [t_B], [t_B], scale=-1.0)
                    tt("dve", qtil[:], qf[:], bG[:], ALU.mult, [t_qf, t_G], [t_qt])
                    tt("dve", ktil[:], bA[:], bB[:], ALU.mult, [t_A, t_B], [t_kt])
                    for half in range(2):
                        p, tp = ptr.next()

                        def fn(e, p=p, half=half):
                            ins = None
                            for j in range(8):
                                c = (half * 8 + j) * 128
                                ins = e.transpose(out=p[:, j * 128:(j + 1) * 128], in_=ktil[:, c:c + 128], identity=ident[:])
                            return ins
                        S.op("pe", fn, r=[t_kt, t_ident], w=[tp])
                        for hf in range(2):
                            act(ktok[hf][:, half * 8:(half + 1) * 8, :], p[:].rearrange("p (k t) -> p k t", k=8), AF.Copy, [tp, t_rm], [t_ktok[hf]],
                                scale=rm[:, hf:hf + 1])
                    sbc = None
                    for tc in range(16):
                        pa, tpa = psA.next()
                        at, tat = AT.next()
                        for hf in range(2):
                            c = 2 * tc + hf
                            mm(S, pa[:, hf * 64:(hf + 1) * 64], [(ktil[:, tc * 128:(tc + 1) * 128], qtil[:, c * 64:(c + 1) * 64])],
                               r=[t_kt, t_qt], w=[tpa])
                        tt("dve", at[:], pa[:].rearrange("p (h t) -> p h t", h=2), tri2[:], ALU.mult, [tpa, t_tri], [tat])
                        po, tpo = psO.next()
                        for hf in range(2):
                            c = 2 * tc + hf
                            pu, tpu = psU.next()
                            mm(S, pu[:], [(ktok[hf][:, tc, :], vtok[:, tc, :])], r=[t_ktok[hf], t_v], w=[tpu])
                            mm(S, po[:, hf * 64:(hf + 1) * 64], [(vtok[:, tc, :], at[:, hf, :])], r=[t_v, tat], w=[tpo],
                               start=True, stop=(c == 0))
                            if c > 0:
                                mm(S, po[:, hf * 64:(hf + 1) * 64], [(sbc[0][:], qtil[:, c * 64:(c + 1) * 64])], r=[sbc[1], t_qt], w=[tpo],
                                   start=False, stop=True)
                            if c == 0:
                                cp("dve", Tst[:], pu[:], [tpu], [t_T])
                            else:
                                stt(Tst[:], Tst[:], dd[:, c - 1:c], pu[:], ALU.mult, ALU.add, [t_T, t_dd, tpu], [t_T])
                            if c < 31:
                                sbc = Sb.next()
                                ts("dve", sbc[0][:], Tst[:], dd[:, c:c + 1], None, ALU.mult, None, [t_T, t_dd], [sbc[1]])
                        act(oT[:, tc * 128:(tc + 1) * 128], po[:], AF.Copy, [tpo], [t_oT])
                    for tti in range(4):
                        cs = slice(tti * 512, (tti + 1) * 512)
                        rms_part(sq, oT[:, cs], 512, psr, [t_oT], rs_t, t_rs)
                        stt(t1[:], oT[:, cs], gn[:, 0:1], rs_t[:], ALU.mult, ALU.mult, [t_oT, t_gn, t_rs], [t_t1])
                        tt("dve", yc[:, cs], t1[:], gate[:, cs], ALU.mult, [t_t1, t_gate], [t_yc])
                    dma("sp", yT_d[1024 + h * 128:1024 + (h + 1) * 128, :], yc[:], r=[t_yc])
                S.flush()

        def mixer_d(l, win, xnT, xtok):
            lam_init = 0.8 - 0.6 * float(np.exp(-0.3 * l))
            with ExitStack() as k:
                wr = Ring([sb(k, [128, 16, 128], BF16) for _ in range(6)])
                psr = Ring([pst(k, [128, 512]) for _ in range(4)])
                pacc = [pst(k, [128, 512]) for _ in range(4)]; t_acc = [Tok() for _ in range(4)]
                cosT = sb(k, [128, T]); t_cos = Tok()
                sinT = sb(k, [128, T]); t_sin = Tok()
                mk = sb(k, [128, 2]); t_mk = Tok()
                lamt = sb(k, [128, 256]); t_lam = Tok()
                lw_ = sb(k, [128, 128]); t_lw = Tok()
                ls = sb(k, [128, 8]); t_ls = Tok()
                gD = sb(k, [128, 1]); t_gD = Tok()
                qr = sb(k, [128, T], BF16); t_qr = Tok()
                k1p = sb(k, [128, T], BF16); t_k1 = Tok()
                k2p = sb(k, [128, T], BF16); t_k2 = Tok()
                vtok = sb(k, [128, 16, 128], BF16); t_v = Tok()
                tA = Ring([sb(k, [128, 512]) for _ in range(2)])
                tB = Ring([sb(k, [128, 512]) for _ in range(2)])
                PT = Ring([sb(k, [128, 512], BF16) for _ in range(6)])
                r1r = Ring([sb(k, [128, 512]) for _ in range(2)])
                r2r = Ring([sb(k, [128, 512]) for _ in range(2)])
                oD = sb(k, [128, 512]); t_oD = Tok()
                sq = (sb(k, [128, 512], BF16), Tok())
                rs_t = sb(k, [128, 512]); t_rs = Tok()
                ydT = Ring([sb(k, [128, T], BF16) for _ in range(2)])
                dma("sp", cosT[:], c_cos, w=[t_cos])
                dma("sp", sinT[:], c_sinA, w=[t_sin])
                dma("sp", mk[:], c_mk, w=[t_mk])
                dma("sp", lamt[:], diff_lambda[l].partition_broadcast(128), w=[t_lam])
                dma("sp", gD[:], diff_norm_g[l].rearrange("(p o) -> p o", o=1), w=[t_gD])
                ts("dve", gD[:], gD[:], 1.0 - lam_init, None, ALU.mult, None, [t_gD], [t_gD])
                tt("dve", lw_[:, 0:64], lamt[:, 0:64], lamt[:, 64:128], ALU.mult, [t_lam], [t_lw])
                tt("dve", lw_[:, 64:128], lamt[:, 128:192], lamt[:, 192:256], ALU.mult, [t_lam], [t_lw])
                S.op("dve", lambda e: e.reduce_sum(out=ls[:, 0:1], in_=lw_[:, 0:64], axis=AX.X), r=[t_lw], w=[t_ls])
                S.op("dve", lambda e: e.reduce_sum(out=ls[:, 1:2], in_=lw_[:, 64:128], axis=AX.X), r=[t_lw], w=[t_ls])
                act(ls[:, 2:4], ls[:, 0:2], AF.Exp, [t_ls], [t_ls])
                tt("dve", ls[:, 4:5], ls[:, 3:4], ls[:, 2:3], ALU.subtract, [t_ls], [t_ls])
                ts("dve", ls[:, 5:6], ls[:, 4:5], -lam_init, None, ALU.add, None, [t_ls], [t_ls])
                def ldd(h_):
                    return (load_w(wr, win, D0 + h_ * 128, 128), load_w(wr, win, D0 + 512 + h_ * 128, 128),
                            load_w(wr, win, D0 + 1024 + h_ * 128, 128))
                nxtw = ldd(0)
                for h in range(4):
                    yd, t_yd = ydT.next()
                    (wq, twq), (wk, twk), (wv, twv) = nxtw
                    if h + 1 < 4:
                        nxtw = ldd(h + 1)

                    def rope(tti, p, tp, dst):
                        cs = slice(tti * 512, (tti + 1) * 512)
                        a_, ta_ = tA.next()
                        b_, tb_ = tB.next()
                        tt("dve", a_[:], p[:], cosT[:, cs], ALU.mult, [tp, t_cos], [ta_])
                        tt("dve", b_[0:64, :], p[64:128, :], sinT[64:128, cs], ALU.mult, [tp, t_sin], [tb_])
                        tt("dve", b_[64:128, :], p[0:64, :], sinT[0:64, cs], ALU.mult, [tp, t_sin], [tb_])
                        return a_, ta_, b_, tb_, cs

                    def evq(tti, p, tp):
                        a_, ta_, b_, tb_, cs = rope(tti, p, tp, None)
                        tt("dve", qr[:, cs], a_[:], b_[:], ALU.add, [ta_, tb_], [t_qr])

                    def evk(tti, p, tp):
                        a_, ta_, b_, tb_, cs = rope(tti, p, tp, None)
                        tt("dve", a_[:], a_[:], b_[:], ALU.add, [ta_, tb_], [ta_])
                        act(k1p[:, cs], a_[:], AF.Copy, [ta_, t_mk], [t_k1], scale=mk[:, 0:1])
                        act(k2p[:, cs], a_[:], AF.Copy, [ta_, t_mk], [t_k2], scale=mk[:, 1:2])

                    def evv(tc, p, tp):
                        act(vtok[:, tc, :], p[:, 0:128], AF.Copy, [tp], [t_v])
                    proj_fm(xnT, xtok, wq, twq, 0, 128, 16, psr, evq)
                    proj_fm(xnT, xtok, wk, twk, 0, 128, 16, psr, evk)
                    proj_tm(xnT, xtok, wv, twv, 128, 16, psr, evv)
                    pend = None
                    for i in range(4):
                        nj = 4 * i + 4

                        def s_stage(j, i=i):
                            t0 = max(i * 512, j * 128)
                            ncol = (i + 1) * 512 - t0
                            c0 = t0 - i * 512
                            res_ = []
                            for m, (kp, tkp) in enumerate(((k1p, t_k1), (k2p, t_k2))):
                                p, tp = psr.next()
                                mm(S, p[:, 0:ncol], [(kp[:, j * 128:(j + 1) * 128], qr[:, t0:t0 + ncol])], r=[tkp, t_qr], w=[tp])
                                pt_, tpt = PT.next()
                                act(pt_[:, 0:ncol], p[:, 0:ncol], AF.Exp, [tp], [tpt], scale=0.125)
                                if j >= 4 * i:
                                    memset("pool", pt_[64:128, 0:64], 0.0, [tpt])
                                res_.append((pt_, tpt))
                            return (j, res_, c0, ncol)

                        def pv_stage(st_, nj=nj):
                            j, res_, c0, ncol = st_
                            for m, (pt_, tpt) in enumerate(res_):
                                mm(S, pacc[2 * m][:, c0:c0 + ncol], [(vtok[:, j, :], pt_[:, 0:ncol])], r=[t_v, tpt], w=[t_acc[2 * m]],
                                   start=(j == 0), stop=(j == nj - 1))
                                mm(S, pacc[2 * m + 1][:, c0:c0 + ncol], [(ones_bf[:], pt_[:, 0:ncol])], r=[t_ones, tpt], w=[t_acc[2 * m + 1]],
                                   start=(j == 0), stop=(j == nj - 1))
                        def fin2(i_, r1, t_r1, r2, t_r2, yd=yd, t_yd=t_yd):
                            cs = slice(i_ * 512, (i_ + 1) * 512)
                            stt(oD[:], r2[:], ls[:, 5:6], r1[:], ALU.mult, ALU.add, [t_r1, t_r2, t_ls], [t_oD])
                            rms_part(sq, oD[:], 512, psr, [t_oD], rs_t, t_rs)
                            stt(yd[:, cs], oD[:], gD[:, 0:1], rs_t[:], ALU.mult, ALU.mult, [t_oD, t_gD, t_rs], [t_yd])
                        prev = None
                        for j in range(nj):
                            cur = s_stage(j)
                            if prev is not None:
                                pv_stage(prev)
                            prev = cur
                            if j == 1 and pend is not None:
                                fin2(*pend)
                                pend = None
                        pv_stage(prev)
                        r1, t_r1 = r1r.next()
                        r2, t_r2 = r2r.next()
                        recip(r1[:], pacc[1][:], [t_acc[1]], [t_r1])
                        tt("dve", r1[:], pacc[0][:], r1[:], ALU.mult, [t_acc[0], t_r1], [t_r1])
                        recip(r2[:], pacc[3][:], [t_acc[3]], [t_r2])
                        tt("dve", r2[:], pacc[2][:], r2[:], ALU.mult, [t_acc[2], t_r2], [t_r2])
                        pend = (i, r1, t_r1, r2, t_r2)
                    fin2(*pend)
                    pend = None
                    dma("sp", yT_d[1536 + h * 128:1536 + (h + 1) * 128, :], yd[:], r=[t_yd])
                S.flush()

        def phase_gate(l, xnT, xtok):
            with ExitStack() as k:
                yall = sb(k, [128, 16, T], BF16); t_y = Tok()
                wgr = Ring([sb(k, [128, 16, 128], BF16) for _ in range(3)])
                wbr = Ring([sb(k, [128, 4, 128], BF16) for _ in range(3)])
                bg = sb(k, [128, 4, 16]); t_bg = Tok()
                psg = Ring([pst(k, [128, 512]) for _ in range(4)])
                psb = Ring([pst(k, [128, 512]) for _ in range(4)])
                gt = Ring([sb(k, [128, 512]) for _ in range(3)])
                tmp = Ring([sb(k, [128, 512]) for _ in range(3)])
                accs = [sb(k, [128, 512]) for _ in range(4)]; t_accs = [Tok() for _ in range(4)]
                mo = Ring([sb(k, [128, T], BF16) for _ in range(2)])
                for q4 in range(4):
                    dma("sp", yall[:, q4 * 4:(q4 + 1) * 4, :], yT_d[q4 * 512:(q4 + 1) * 512, :].rearrange("(kc p) t -> p kc t", p=128), w=[t_y])
                bgin = sb(k, [64, 128]); t_bgin = Tok()
                identf = sb(k, [128, 128]); t_idf = Tok()
                dma("sp", identf[:], c_identf, w=[t_idf])
                dma("sp", bgin[:], b_gate[l].rearrange("n (oc p) -> (n oc) p", p=128), w=[t_bgin])
                p0, tp0 = psg.next()
                S.op("pe", lambda e: e.transpose(out=p0[:, 0:64], in_=bgin[:], identity=identf[0:64, 0:64]), r=[t_bgin, t_idf], w=[tp0])
                cp("dve", bg[:], p0[:, 0:64].rearrange("p (n oc) -> p n oc", n=4), [tp0], [t_bg])
                def ldg(i_):
                    oc_, n_ = divmod(i_, 4)
                    return load_w(wgr, w_gate[l, n_], oc_ * 128, 128), load_w(wbr, w_branch[l, n_], oc_ * 128, 128, KC=4)
                nxtw = ldg(0)
                for oc in range(16):
                    m_, tm_ = mo.next()
                    for n in range(4):
                        (wg, twg), (wb_, twb) = nxtw
                        if oc * 4 + n + 1 < 64:
                            nxtw = ldg(oc * 4 + n + 1)
                        for tti in range(4):
                            cs = slice(tti * 512, (tti + 1) * 512)
                            pg, tpg = psg.next()
                            pb, tpb = psb.next()
                            mm(S, pg[:], [(wg[:, kc, :], xnT[:, kc, cs]) for kc in range(16)], r=[xtok, twg], w=[tpg])
                            mm(S, pb[:], [(wb_[:, kc, :], yall[:, n * 4 + kc, cs]) for kc in range(4)], r=[t_y, twb], w=[tpb])
                            g_, tg_ = gt.next()
                            act(g_[:], pg[:], AF.Sigmoid, [tpg, t_bg], [tg_], bias=bg[:, n, oc:oc + 1])
                            if n == 0:
                                tt("dve", accs[tti][:], g_[:], pb[:], ALU.mult, [tg_, tpb], [t_accs[tti]])
                            else:
                                t_, tt_ = tmp.next()
                                tt("dve", t_[:], g_[:], pb[:], ALU.mult, [tg_, tpb], [tt_])
                                if n < 3:
                                    tt("pool", accs[tti][:], accs[tti][:], t_[:], ALU.add, [t_accs[tti], tt_], [t_accs[tti]])
                                else:
                                    tt("pool", m_[:, cs], accs[tti][:], t_[:], ALU.add, [t_accs[tti], tt_], [tm_])
                    dma("sp", mixT_d[oc * 128:(oc + 1) * 128, :], m_[:], r=[tm_])
                S.flush()

        def phase_wout(l, src_rows):
            with ExitStack() as k:
                mT = sb(k, [128, 16, T], BF16); t_m = Tok()
                wo = sb(k, [128, 16, D], BF16); t_wo = [Tok() for _ in range(4)]
                xr = Ring([sb(k, [128, D]) for _ in range(2)])
                xo = Ring([sb(k, [128, D]) for _ in range(2)])
                psr = Ring([pst(k, [128, 512]) for _ in range(4)])
                for q4 in range(4):
                    dma("sp", mT[:, q4 * 4:(q4 + 1) * 4, :], mixT_d[q4 * 512:(q4 + 1) * 512, :].rearrange("(kc p) t -> p kc t", p=128), w=[t_m])
                for ct in range(4):
                    dma("pool", wo[:, :, ct * 512:(ct + 1) * 512], w_out[l][:, ct * 512:(ct + 1) * 512].rearrange("(kc p) n -> p kc n", p=128), w=[t_wo[ct]], ndesc=128)
                for tc in range(16):
                    xi, txi = xr.next()
                    xo_, txo = xo.next()
                    dma("sp", xi[:], src_rows[tc * 128:(tc + 1) * 128, :], w=[txi])
                    for ct in range(4):
                        cs = slice(ct * 512, (ct + 1) * 512)
                        p, tp = psr.next()
                        mm(S, p[:], [(mT[:, kc, tc * 128:(tc + 1) * 128], wo[:, kc, cs]) for kc in range(16)], r=[t_m, t_wo[ct]], w=[tp])
                        tt("dve", xo_[:, cs], p[:], xi[:, cs], ALU.add, [tp, txi], [txo])
                    dma("sp", xa_d[tc * 128:(tc + 1) * 128, :], xo_[:], r=[txo])
                S.flush()

        def phase_ffn_up(l, hnT, htok):
            with ExitStack() as k:
                wr = Ring([sb(k, [128, 16, 128], BF16) for _ in range(4)])
                cwin = sb(k, [88, 4, 128]); t_cwin = Tok()
                cw = sb(k, [128, 4, 88]); t_cw = Tok()
                identf = sb(k, [128, 128]); t_idf = Tok()
                ha = Ring([sb(k, [128, T + 2]) for _ in range(2)])
                hg = Ring([sb(k, [128, T + 2]) for _ in range(2)])
                ca = Ring([sb(k, [128, T]) for _ in range(2)])
                cg = Ring([sb(k, [128, T]) for _ in range(2)])
                ao = Ring([sb(k, [128, T], BF16) for _ in range(2)])
                psa = Ring([pst(k, [128, 512]) for _ in range(4)])
                psg = Ring([pst(k, [128, 512]) for _ in range(4)])
                dma("sp", identf[:], c_identf, w=[t_idf])
                for kk in range(3):
                    dma("sp", cwin[:, kk, :], ffn_conv_w[l, kk].rearrange("(c p) -> c p", p=128), w=[t_cwin])
                dma("sp", cwin[:, 3, :], ffn_conv_b[l].rearrange("(c p) -> c p", p=128), w=[t_cwin])
                p0, tp0 = psa.next()

                def fnT(e):
                    ins = None
                    for kk in range(4):
                        ins = e.transpose(out=p0[:, kk * 88:(kk + 1) * 88], in_=cwin[:, kk, :], identity=identf[0:88, 0:88])
                    return ins
                S.op("pe", fnT, r=[t_cwin, t_idf], w=[tp0])
                cp("dve", cw[:], p0[:, 0:352].rearrange("p (k c) -> p k c", k=4), [tp0], [t_cw])
                for r_ in (ha, hg):
                    for (tb, ttk) in r_.items:
                        memset("pool", tb[:, 0:2], 0.0, [ttk])
                wup = ffn_w_up[l]
                def ldw(j):
                    return load_w(wr, wup, j * 128, 128), load_w(wr, wup, (44 + j) * 128, 128)
                nxtw = ldw(0)
                for j in range(44):
                    (wa, twa), (wg, twg) = nxtw
                    if j + 1 < 44:
                        nxtw = ldw(j + 1)
                    ha_, tha = ha.next()
                    hg_, thg = hg.next()
                    for tti in range(4):
                        cs = slice(tti * 512, (tti + 1) * 512)
                        pa, tpa = psa.next()
                        pg, tpg = psg.next()
                        mm(S, pa[:], [(wa[:, kc, :], hnT[:, kc, cs]) for kc in range(16)], r=[htok, twa], w=[tpa])
                        mm(S, pg[:], [(wg[:, kc, :], hnT[:, kc, cs]) for kc in range(16)], r=[htok, twg], w=[tpg])
                        act(ha_[:, 2 + tti * 512:2 + (tti + 1) * 512], pa[:], AF.Copy, [tpa], [tha])
                        act(hg_[:, 2 + tti * 512:2 + (tti + 1) * 512], pg[:], AF.Copy, [tpg], [thg])
                    ca_, tca = ca.next()
                    cg_, tcg = cg.next()
                    ao_, tao = ao.next()
                    for (hb, thb, c_, tc_, jj) in ((ha_, tha, ca_, tca, j), (hg_, thg, cg_, tcg, 44 + j)):
                        act(c_[:], hb[:, 2:T + 2], AF.Identity, [thb, t_cw], [tc_], scale=cw[:, 2, jj:jj + 1], bias=cw[:, 3, jj:jj + 1])
                        stt(c_[:], hb[:, 1:T + 1], cw[:, 1, jj:jj + 1], c_[:], ALU.mult, ALU.add, [thb, t_cw, tc_], [tc_])
                        stt(c_[:], hb[:, 0:T], cw[:, 0, jj:jj + 1], c_[:], ALU.mult, ALU.add, [thb, t_cw, tc_], [tc_])
                    act(ca_[:], ca_[:], AF.Gelu_apprx_tanh, [tca], [tca])
                    tt("pool", ao_[:], ca_[:], cg_[:], ALU.mult, [tca, tcg], [tao])
                    dma("sp", actT_d[j * 128:(j + 1) * 128, :], ao_[:], r=[tao])
                S.flush()

        def phase_ffn_down(l, dst_rows):
            with ExitStack() as k:
                aT = sb(k, [128, 44, 1024], BF16); t_a = Tok()
                wd = Ring([sb(k, [128, 44, 256], BF16) for _ in range(2)])
                xr = Ring([sb(k, [128, 256]) for _ in range(4)])
                xo = Ring([sb(k, [128, 256]) for _ in range(4)])
                psr = Ring([pst(k, [128, 512]) for _ in range(4)])
                wdn = ffn_w_down[l]
                for half in range(2):
                    for q4 in range(4):
                        dma("sp", aT[:, q4 * 11:(q4 + 1) * 11, :],
                            actT_d[q4 * 1408:(q4 + 1) * 1408, half * 1024:(half + 1) * 1024].rearrange("(kc p) t -> p kc t", p=128), w=[t_a])
                    for ct in range(8):
                        w_, tw_ = load_w(wd, wdn, ct * 256, 256, KC=44)
                        for tc in range(8):
                            r0 = half * 1024 + tc * 128
                            xi, txi = xr.next()
                            xo_, txo = xo.next()
                            dma("sp", xi[:], xa_d[r0:r0 + 128, ct * 256:(ct + 1) * 256], w=[txi])
                            p, tp = psr.next()
                            mm(S, p[:, 0:256], [(aT[:, kc, tc * 128:(tc + 1) * 128], w_[:, kc, :]) for kc in range(44)], r=[t_a, tw_], w=[tp])
                            tt("dve", xo_[:], p[:, 0:256], xi[:], ALU.add, [tp, txi], [txo])
                            dma("sp", dst_rows[r0:r0 + 128, ct * 256:(ct + 1) * 256], xo_[:], r=[txo])
                S.flush()

        def phase_final(src, dst):
            with ExitStack() as k:
                Gt = sb(k, [128, D]); tG = Tok()
                xin = Ring([sb(k, [128, D]) for _ in range(3)])
                xs = Ring([sb(k, [128, D]) for _ in range(3)])
                junk = sb(k, [128, D], BF16); tj = Tok()
                st = Ring([sb(k, [128, 4]) for _ in range(3)])
                dma("sp", Gt[:], norm_final_g.partition_broadcast(128), w=[tG])

                def fa(tc):
                    xi, txi = xin.next()
                    xo, txo = xs.next()
                    sv, tsv = st.next()
                    dma("sp", xi[:], src[tc * 128:(tc + 1) * 128, :], w=[txi])
                    act(junk[:], xi[:], AF.Square, [txi], [tj, tsv], accum_out=sv[:, 0:1])
                    act(sv[:, 1:2], sv[:, 0:1], AF.Sqrt, [tsv, t_eps], [tsv], scale=1.0 / D, bias=epst[:])
                    recip(sv[:, 2:3], sv[:, 1:2], [tsv], [tsv])
                    stt(xo[:], xi[:], sv[:, 2:3], Gt[:], ALU.mult, ALU.mult, [txi, tsv, tG], [txo])
                    return xo, txo
                cur = fa(0)
                for tc in range(16):
                    nxt = fa(tc + 1) if tc + 1 < 16 else None
                    dma("sp", dst[tc * 128:(tc + 1) * 128, :], cur[0][:], r=[cur[1]])
                    cur = nxt
                S.flush()

        scr = [xb_d, xc_d]
        for s in range(NS):
            src = x_in[s]
            for l in range(NLAY):
                dst = scr[l % 2]
                layer(l, src, dst)
                src = dst
            if STOP >= 10: phase_final(src, out[s])
    return nc


def _constants():
    ident = np.eye(128, dtype=np.float32)
    half = 32
    inv_freq = (10000.0 ** (-np.arange(half, dtype=np.float32) / half)).astype(np.float32)
    pos = np.arange(T, dtype=np.float32)
    ang = (pos[None, :] * inv_freq[:, None]).astype(np.float32)
    cos = np.cos(ang).astype(np.float32)
    sin = np.sin(ang).astype(np.float32)
    c_cos = np.tile(cos, (4, 1))
    c_sinA = np.concatenate([sin, sin, -sin, -sin], axis=0)
    tri = (np.arange(64)[None, :] >= (np.arange(128)[:, None] % 64)).astype(np.float32)
    g = (np.arange(128) // 32) % 2
    mk = np.stack([(g == 0), (g == 1)], axis=1).astype(np.float32)
    return {
        "c_ident": ident.astype(ml_dtypes.bfloat16),
        "c_identf": ident,
        "c_cos": np.ascontiguousarray(c_cos),
        "c_sinA": np.ascontiguousarray(c_sinA),
        "c_tri": tri,
        "c_mk": mk,
        "c_rm": np.stack([np.arange(128) < 64, np.arange(128) >= 64], axis=1).astype(np.float32),
        "c_tri2": np.ascontiguousarray(np.stack([tri * (np.arange(128)[:, None] < 64), tri * (np.arange(128)[:, None] >= 64)], axis=1).astype(np.float32)),
    }


def _perm_cols():
    r = np.arange(128)
    comp = (r // 32) % 2
    part = r // 64
    return comp * 64 + part * 32 + (r % 32)


def _prep_shared(inp):
    f = lambda a: np.ascontiguousarray(np.asarray(a, dtype=np.float32))
    w_in = f(inp["w_in"]).copy()
    pc = _perm_cols()
    for base in (D0, D0 + 512):
        for h in range(4):
            c = base + h * 128
            w_in[:, :, c:c + 128] = w_in[:, :, c + pc]
    m = {
        "norm_mix_g": f(inp["norm_mix_g"]), "w_in": w_in, "fox_b_f": f(inp["fox_b_f"]),
        "gmlp_ln_g": f(inp["gmlp_ln_g"]), "gmlp_ln_b": f(inp["gmlp_ln_b"]),
        "gmlp_w_sT": f(np.transpose(np.asarray(inp["gmlp_w_s"]), (0, 1, 3, 2))),
        "gmlp_b_s": f(np.asarray(inp["gmlp_b_s"]).reshape(DEPTH, 512)),
        "hgrn_lb_logits": f(inp["hgrn_lb_logits"]), "hgrn_norm_g": f(inp["hgrn_norm_g"]),
        "diff_lambda": f(np.asarray(inp["diff_lambda"]).reshape(DEPTH, 256)), "diff_norm_g": f(inp["diff_norm_g"]),
        "w_branch": f(inp["w_branch"]), "w_gate": f(inp["w_gate"]), "b_gate": f(inp["b_gate"]),
        "w_out": f(inp["w_out"]), "norm_ffn_g": f(inp["norm_ffn_g"]), "ffn_w_up": f(inp["ffn_w_up"]),
        "ffn_conv_w": f(inp["ffn_conv_w"]), "ffn_conv_b": f(inp["ffn_conv_b"]), "ffn_w_down": f(inp["ffn_w_down"]),
        "norm_final_g": f(inp["norm_final_g"]),
    }
    m.update(_constants())
    return m


def kernel(**inputs):
    x = np.ascontiguousarray(np.asarray(inputs["x"], dtype=np.float32))
    shared = _prep_shared(inputs)
    NS = x.shape[0] // NCORES
    nc = build_nc(NS=NS, NLAY=DEPTH, dbg=False)
    in_maps = []
    for c in range(NCORES):
        m = dict(shared)
        m["x"] = np.ascontiguousarray(x[c * NS:(c + 1) * NS])
        in_maps.append(m)
    res = run_bass_kernel_spmd(nc, in_maps, core_ids=list(range(NCORES)))
    return np.concatenate([np.asarray(r["out"], dtype=np.float32) for r in res.results], axis=0)
```

```python
import numpy as np
import ml_dtypes
import concourse.bass as bass
import concourse.mybir as mybir
from concourse.bass_utils import run_bass_kernel_spmd

F32 = mybir.dt.float32
BF16 = mybir.dt.bfloat16
ALU = mybir.AluOpType
AF = mybir.ActivationFunctionType
AX = mybir.AxisListType

D = 2048
T = 2048
DEPTH = 2
BW = 512
DFF = 5632
EPS = 1e-6
A0 = 0
B0 = 1024
C0 = 1024 + 1544
D0 = C0 + 2048
IN_COLS = 6152
NCORES = 8


class Tok:
    __slots__ = ("lw", "rd", "name")

    def __init__(self, name=""):
        self.lw = None
        self.rd = []
        self.name = name


class Op:
    __slots__ = ("eng", "fn", "deps", "sig", "val", "dma", "sem", "lane_wait")


CE = ("pe", "act", "dve", "pool")
FULLSYNC = True
NLANE = 12


class Sched:
    def __init__(self, nc):
        self.nc = nc
        self.sem = {e: nc.alloc_semaphore("c_" + e) for e in CE}
        self.cnt = {e: 0 for e in CE}
        self.lanes = {q: [[nc.alloc_semaphore("l_%s%d" % (q, i)), 0] for i in range(NLANE)] for q in ("sp", "pool")}
        self.rr = {"sp": 0, "pool": 0}
        self.ops = {e: [] for e in ("pe", "act", "dve", "pool", "sp")}
        self.toks = set()
        self.waited = {}
        self.nphase = 0
        self.pool_out = []

    def _deps(self, o, r, w):
        deps = []
        seen = set()

        def add(d, raw):
            if d is None or d is o or id(d) in seen:
                return
            if (not d.dma) and (not o.dma) and d.eng == o.eng and (d.eng == "pe" or (not raw and not FULLSYNC)):
                return
            seen.add(id(d))
            deps.append(d)
            if not d.dma:
                d.sig = True

        for t in r:
            add(t.lw, True)
        for t in w:
            add(t.lw, False)
            for d in t.rd:
                add(d, False)
        o.deps = deps
        for t in r:
            t.rd.append(o)
            self.toks.add(t)
        for t in w:
            t.lw = o
            t.rd = []
            self.toks.add(t)

    def op(self, eng, fn, r=(), w=()):
        o = Op()
        o.eng = eng
        o.fn = fn
        o.dma = False
        o.sig = False
        o.val = None
        o.sem = None
        o.lane_wait = 0
        self._deps(o, r, w)
        self.ops[eng].append(o)
        return o

    def dma(self, q, fn, r=(), w=(), ndesc=0):
        o = Op()
        o.eng = q
        o.fn = fn
        o.dma = True
        o.sig = True
        extra = []
        if q == "pool" and ndesc:
            while self.pool_out and sum(n for _, n in self.pool_out) + ndesc > 640:
                extra.append(self.pool_out.pop(0)[0])
            self.pool_out.append((o, ndesc))
        lane = self.lanes[q][self.rr[q]]
        self.rr[q] = (self.rr[q] + 1) % NLANE
        o.lane_wait = 16 * lane[1]
        lane[1] += 1
        o.sem = lane[0]
        o.val = 16 * lane[1]
        self._deps(o, r, w)
        for d in extra:
            if d not in o.deps:
                o.deps.append(d)
        self.ops[q].append(o)
        return o

    def flush(self):
        self.pool_out = []
        for e in CE:
            c = self.cnt[e]
            for o in self.ops[e]:
                if (not o.dma) and o.sig:
                    c += 1
                    o.val = c
            self.cnt[e] = c
        ops = self.ops
        self.ops = {e: [] for e in ("pe", "act", "dve", "pool", "sp")}
        waited = self.waited
        sems = self.sem
        lanes = self.lanes

        def emit(e, name):
            def w8(sem, val):
                key = (name, sem.num)
                if waited.get(key, 0) >= val:
                    return
                e.wait_ge(sem, val)
                waited[key] = val

            for o in ops[name]:
                for d in o.deps:
                    if d.dma:
                        w8(d.sem, d.val)
                    else:
                        w8(sems[d.eng], d.val)
                if o.dma and o.lane_wait:
                    w8(o.sem, o.lane_wait)
                ins = o.fn(e)
                if o.dma:
                    ins.then_inc(o.sem, 16)
                elif o.sig:
                    ins.then_inc(sems[o.eng], 1)
            if name in lanes:
                for sem, cnt in lanes[name]:
                    if cnt:
                        w8(sem, 16 * cnt)

        with self.nc.Block() as blk:
            @blk.sync
            def _(e):
                emit(e, "sp")

            @blk.gpsimd
            def _(e):
                emit(e, "pool")

            @blk.tensor
            def _(e):
                emit(e, "pe")

            @blk.scalar
            def _(e):
                emit(e, "act")

            @blk.vector
            def _(e):
                emit(e, "dve")
        for t in self.toks:
            t.lw = None
            t.rd = []
        self.toks = set()
        self.nphase += 1


class Ring:
    def __init__(self, items):
        self.items = [(t, Tok()) for t in items]
        self.i = 0

    def next(self):
        it = self.items[self.i]
        self.i = (self.i + 1) % len(self.items)
        return it


def mm(S, out_ap, pairs, r, w, start=True, stop=True):
    def fn(e, pairs=pairs, out_ap=out_ap, start=start, stop=stop):
        n = len(pairs)
        ins = None
        for i, (l, rh) in enumerate(pairs):
            ins = e.matmul(out_ap, l, rh, start=(start and i == 0), stop=(stop and i == n - 1))
        return ins
    return S.op("pe", fn, r=r, w=w)


from contextlib import ExitStack

_uid = [0]


def build_nc(NS=2, NLAY=2, dbg=False, STOP=99):
    nc = bass.Bass("TRN2", target_bir_lowering=False)

    def din(name, shape, dt=F32):
        return nc.dram_tensor(name, list(shape), dt, kind="ExternalInput").ap()

    x_in = din("x", [NS, T, D])
    norm_mix_g = din("norm_mix_g", [DEPTH, D])
    w_in = din("w_in", [DEPTH, D, IN_COLS])
    fox_b_f = din("fox_b_f", [DEPTH, 8])
    gmlp_ln_g = din("gmlp_ln_g", [DEPTH, BW])
    gmlp_ln_b = din("gmlp_ln_b", [DEPTH, BW])
    gmlp_w_sT = din("gmlp_w_sT", [DEPTH, 4, 128, 128])
    gmlp_b_s = din("gmlp_b_s", [DEPTH, 512])
    hgrn_lb = din("hgrn_lb_logits", [DEPTH, 512])
    hgrn_norm_g = din("hgrn_norm_g", [DEPTH, 128])
    diff_lambda = din("diff_lambda", [DEPTH, 256])
    diff_norm_g = din("diff_norm_g", [DEPTH, 128])
    w_branch = din("w_branch", [DEPTH, 4, BW, D])
    w_gate = din("w_gate", [DEPTH, 4, D, D])
    b_gate = din("b_gate", [DEPTH, 4, D])
    w_out = din("w_out", [DEPTH, D, D])
    norm_ffn_g = din("norm_ffn_g", [DEPTH, D])
    ffn_w_up = din("ffn_w_up", [DEPTH, D, 2 * DFF])
    ffn_conv_w = din("ffn_conv_w", [DEPTH, 3, 2 * DFF])
    ffn_conv_b = din("ffn_conv_b", [DEPTH, 2 * DFF])
    ffn_w_down = din("ffn_w_down", [DEPTH, DFF, D])
    norm_final_g = din("norm_final_g", [D])
    c_ident = din("c_ident", [128, 128], BF16)
    c_cos = din("c_cos", [128, T])
    c_sinA = din("c_sinA", [128, T])
    c_tri = din("c_tri", [128, 64])
    c_mk = din("c_mk", [128, 2])
    c_identf = din("c_identf", [128, 128])
    c_rm = din("c_rm", [128, 2])
    c_tri2 = din("c_tri2", [128, 2, 64])

    out = nc.dram_tensor("out", [NS, T, D], F32, kind="ExternalOutput").ap()
    okind = "ExternalOutput" if dbg else "Internal"

    def dscr(name, shape, dt):
        if dbg:
            return nc.dram_tensor(name, list(shape), dt, kind="ExternalOutput").ap()
        return nc.dram_tensor(name, list(shape), dt).ap()

    yT_d = dscr("yT_d", [D, T], BF16)
    mixT_d = dscr("mixT_d", [D, T], BF16)
    xa_d = dscr("xa_d", [T, D], F32)
    xb_d = dscr("xb_d", [T, D], F32)
    xc_d = dscr("xc_d", [T, D], F32)
    actT_d = dscr("actT_d", [DFF, T], BF16)
    ex_d = dscr("ex_d", [2, 8, 6, T], BF16)

    S = Sched(nc)

    def sb(k, shape, dt=F32):
        _uid[0] += 1
        return k.enter_context(nc.sbuf_tensor("s%d" % _uid[0], list(shape), dt))

    def pst(k, shape, dt=F32):
        _uid[0] += 1
        return k.enter_context(nc.psum_tensor("p%d" % _uid[0], list(shape), dt))

    def dma(q, out_ap, in_ap, r=(), w=(), slow=False, ndesc=0):
        if slow:
            return S.dma(q, lambda e, o=out_ap, i=in_ap: e.dma_start(out=o, in_=i, allow_slow_non_contiguous=True), r=r, w=w, ndesc=ndesc)
        return S.dma(q, lambda e, o=out_ap, i=in_ap: e.dma_start(out=o, in_=i), r=r, w=w, ndesc=ndesc)

    def act(out_ap, in_ap, func, r, w, **kw):
        return S.op("act", lambda e, o=out_ap, i=in_ap, f=func, kw=kw: e.activation(out=o, in_=i, func=f, **kw), r=r, w=w)

    def tt(eng, out_ap, a, b, op, r, w):
        return S.op(eng, lambda e, o=out_ap, a=a, b=b, op=op: e.tensor_tensor(out=o, in0=a, in1=b, op=op), r=r, w=w)

    def ts(eng, out_ap, a, s1, s2, op0, op1, r, w):
        if op1 is None:
            return S.op(eng, lambda e, o=out_ap, a=a, s1=s1, op0=op0: e.tensor_scalar(out=o, in0=a, scalar1=s1, scalar2=None, op0=op0), r=r, w=w)
        return S.op(eng, lambda e, o=out_ap, a=a, s1=s1, s2=s2, op0=op0, op1=op1: e.tensor_scalar(out=o, in0=a, scalar1=s1, scalar2=s2, op0=op0, op1=op1), r=r, w=w)

    def stt(out_ap, a, sc, b, op0, op1, r, w):
        return S.op("dve", lambda e, o=out_ap, a=a, sc=sc, b=b, op0=op0, op1=op1: e.scalar_tensor_tensor(out=o, in0=a, scalar=sc, in1=b, op0=op0, op1=op1), r=r, w=w)

    def recip(out_ap, in_ap, r, w):
        return S.op("dve", lambda e, o=out_ap, i=in_ap: e.reciprocal(out=o, in_=i), r=r, w=w)

    def cp(eng, out_ap, in_ap, r, w):
        return S.op(eng, lambda e, o=out_ap, i=in_ap: e.tensor_copy(out=o, in_=i), r=r, w=w)

    def memset(eng, ap, v, w):
        return S.op(eng, lambda e, ap=ap, v=v: e.memset(ap, v), w=w)

    G = ExitStack()
    with G:
        ident = sb(G, [128, 128], BF16); t_ident = Tok()
        ones_bf = sb(G, [128, 128], BF16); t_ones = Tok()
        epst = sb(G, [128, 1]); t_eps = Tok()
        dma("sp", ident[:], c_ident, w=[t_ident])
        memset("dve", ones_bf[:], 1.0, [t_ones])
        memset("dve", epst[:], EPS, [t_eps])
        CONST_R = [t_ident, t_ones, t_eps]

        def load_w(ring, wap, c0, ncol, KC=16):
            wt, tw = ring.next()
            dma("pool", wt[:, 0:KC, 0:ncol], wap[:, c0:c0 + ncol].rearrange("(kc p) n -> p kc n", p=128), w=[tw], ndesc=KC * 8)
            return wt, tw

        def proj_fm(xT, xtok, wt, tw, col_off, M, KC, psr, evac):
            for tti in range(4):
                p, tp = psr.next()
                pairs = [(wt[:, kc, col_off:col_off + M], xT[:, kc, tti * 512:(tti + 1) * 512]) for kc in range(KC)]
                mm(S, p[0:M, :], pairs, r=[xtok, tw], w=[tp])
                evac(tti, p, tp)

        def proj_tm(xT, xtok, wt, tw, ncol, KC, psr, evac):
            for tc in range(16):
                p, tp = psr.next()
                pairs = [(xT[:, kc, tc * 128:(tc + 1) * 128], wt[:, kc, 0:ncol]) for kc in range(KC)]
                mm(S, p[:, 0:ncol], pairs, r=[xtok, tw], w=[tp])
                evac(tc, p, tp)

        def phase_norm(src, gvec, xnT, xtok):
            with ExitStack() as k:
                Gt = sb(k, [128, D]); tG = Tok()
                xin = Ring([sb(k, [128, D]) for _ in range(3)])
                xs = Ring([sb(k, [128, D], BF16) for _ in range(3)])
                junk = sb(k, [128, D], BF16); tj = Tok()
                st = Ring([sb(k, [128, 4]) for _ in range(3)])
                ptr = Ring([pst(k, [128, 1024], BF16) for _ in range(4)])
                dma("sp", Gt[:], gvec.partition_broadcast(128), w=[tG])
                def stage_a(tc):
                    xi, txi = xin.next()
                    xo, txo = xs.next()
                    sv, tsv = st.next()
                    dma("sp", xi[:], src[tc * 128:(tc + 1) * 128, :], w=[txi])
                    act(junk[:], xi[:], AF.Square, [txi], [tj, tsv], accum_out=sv[:, 0:1])
                    act(sv[:, 1:2], sv[:, 0:1], AF.Sqrt, [tsv, t_eps], [tsv], scale=1.0 / D, bias=epst[:])
                    recip(sv[:, 2:3], sv[:, 1:2], [tsv], [tsv])
                    stt(xo[:], xi[:], sv[:, 2:3], Gt[:], ALU.mult, ALU.mult, [txi, tsv, tG], [txo])
                    return xo, txo

                def stage_b(tc, xo, txo):
                    for half in range(2):
                        p, tp = ptr.next()

                        def fn(e, p=p, xo=xo, half=half):
                            ins = None
                            for j in range(8):
                                c = (half * 8 + j) * 128
                                ins = e.transpose(out=p[:, j * 128:(j + 1) * 128], in_=xo[:, c:c + 128], identity=ident[:])
                            return ins
                        S.op("pe", fn, r=[txo, t_ident], w=[tp])
                        act(xnT[:, half * 8:(half + 1) * 8, tc * 128:(tc + 1) * 128],
                            p[:].rearrange("p (k t) -> p k t", k=8), AF.Copy, [tp], [xtok])
                cur = stage_a(0)
                for tc in range(16):
                    nxt = stage_a(tc + 1) if tc + 1 < 16 else None
                    stage_b(tc, *cur)
                    cur = nxt
                S.flush()

        def rms_part(k_sq, o_ap, ncol, psr, r_o, rs_t, t_rs):
            sq, tsq = k_sq
            act(sq[:, 0:ncol], o_ap, AF.Square, r_o, [tsq])
            p, tp = psr.next()
            mm(S, p[:, 0:ncol], [(ones_bf[:], sq[:, 0:ncol])], r=[tsq, t_ones], w=[tp])
            act(rs_t[:, 0:ncol], p[:, 0:ncol], AF.Sqrt, [tp, t_eps], [t_rs], scale=1.0 / 128, bias=epst[:])
            recip(rs_t[:, 0:ncol], rs_t[:, 0:ncol], [t_rs], [t_rs])

        def layer(l, src_rows, dst_rows):
            win = w_in[l]
            LK = ExitStack()
            with LK:
                xnT = sb(LK, [128, 16, T], BF16); xtok = Tok()
                phase_norm(src_rows, norm_mix_g[l], xnT, xtok)
                if STOP >= 2: mixer_a(l, win, xnT, xtok)
                if STOP >= 3: mixer_b(l, win, xnT, xtok)
                if STOP >= 4: mixer_c(l, win, xnT, xtok)
                if STOP >= 5: mixer_d(l, win, xnT, xtok)
                if STOP >= 6: phase_gate(l, xnT, xtok)
            if STOP >= 7: phase_wout(l, src_rows)
            if STOP >= 8:
                with ExitStack() as k2:
                    hnT = sb(k2, [128, 16, T], BF16); htok = Tok()
                    phase_norm(xa_d, norm_ffn_g[l], hnT, htok)
                    phase_ffn_up(l, hnT, htok)
            if STOP >= 9: phase_ffn_down(l, dst_rows)

        def mixer_a(l, win, xnT, xtok):
            with ExitStack() as k:
                wr = Ring([sb(k, [128, 16, 512], BF16) for _ in range(2)])
                uaT = sb(k, [128, 4, T]); t_ua = Tok()
                yaT = sb(k, [128, 4, T], BF16); t_ya = Tok()
                WsT = sb(k, [128, 4, 128], BF16); t_ws = Tok()
                Wsf = sb(k, [128, 4, 128]); t_wsf = Tok()
                BS = sb(k, [128, 512]); t_bs = Tok()
                Gl = sb(k, [128, 512]); t_gl = Tok()
                Bl = sb(k, [128, 512]); t_bl = Tok()
                vg = Ring([sb(k, [128, 512]) for _ in range(2)])
                vc = Ring([sb(k, [128, 512]) for _ in range(2)])
                vj = sb(k, [128, 512], BF16); t_vj = Tok()
                vn = Ring([sb(k, [128, 512], BF16) for _ in range(4)])
                sv_r = Ring([sb(k, [128, 8]) for _ in range(2)])
                mx = Ring([sb(k, [128, 512]) for _ in range(2)])
                psr = Ring([pst(k, [128, 512]) for _ in range(6)])
                dma("sp", Wsf[:], gmlp_w_sT[l].rearrange("g s t -> s g t"), w=[t_wsf])
                memset("pool", Wsf[64:128, :, 0:64], 0.0, [t_wsf])
                cp("pool", WsT[:], Wsf[:], [t_wsf], [t_ws])
                dma("sp", BS[:], gmlp_b_s[l].partition_broadcast(128), w=[t_bs])
                dma("sp", Gl[:], gmlp_ln_g[l].partition_broadcast(128), w=[t_gl])
                dma("sp", Bl[:], gmlp_ln_b[l].partition_broadcast(128), w=[t_bl])
                wt, tw = load_w(wr, win, A0, 512)
                for oc in range(4):
                    def ev(tti, p, tp, oc=oc):
                        act(uaT[:, oc, tti * 512:(tti + 1) * 512], p[:], AF.Gelu_apprx_tanh, [tp], [t_ua])
                    proj_fm(xnT, xtok, wt, tw, oc * 128, 128, 16, psr, ev)
                wt2, tw2 = load_w(wr, win, A0 + 512, 512)

                def ev2(tc, p, tp):
                    g_, tg_ = vg.next()
                    c_, tc_ = vc.next()
                    n_, tn_ = vn.next()
                    sv, tsv = sv_r.next()
                    act(g_[:], p[:], AF.Gelu_apprx_tanh, [tp], [tg_, tsv], accum_out=sv[:, 0:1])
                    ts("dve", sv[:, 1:2], sv[:, 0:1], 1.0 / 512, None, ALU.mult, None, [tsv], [tsv])
                    ts("dve", c_[:], g_[:], sv[:, 1:2], None, ALU.subtract, None, [tg_, tsv], [tc_])
                    act(vj[:], c_[:], AF.Square, [tc_], [t_vj, tsv], accum_out=sv[:, 2:3])
                    act(sv[:, 3:4], sv[:, 2:3], AF.Sqrt, [tsv, t_eps], [tsv], scale=1.0 / 512, bias=epst[:])
                    recip(sv[:, 4:5], sv[:, 3:4], [tsv], [tsv])
                    stt(c_[:], c_[:], sv[:, 4:5], Gl[:], ALU.mult, ALU.mult, [tc_, tsv, t_gl], [tc_])
                    tt("dve", n_[:], c_[:], Bl[:], ALU.add, [tc_, t_bl], [tn_])

                    def stage_b(tc=tc, n_=n_, tn_=tn_):
                        m_, tm_ = mx.next()
                        p2, tp2 = psr.next()

                        def fn(e, p2=p2, n_=n_):
                            ins = None
                            for g in range(4):
                                ins = e.matmul(p2[:, g * 128:(g + 1) * 128], n_[:, g * 128:(g + 1) * 128], WsT[:, g, :], start=True, stop=True)
                            return ins
                        S.op("pe", fn, r=[tn_, t_ws], w=[tp2])
                        tt("dve", m_[:], p2[:], BS[:], ALU.add, [tp2, t_bs], [tm_])
                        tt("dve", yaT[:, :, tc * 128:(tc + 1) * 128], m_[:].rearrange("p (g t) -> p g t", g=4),
                           uaT[:, :, tc * 128:(tc + 1) * 128], ALU.mult, [tm_, t_ua], [t_ya])
                    pendA.append(stage_b)
                    if len(pendA) > 2:
                        pendA.pop(0)()
                pendA = []
                proj_tm(xnT, xtok, wt2, tw2, 512, 16, psr, ev2)
                while pendA:
                    pendA.pop(0)()
                dma("sp", yT_d[0:512, :].rearrange("(g p) t -> p g t", p=128), yaT[:], r=[t_ya])
                S.flush()

        def mixer_b(l, win, xnT, xtok):
            with ExitStack() as k:
                wr = Ring([sb(k, [128, 16, 128], BF16) for _ in range(6)])
                wz = sb(k, [128, 16, 8], BF16); t_wz = Tok()
                psr = Ring([pst(k, [128, 512]) for _ in range(4)])
                k1 = k
                t_exd = [[Tok() for _ in range(6)] for _ in range(2)]
                if True:
                    cn = sb(k1, [8, T]); t_cn = Tok()
                    ex = sb(k1, [8, T]); t_ex = Tok()
                    rr_ = sb(k1, [8, T]); t_rr = Tok()
                    onesf = sb(k1, [8, T]); t_of = Tok()
                    nbf = sb(k1, [8, 2]); t_nbf = Tok()
                    cbs = [sb(k1, [8, T], BF16) for _ in range(3)]; t_cb = [Tok() for _ in range(3)]
                    nbs = [sb(k1, [8, T], BF16) for _ in range(3)]; t_nb = [Tok() for _ in range(3)]
                    onesb = sb(k1, [8, T], BF16); t_ob = Tok()
                    dma("pool", wz[:], win[:, B0 + 1536:B0 + 1544].rearrange("(kc p) n -> p kc n", p=128), w=[t_wz], ndesc=128)
                    dma("sp", nbf[:, 0:1], fox_b_f[l].rearrange("(h o) -> h o", o=1), w=[t_nbf])
                    ts("dve", nbf[:, 1:2], nbf[:, 0:1], -1.0, None, ALU.mult, None, [t_nbf], [t_nbf])
                    memset("pool", onesf[:], 1.0, [t_of])
                    memset("pool", onesb[:], 1.0, [t_ob])
                    for tti in range(4):
                        p, tp = psr.next()
                        pairs = [(wz[:, kc, :], xnT[:, kc, tti * 512:(tti + 1) * 512]) for kc in range(16)]
                        mm(S, p[0:8, :], pairs, r=[xtok, t_wz], w=[tp])
                        act(ex[:, tti * 512:(tti + 1) * 512], p[0:8, :], AF.Exp, [tp, t_nbf], [t_ex], scale=-1.0, bias=nbf[:, 1:2])
                    act(ex[:], ex[:], AF.Ln, [t_ex], [t_ex], bias=1.0)
                    S.op("dve", lambda e: e.tensor_tensor_scan(out=cn[:], data0=onesf[:], data1=ex[:], initial=0.0, op0=ALU.mult, op1=ALU.add),
                         r=[t_ex, t_of], w=[t_cn])
                    cp("dve", cbs[0][:], cn[:], [t_cn], [t_cb[0]])
                    tt("dve", rr_[:], cn[:], cbs[0][:], ALU.subtract, [t_cn, t_cb[0]], [t_rr])
                    cp("dve", cbs[1][:], rr_[:], [t_rr], [t_cb[1]])
                    tt("dve", rr_[:], rr_[:], cbs[1][:], ALU.subtract, [t_rr, t_cb[1]], [t_rr])
                    cp("dve", cbs[2][:], rr_[:], [t_rr], [t_cb[2]])
                    for j in range(3):
                        ts("dve", nbs[j][:], cbs[j][:], -1.0, None, ALU.mult, None, [t_cb[j]], [t_nb[j]])
                        dma("sp", ex_d[1, :, 3 + j, :], cbs[j][:], r=[t_cb[j]], w=[t_exd[1][3 + j]])
                        dma("sp", ex_d[0, :, j, :], nbs[j][:], r=[t_nb[j]], w=[t_exd[0][j]])
                        dma("sp", ex_d[1, :, j, :], onesb[:], r=[t_ob], w=[t_exd[1][j]])
                        dma("sp", ex_d[0, :, 3 + j, :], onesb[:], r=[t_ob], w=[t_exd[0][3 + j]])
                qh = [sb(k, [128, T], BF16) for _ in range(2)]; t_qh = [Tok(), Tok()]; t_qx = [Tok(), Tok()]; t_kx = [Tok(), Tok()]
                kh = [sb(k, [128, T], BF16) for _ in range(2)]; t_kh = [Tok(), Tok()]
                vaug = sb(k, [128, 16, 2, 128], BF16); t_v = Tok()
                ybT = Ring([sb(k, [128, T], BF16) for _ in range(2)])
                PT = Ring([sb(k, [128, 512], BF16) for _ in range(4)])
                rz = Ring([sb(k, [64, 512]) for _ in range(2)])
                pso = Ring([pst(k, [128, 512]) for _ in range(4)])
                for hl_ in range(2):
                    memset("pool", vaug[:, :, hl_, 64:128], 1.0, [t_v])
                def ldb(hp_):
                    return (load_w(wr, win, B0 + hp_ * 128, 128), load_w(wr, win, B0 + 512 + hp_ * 128, 128),
                            load_w(wr, win, B0 + 1024 + hp_ * 128, 128))
                nxtw = ldb(0)
                for hp in range(4):
                    yb, t_yb = ybT.next()
                    for hl in range(2):
                        h = hp * 2 + hl
                        dma("sp", qh[hl][64:70, :], ex_d[0, h], r=t_exd[0], w=[t_qx[hl]])
                        dma("sp", kh[hl][64:70, :], ex_d[1, h], r=t_exd[1], w=[t_kx[hl]])
                    (wq, twq), (wk, twk), (wv, twv) = nxtw
                    if hp + 1 < 4:
                        nxtw = ldb(hp + 1)

                    def evq(tti, p, tp):
                        for hl in range(2):
                            act(qh[hl][0:64, tti * 512:(tti + 1) * 512], p[hl * 64:(hl + 1) * 64, :], AF.Copy, [tp], [t_qh[hl]], scale=0.125)

                    def evk(tti, p, tp):
                        for hl in range(2):
                            cp("dve", kh[hl][0:64, tti * 512:(tti + 1) * 512], p[hl * 64:(hl + 1) * 64, :], [tp], [t_kh[hl]])

                    def evv(tc, p, tp):
                        act(vaug[:, tc, :, 0:64], p[:, 0:128].rearrange("p (h d) -> p h d", h=2), AF.Copy, [tp], [t_v])
                    proj_fm(xnT, xtok, wq, twq, 0, 128, 16, psr, evq)
                    proj_fm(xnT, xtok, wk, twk, 0, 128, 16, psr, evk)
                    proj_tm(xnT, xtok, wv, twv, 128, 16, psr, evv)
                    for hl in range(2):
                        for i in range(4):
                            po, tpo = pso.next()
                            nj = 4 * i + 4

                            def s_stage(j, i=i, hl=hl):
                                t0 = max(i * 512, j * 128)
                                ncol = (i + 1) * 512 - t0
                                c0 = t0 - i * 512
                                p, tp = psr.next()
                                mm(S, p[:, 0:ncol], [(kh[hl][0:70, j * 128:(j + 1) * 128], qh[hl][0:70, t0:t0 + ncol])],
                                   r=[t_kh[hl], t_qh[hl], t_kx[hl], t_qx[hl]], w=[tp])
                                pt_, tpt = PT.next()
                                act(pt_[:, 0:ncol], p[:, 0:ncol], AF.Exp, [tp], [tpt])
                                if j >= 4 * i:
                                    S.op("pool", lambda e, pt_=pt_: e.affine_select(out=pt_[:, 0:128], in_=pt_[:, 0:128], pattern=[[1, 128]],
                                                                                   compare_op=ALU.is_ge, fill=0.0, base=0, channel_multiplier=-1),
                                         r=[tpt], w=[tpt])
                                return (j, pt_, tpt, c0, ncol)

                            def pv_stage(st_, hl=hl, po=po, tpo=tpo, nj=nj):
                                j, pt_, tpt, c0, ncol = st_
                                mm(S, po[:, c0:c0 + ncol], [(vaug[:, j, hl, :], pt_[:, 0:ncol])],
                                   r=[t_v, tpt], w=[tpo], start=(j == 0), stop=(j == nj - 1))
                            inflight = []
                            for j in range(nj):
                                inflight.append(s_stage(j))
                                if len(inflight) > 2:
                                    pv_stage(inflight.pop(0))
                            while inflight:
                                pv_stage(inflight.pop(0))
                            rz_, trz = rz.next()
                            act(rz_[:], po[64:128, :], AF.Copy, [tpo], [trz])
                            recip(rz_[:], rz_[:], [trz], [trz])
                            tt("dve", yb[hl * 64:(hl + 1) * 64, i * 512:(i + 1) * 512], po[0:64, :], rz_[:], ALU.mult, [tpo, trz], [t_yb])
                    dma("sp", yT_d[512 + hp * 128:512 + (hp + 1) * 128, :], yb[:], r=[t_yb])
                S.flush()

        def mixer_c(l, win, xnT, xtok):
            with ExitStack() as k:
                wr = Ring([sb(k, [128, 16, 128], BF16) for _ in range(8)])
                psr = Ring([pst(k, [128, 512]) for _ in range(2)])
                ptr = Ring([pst(k, [128, 1024], BF16) for _ in range(1)])
                pmy = pst(k, [128, 512])
                psA = Ring([pst(k, [128, 512])[:, 0:128]])
                psU = Ring([pst(k, [128, 512])[:, 0:128] for _ in range(2)])
                psO = Ring([pmy[:, 0:128], pst(k, [128, 512])[:, 0:128]])
                t_pmy = psO.items[0][1]
                lbt = sb(k, [128, 16]); t_lb = Tok()
                gn = sb(k, [128, 1]); t_gn = Tok()
                onesf = sb(k, [128, T]); t_of = Tok()
                qf = sb(k, [128, T]); t_qf = Tok()
                bA = sb(k, [128, T]); t_A = Tok()
                bB = sb(k, [128, T]); t_B = Tok()
                bG = sb(k, [128, T]); t_G = Tok()
                dd = sb(k, [128, 32]); t_dd = Tok()
                qtil = sb(k, [128, T], BF16); t_qt = Tok()
                ktil = sb(k, [128, T], BF16); t_kt = Tok()
                ktok = [sb(k, [128, 16, 128], BF16) for _ in range(2)]; t_ktok = [Tok(), Tok()]
                vtok = sb(k, [128, 16, 128], BF16); t_v = Tok()
                gate = sb(k, [128, T], BF16); t_gate = Tok()
                oT = sb(k, [128, T]); t_oT = Tok()
                AT = Ring([sb(k, [128, 2, 64], BF16) for _ in range(2)])
                Tst = sb(k, [128, 128]); t_T = Tok()
                Sb = Ring([sb(k, [128, 128], BF16) for _ in range(2)])
                sq = (sb(k, [128, 512], BF16), Tok())
                rs_t = sb(k, [128, 512]); t_rs = Tok()
                t1 = sb(k, [128, 512]); t_t1 = Tok()
                ycT = Ring([sb(k, [128, T], BF16) for _ in range(2)])
                lbin = sb(k, [8, 128]); t_lbin = Tok()
                identf = sb(k, [128, 128]); t_idf = Tok()
                rm = sb(k, [128, 2]); t_rm = Tok()
                tri2 = sb(k, [128, 2, 64]); t_tri = Tok()
                dma("sp", identf[:], c_identf, w=[t_idf])
                dma("sp", rm[:], c_rm, w=[t_rm])
                dma("sp", tri2[:], c_tri2, w=[t_tri])
                dma("sp", lbin[:], hgrn_lb.rearrange("l (c p) -> (l c) p", p=128), w=[t_lbin])
                S.op("pe", lambda e: e.transpose(out=pmy[:, 256:264], in_=lbin[:], identity=identf[0:8, 0:8]), r=[t_lbin, t_idf], w=[t_pmy])
                cp("dve", lbt[:, 0:8], pmy[:, 256:264], [t_pmy], [t_lb])
                dma("sp", gn[:], hgrn_norm_g[l].rearrange("(p o) -> p o", o=1), w=[t_gn])
                memset("pool", onesf[:], 1.0, [t_of])
                if l == 0:
                    memset("dve", lbt[:, 8:12], 0.0, [t_lb])
                else:
                    tt("dve", lbt[:, 8:12], lbt[:, 4:8], lbt[:, 0:4], ALU.subtract, [t_lb], [t_lb])
                    act(lbt[:, 8:12], lbt[:, 8:12], AF.Sigmoid, [t_lb], [t_lb])
                ts("dve", lbt[:, 12:16], lbt[:, 8:12], -1.0, 1.0, ALU.mult, ALU.add, [t_lb], [t_lb])
                def ldc(h_):
                    return (load_w(wr, win, C0 + h_ * 128, 128), load_w(wr, win, C0 + 512 + h_ * 128, 128),
                            load_w(wr, win, C0 + 1024 + h_ * 128, 128), load_w(wr, win, C0 + 1536 + h_ * 128, 128))
                nxtw = ldc(0)
                for h in range(4):
                    yc, t_yc = ycT.next()
                    (wq, twq), (wzz, twz), (wi, twi), (wg, twg) = nxtw
                    if h + 1 < 4:
                        nxtw = ldc(h + 1)

                    def evq(tti, p, tp):
                        act(qf[:, tti * 512:(tti + 1) * 512], p[:], AF.Copy, [tp], [t_qf], scale=128 ** -0.5)

                    def evz(tti, p, tp, h=h):
                        act(bA[:, tti * 512:(tti + 1) * 512], p[:], AF.Sigmoid, [tp], [t_A])

                    def evv(tc, p, tp):
                        cp("dve", vtok[:, tc, :], p[:, 0:128], [tp], [t_v])

                    def evg(tti, p, tp):
                        act(gate[:, tti * 512:(tti + 1) * 512], p[:], AF.Sigmoid, [tp], [t_gate])
                    proj_fm(xnT, xtok, wq, twq, 0, 128, 16, psr, evq)
                    proj_fm(xnT, xtok, wzz, twz, 0, 128, 16, psr, evz)
                    proj_tm(xnT, xtok, wi, twi, 128, 16, psr, evv)
                    proj_fm(xnT, xtok, wg, twg, 0, 128, 16, psr, evg)
                    ts("dve", bA[:], bA[:], lbt[:, 12 + h:13 + h], lbt[:, 8 + h:9 + h], ALU.mult, ALU.add, [t_A, t_lb], [t_A])
                    act(bB[:], bA[:], AF.Ln, [t_A], [t_B])
                    ts("dve", bA[:], bA[:], -1.0, 1.0, ALU.mult, ALU.add, [t_A], [t_A])
                    S.op("dve", lambda e: e.tensor_tensor_scan(out=bG[:], data0=onesf[:], data1=bB[:], initial=0.0, op0=ALU.mult, op1=ALU.add),
                         r=[t_B, t_of], w=[t_G])
                    G3 = bG[:].rearrange("p (c t) -> p c t", t=64)
                    E3 = bB[:].rearrange("p (c t) -> p c t", t=64)
                    tt("dve", E3, G3, G3[:, :, 31:32].broadcast_to([128, 32, 64]), ALU.subtract, [t_G], [t_B])
                    tt("dve", dd[:, 0:31], G3[:, 1:32, 31], G3[:, 0:31, 31], ALU.subtract, [t_G], [t_dd])
                    act(dd[:, 0:31], dd[:, 0:31], AF.Exp, [t_dd], [t_dd])
                    act(bG[:], bB[:], AF.Exp, [t_B], [t_G])
                    act(bB[:], bB[:], AF.Exp, [t_B], [t_B], scale=-1.0)
                    tt("dve", qtil[:], qf[:], bG[:], ALU.mult, [t_qf, t_G], [t_qt])
                    tt("dve", ktil[:], bA[:], bB[:], ALU.mult, [t_A, t_B], [t_kt])
                    for half in range(2):
                        p, tp = ptr.next()

                        def fn(e, p=p, half=half):
                            ins = None
                            for j in range(8):
                                c = (half * 8 + j) * 128
                                ins = e.transpose(out=p[:, j * 128:(j + 1) * 128], in_=ktil[:, c:c + 128], identity=ident[:])
                            return ins
                        S.op("pe", fn, r=[t_kt, t_ident], w=[tp])
                        for hf in range(2):
                            act(ktok[hf][:, half * 8:(half + 1) * 8, :], p[:].rearrange("p (k t) -> p k t", k=8), AF.Copy, [tp, t_rm], [t_ktok[hf]],
                                scale=rm[:, hf:hf + 1])
                    sbc = None
                    for tc in range(16):
                        pa, tpa = psA.next()
                        at, tat = AT.next()
                        for hf in range(2):
                            c = 2 * tc + hf
                            mm(S, pa[:, hf * 64:(hf + 1) * 64], [(ktil[:, tc * 128:(tc + 1) * 128], qtil[:, c * 64:(c + 1) * 64])],
                               r=[t_kt, t_qt], w=[tpa])
                        tt("dve", at[:], pa[:].rearrange("p (h t) -> p h t", h=2), tri2[:], ALU.mult, [tpa, t_tri], [tat])
                        po, tpo = psO.next()
                        for hf in range(2):
                            c = 2 * tc + hf
                            pu, tpu = psU.next()
                            mm(S, pu[:], [(ktok[hf][:, tc, :], vtok[:, tc, :])], r=[t_ktok[hf], t_v], w=[tpu])
                            mm(S, po[:, hf * 64:(hf + 1) * 64], [(vtok[:, tc, :], at[:, hf, :])], r=[t_v, tat], w=[tpo],
                               start=True, stop=(c == 0))
                            if c > 0:
                                mm(S, po[:, hf * 64:(hf + 1) * 64], [(sbc[0][:], qtil[:, c * 64:(c + 1) * 64])], r=[sbc[1], t_qt], w=[tpo],
                                   start=False, stop=True)
                            if c == 0:
                                cp("dve", Tst[:], pu[:], [tpu], [t_T])
                            else:
                                stt(Tst[:], Tst[:], dd[:, c - 1:c], pu[:], ALU.mult, ALU.add, [t_T, t_dd, tpu], [t_T])
                            if c < 31:
                                sbc = Sb.next()
                                ts("dve", sbc[0][:], Tst[:], dd[:, c:c + 1], None, ALU.mult, None, [t_T, t_dd], [sbc[1]])
                        act(oT[:, tc * 128:(tc + 1) * 128], po[:], AF.Copy, [tpo], [t_oT])
                    for tti in range(4):
                        cs = slice(tti * 512, (tti + 1) * 512)
                        rms_part(sq, oT[:, cs], 512, psr, [t_oT], rs_t, t_rs)
                        stt(t1[:], oT[:, cs], gn[:, 0:1], rs_t[:], ALU.mult, ALU.mult, [t_oT, t_gn, t_rs], [t_t1])
                        tt("dve", yc[:, cs], t1[:], gate[:, cs], ALU.mult, [t_t1, t_gate], [t_yc])
                    dma("sp", yT_d[1024 + h * 128:1024 + (h + 1) * 128, :], yc[:], r=[t_yc])
                S.flush()

        def mixer_d(l, win, xnT, xtok):
            lam_init = 0.8 - 0.6 * float(np.exp(-0.3 * l))
            with ExitStack() as k:
                wr = Ring([sb(k, [128, 16, 128], BF16) for _ in range(6)])
                psr = Ring([pst(k, [128, 512]) for _ in range(4)])
                pacc = [pst(k, [128, 512]) for _ in range(4)]; t_acc = [Tok() for _ in range(4)]
                cosT = sb(k, [128, T]); t_cos = Tok()
                sinT = sb(k, [128, T]); t_sin = Tok()
                mk = sb(k, [128, 2]); t_mk = Tok()
                lamt = sb(k, [128, 256]); t_lam = Tok()
                lw_ = sb(k, [128, 128]); t_lw = Tok()
                ls = sb(k, [128, 8]); t_ls = Tok()
                gD = sb(k, [128, 1]); t_gD = Tok()
                qr = sb(k, [128, T], BF16); t_qr = Tok()
                k1p = sb(k, [128, T], BF16); t_k1 = Tok()
                k2p = sb(k, [128, T], BF16); t_k2 = Tok()
                vtok = sb(k, [128, 16, 128], BF16); t_v = Tok()
                tA = Ring([sb(k, [128, 512]) for _ in range(2)])
                tB = Ring([sb(k, [128, 512]) for _ in range(2)])
                PT = Ring([sb(k, [128, 512], BF16) for _ in range(6)])
                r1r = Ring([sb(k, [128, 512]) for _ in range(2)])
                r2r = Ring([sb(k, [128, 512]) for _ in range(2)])
                oD = sb(k, [128, 512]); t_oD = Tok()
                sq = (sb(k, [128, 512], BF16), Tok())
                rs_t = sb(k, [128, 512]); t_rs = Tok()
                ydT = Ring([sb(k, [128, T], BF16) for _ in range(2)])
                dma("sp", cosT[:], c_cos, w=[t_cos])
                dma("sp", sinT[:], c_sinA, w=[t_sin])
                dma("sp", mk[:], c_mk, w=[t_mk])
                dma("sp", lamt[:], diff_lambda[l].partition_broadcast(128), w=[t_lam])
                dma("sp", gD[:], diff_norm_g[l].rearrange("(p o) -> p o", o=1), w=[t_gD])
                ts("dve", gD[:], gD[:], 1.0 - lam_init, None, ALU.mult, None, [t_gD], [t_gD])
                tt("dve", lw_[:, 0:64], lamt[:, 0:64], lamt[:, 64:128], ALU.mult, [t_lam], [t_lw])
                tt("dve", lw_[:, 64:128], lamt[:, 128:192], lamt[:, 192:256], ALU.mult, [t_lam], [t_lw])
                S.op("dve", lambda e: e.reduce_sum(out=ls[:, 0:1], in_=lw_[:, 0:64], axis=AX.X), r=[t_lw], w=[t_ls])
                S.op("dve", lambda e: e.reduce_sum(out=ls[:, 1:2], in_=lw_[:, 64:128], axis=AX.X), r=[t_lw], w=[t_ls])
                act(ls[:, 2:4], ls[:, 0:2], AF.Exp, [t_ls], [t_ls])
                tt("dve", ls[:, 4:5], ls[:, 3:4], ls[:, 2:3], ALU.subtract, [t_ls], [t_ls])
                ts("dve", ls[:, 5:6], ls[:, 4:5], -lam_init, None, ALU.add, None, [t_ls], [t_ls])
                def ldd(h_):
                    return (load_w(wr, win, D0 + h_ * 128, 128), load_w(wr, win, D0 + 512 + h_ * 128, 128),
                            load_w(wr, win, D0 + 1024 + h_ * 128, 128))
                nxtw = ldd(0)
                for h in range(4):
                    yd, t_yd = ydT.next()
                    (wq, twq), (wk, twk), (wv, twv) = nxtw
                    if h + 1 < 4:
                        nxtw = ldd(h + 1)

                    def rope(tti, p, tp, dst):
                        cs = slice(tti * 512, (tti + 1) * 512)
                        a_, ta_ = tA.next()
                        b_, tb_ = tB.next()
                        tt("dve", a_[:], p[:], cosT[:, cs], ALU.mult, [tp, t_cos], [ta_])
                        tt("dve", b_[0:64, :], p[64:128, :], sinT[64:128, cs], ALU.mult, [tp, t_sin], [tb_])
                        tt("dve", b_[64:128, :], p[0:64, :], sinT[0:64, cs], ALU.mult, [tp, t_sin], [tb_])
                        return a_, ta_, b_, tb_, cs

                    def evq(tti, p, tp):
                        a_, ta_, b_, tb_, cs = rope(tti, p, tp, None)
                        tt("dve", qr[:, cs], a_[:], b_[:], ALU.add, [ta_, tb_], [t_qr])

                    def evk(tti, p, tp):
                        a_, ta_, b_, tb_, cs = rope(tti, p, tp, None)
                        tt("dve", a_[:], a_[:], b_[:], ALU.add, [ta_, tb_], [ta_])
                        act(k1p[:, cs], a_[:], AF.Copy, [ta_, t_mk], [t_k1], scale=mk[:, 0:1])
                        act(k2p[:, cs], a_[:], AF.Copy, [ta_, t_mk], [t_k2], scale=mk[:, 1:2])

                    def evv(tc, p, tp):
                        act(vtok[:, tc, :], p[:, 0:128], AF.Copy, [tp], [t_v])
                    proj_fm(xnT, xtok, wq, twq, 0, 128, 16, psr, evq)
                    proj_fm(xnT, xtok, wk, twk, 0, 128, 16, psr, evk)
                    proj_tm(xnT, xtok, wv, twv, 128, 16, psr, evv)
                    pend = None
                    for i in range(4):
                        nj = 4 * i + 4

                        def s_stage(j, i=i):
                            t0 = max(i * 512, j * 128)
                            ncol = (i + 1) * 512 - t0
                            c0 = t0 - i * 512
                            res_ = []
                            for m, (kp, tkp) in enumerate(((k1p, t_k1), (k2p, t_k2))):
                                p, tp = psr.next()
                                mm(S, p[:, 0:ncol], [(kp[:, j * 128:(j + 1) * 128], qr[:, t0:t0 + ncol])], r=[tkp, t_qr], w=[tp])
                                pt_, tpt = PT.next()
                                act(pt_[:, 0:ncol], p[:, 0:ncol], AF.Exp, [tp], [tpt], scale=0.125)
                                if j >= 4 * i:
                                    memset("pool", pt_[64:128, 0:64], 0.0, [tpt])
                                res_.append((pt_, tpt))
                            return (j, res_, c0, ncol)

                        def pv_stage(st_, nj=nj):
                            j, res_, c0, ncol = st_
                            for m, (pt_, tpt) in enumerate(res_):
                                mm(S, pacc[2 * m][:, c0:c0 + ncol], [(vtok[:, j, :], pt_[:, 0:ncol])], r=[t_v, tpt], w=[t_acc[2 * m]],
                                   start=(j == 0), stop=(j == nj - 1))
                                mm(S, pacc[2 * m + 1][:, c0:c0 + ncol], [(ones_bf[:], pt_[:, 0:ncol])], r=[t_ones, tpt], w=[t_acc[2 * m + 1]],
                                   start=(j == 0), stop=(j == nj - 1))
                        def fin2(i_, r1, t_r1, r2, t_r2, yd=yd, t_yd=t_yd):
                            cs = slice(i_ * 512, (i_ + 1) * 512)
                            stt(oD[:], r2[:], ls[:, 5:6], r1[:], ALU.mult, ALU.add, [t_r1, t_r2, t_ls], [t_oD])
                            rms_part(sq, oD[:], 512, psr, [t_oD], rs_t, t_rs)
                            stt(yd[:, cs], oD[:], gD[:, 0:1], rs_t[:], ALU.mult, ALU.mult, [t_oD, t_gD, t_rs], [t_yd])
                        prev = None
                        for j in range(nj):
                            cur = s_stage(j)
                            if prev is not None:
                                pv_stage(prev)
                            prev = cur
                            if j == 1 and pend is not None:
                                fin2(*pend)
                                pend = None
                        pv_stage(prev)
                        r1, t_r1 = r1r.next()
                        r2, t_r2 = r2r.next()
                        recip(r1[:], pacc[1][:], [t_acc[1]], [t_r1])
                        tt("dve", r1[:], pacc[0][:], r1[:], ALU.mult, [t_acc[0], t_r1], [t_r1])
                        recip(r2[:], pacc[3][:], [t_acc[3]], [t_r2])
                        tt("dve", r2[:], pacc[2][:], r2[:], ALU.mult, [t_acc[2], t_r2], [t_r2])
                        pend = (i, r1, t_r1, r2, t_r2)
                    fin2(*pend)
                    pend = None
                    dma("sp", yT_d[1536 + h * 128:1536 + (h + 1) * 128, :], yd[:], r=[t_yd])
                S.flush()

        def phase_gate(l, xnT, xtok):
            with ExitStack() as k:
                yall = sb(k, [128, 16, T], BF16); t_y = [Tok() for _ in range(4)]
                wgr = Ring([sb(k, [128, 16, 128], BF16) for _ in range(3)])
                wbr = Ring([sb(k, [128, 4, 128], BF16) for _ in range(3)])
                bg = sb(k, [128, 4, 16]); t_bg = Tok()
                psg = Ring([pst(k, [128, 512]) for _ in range(4)])
                psb = Ring([pst(k, [128, 512]) for _ in range(4)])
                gt = Ring([sb(k, [128, 512]) for _ in range(3)])
                tmp = Ring([sb(k, [128, 512]) for _ in range(3)])
                accs = [sb(k, [128, 512]) for _ in range(4)]; t_accs = [Tok() for _ in range(4)]
                mo = Ring([sb(k, [128, T], BF16) for _ in range(2)])
                for q4 in range(4):
                    dma("sp", yall[:, q4 * 4:(q4 + 1) * 4, :], yT_d[q4 * 512:(q4 + 1) * 512, :].rearrange("(kc p) t -> p kc t", p=128), w=[t_y[q4]])
                bgin = sb(k, [64, 128]); t_bgin = Tok()
                identf = sb(k, [128, 128]); t_idf = Tok()
                dma("sp", identf[:], c_identf, w=[t_idf])
                dma("sp", bgin[:], b_gate[l].rearrange("n (oc p) -> (n oc) p", p=128), w=[t_bgin])
                p0, tp0 = psg.next()
                S.op("pe", lambda e: e.transpose(out=p0[:, 0:64], in_=bgin[:], identity=identf[0:64, 0:64]), r=[t_bgin, t_idf], w=[tp0])
                cp("dve", bg[:], p0[:, 0:64].rearrange("p (n oc) -> p n oc", n=4), [tp0], [t_bg])
                def ldg(i_):
                    oc_, n_ = divmod(i_, 4)
                    return load_w(wgr, w_gate[l, n_], oc_ * 128, 128), load_w(wbr, w_branch[l, n_], oc_ * 128, 128, KC=4)
                nxtw = ldg(0)
                for oc in range(16):
                    m_, tm_ = mo.next()
                    for n in range(4):
                        (wg, twg), (wb_, twb) = nxtw
                        if oc * 4 + n + 1 < 64:
                            nxtw = ldg(oc * 4 + n + 1)
                        for tti in range(4):
                            cs = slice(tti * 512, (tti + 1) * 512)
                            pg, tpg = psg.next()
                            pb, tpb = psb.next()
                            mm(S, pg[:], [(wg[:, kc, :], xnT[:, kc, cs]) for kc in range(16)], r=[xtok, twg], w=[tpg])
                            mm(S, pb[:], [(wb_[:, kc, :], yall[:, n * 4 + kc, cs]) for kc in range(4)], r=[t_y[n], twb], w=[tpb])
                            g_, tg_ = gt.next()
                            act(g_[:], pg[:], AF.Sigmoid, [tpg, t_bg], [tg_], bias=bg[:, n, oc:oc + 1])
                            if n == 0:
                                tt("dve", accs[tti][:], g_[:], pb[:], ALU.mult, [tg_, tpb], [t_accs[tti]])
                            else:
                                t_, tt_ = tmp.next()
                                tt("dve", t_[:], g_[:], pb[:], ALU.mult, [tg_, tpb], [tt_])
                                if n < 3:
                                    tt("pool", accs[tti][:], accs[tti][:], t_[:], ALU.add, [t_accs[tti], tt_], [t_accs[tti]])
                                else:
                                    tt("pool", m_[:, cs], accs[tti][:], t_[:], ALU.add, [t_accs[tti], tt_], [tm_])
                    dma("sp", mixT_d[oc * 128:(oc + 1) * 128, :], m_[:], r=[tm_])
                S.flush()

        def phase_wout(l, src_rows):
            with ExitStack() as k:
                mT = sb(k, [128, 16, T], BF16); t_m = [Tok() for _ in range(4)]
                wo = sb(k, [128, 16, D], BF16); t_wo = [Tok() for _ in range(4)]
                xr = Ring([sb(k, [128, D]) for _ in range(2)])
                xo = Ring([sb(k, [128, D]) for _ in range(2)])
                psr = Ring([pst(k, [128, 512]) for _ in range(4)])
                for q4 in range(4):
                    dma("sp", mT[:, q4 * 4:(q4 + 1) * 4, :], mixT_d[q4 * 512:(q4 + 1) * 512, :].rearrange("(kc p) t -> p kc t", p=128), w=[t_m[q4]])
                for ct in range(4):
                    dma("pool", wo[:, :, ct * 512:(ct + 1) * 512], w_out[l][:, ct * 512:(ct + 1) * 512].rearrange("(kc p) n -> p kc n", p=128), w=[t_wo[ct]], ndesc=128)
                for tc in range(16):
                    xi, txi = xr.next()
                    xo_, txo = xo.next()
                    dma("sp", xi[:], src_rows[tc * 128:(tc + 1) * 128, :], w=[txi])
                    for ct in range(4):
                        cs = slice(ct * 512, (ct + 1) * 512)
                        p, tp = psr.next()
                        mm(S, p[:], [(mT[:, kc, tc * 128:(tc + 1) * 128], wo[:, kc, cs]) for kc in range(16)], r=t_m + [t_wo[ct]], w=[tp])
                        tt("dve", xo_[:, cs], p[:], xi[:, cs], ALU.add, [tp, txi], [txo])
                    dma("sp", xa_d[tc * 128:(tc + 1) * 128, :], xo_[:], r=[txo])
                S.flush()

        def phase_ffn_up(l, hnT, htok):
            with ExitStack() as k:
                wr = Ring([sb(k, [128, 16, 128], BF16) for _ in range(4)])
                cwin = sb(k, [88, 4, 128]); t_cwin = Tok()
                cw = sb(k, [128, 4, 88]); t_cw = Tok()
                identf = sb(k, [128, 128]); t_idf = Tok()
                ha = Ring([sb(k, [128, T + 2]) for _ in range(2)])
                hg = Ring([sb(k, [128, T + 2]) for _ in range(2)])
                ca = Ring([sb(k, [128, T]) for _ in range(2)])
                cg = Ring([sb(k, [128, T]) for _ in range(2)])
                ao = Ring([sb(k, [128, T], BF16) for _ in range(2)])
                psa = Ring([pst(k, [128, 512]) for _ in range(4)])
                psg = Ring([pst(k, [128, 512]) for _ in range(4)])
                dma("sp", identf[:], c_identf, w=[t_idf])
                for kk in range(3):
                    dma("sp", cwin[:, kk, :], ffn_conv_w[l, kk].rearrange("(c p) -> c p", p=128), w=[t_cwin])
                dma("sp", cwin[:, 3, :], ffn_conv_b[l].rearrange("(c p) -> c p", p=128), w=[t_cwin])
                p0, tp0 = psa.next()

                def fnT(e):
                    ins = None
                    for kk in range(4):
                        ins = e.transpose(out=p0[:, kk * 88:(kk + 1) * 88], in_=cwin[:, kk, :], identity=identf[0:88, 0:88])
                    return ins
                S.op("pe", fnT, r=[t_cwin, t_idf], w=[tp0])
                cp("dve", cw[:], p0[:, 0:352].rearrange("p (k c) -> p k c", k=4), [tp0], [t_cw])
                for r_ in (ha, hg):
                    for (tb, ttk) in r_.items:
                        memset("pool", tb[:, 0:2], 0.0, [ttk])
                wup = ffn_w_up[l]
                def ldw(j):
                    return load_w(wr, wup, j * 128, 128), load_w(wr, wup, (44 + j) * 128, 128)
                nxtw = ldw(0)
                for j in range(44):
                    (wa, twa), (wg, twg) = nxtw
                    if j + 1 < 44:
                        nxtw = ldw(j + 1)
                    ha_, tha = ha.next()
                    hg_, thg = hg.next()
                    for tti in range(4):
                        cs = slice(tti * 512, (tti + 1) * 512)
                        pa, tpa = psa.next()
                        pg, tpg = psg.next()
                        mm(S, pa[:], [(wa[:, kc, :], hnT[:, kc, cs]) for kc in range(16)], r=[htok, twa], w=[tpa])
                        mm(S, pg[:], [(wg[:, kc, :], hnT[:, kc, cs]) for kc in range(16)], r=[htok, twg], w=[tpg])
                        act(ha_[:, 2 + tti * 512:2 + (tti + 1) * 512], pa[:], AF.Copy, [tpa], [tha])
                        act(hg_[:, 2 + tti * 512:2 + (tti + 1) * 512], pg[:], AF.Copy, [tpg], [thg])
                    ca_, tca = ca.next()
                    cg_, tcg = cg.next()
                    ao_, tao = ao.next()
                    for (hb, thb, c_, tc_, jj) in ((ha_, tha, ca_, tca, j), (hg_, thg, cg_, tcg, 44 + j)):
                        act(c_[:], hb[:, 2:T + 2], AF.Identity, [thb, t_cw], [tc_], scale=cw[:, 2, jj:jj + 1], bias=cw[:, 3, jj:jj + 1])
                        stt(c_[:], hb[:, 1:T + 1], cw[:, 1, jj:jj + 1], c_[:], ALU.mult, ALU.add, [thb, t_cw, tc_], [tc_])
                        stt(c_[:], hb[:, 0:T], cw[:, 0, jj:jj + 1], c_[:], ALU.mult, ALU.add, [thb, t_cw, tc_], [tc_])
                    act(ca_[:], ca_[:], AF.Gelu_apprx_tanh, [tca], [tca])
                    tt("pool", ao_[:], ca_[:], cg_[:], ALU.mult, [tca, tcg], [tao])
                    dma("sp", actT_d[j * 128:(j + 1) * 128, :], ao_[:], r=[tao])
                S.flush()

        def phase_ffn_down(l, dst_rows):
            with ExitStack() as k:
                aT = sb(k, [128, 44, 1024], BF16); t_a = [Tok() for _ in range(4)]
                wd = Ring([sb(k, [128, 44, 256], BF16) for _ in range(2)])
                xr = Ring([sb(k, [128, 256]) for _ in range(4)])
                xo = Ring([sb(k, [128, 256]) for _ in range(4)])
                psr = Ring([pst(k, [128, 512]) for _ in range(4)])
                wdn = ffn_w_down[l]
                for half in range(2):
                    for q4 in range(4):
                        dma("sp", aT[:, q4 * 11:(q4 + 1) * 11, :],
                            actT_d[q4 * 1408:(q4 + 1) * 1408, half * 1024:(half + 1) * 1024].rearrange("(kc p) t -> p kc t", p=128), w=[t_a[q4]])
                    for ct in range(8):
                        w_, tw_ = load_w(wd, wdn, ct * 256, 256, KC=44)
                        for tc in range(8):
                            r0 = half * 1024 + tc * 128
                            xi, txi = xr.next()
                            xo_, txo = xo.next()
                            dma("sp", xi[:], xa_d[r0:r0 + 128, ct * 256:(ct + 1) * 256], w=[txi])
                            p, tp = psr.next()
                            mm(S, p[:, 0:256], [(aT[:, kc, tc * 128:(tc + 1) * 128], w_[:, kc, :]) for kc in range(44)], r=t_a + [tw_], w=[tp])
                            tt("dve", xo_[:], p[:, 0:256], xi[:], ALU.add, [tp, txi], [txo])
                            dma("sp", dst_rows[r0:r0 + 128, ct * 256:(ct + 1) * 256], xo_[:], r=[txo])
                S.flush()

        def phase_final(src, dst):
            with ExitStack() as k:
                Gt = sb(k, [128, D]); tG = Tok()
                xin = Ring([sb(k, [128, D]) for _ in range(3)])
                xs = Ring([sb(k, [128, D]) for _ in range(3)])
                junk = sb(k, [128, D], BF16); tj = Tok()
                st = Ring([sb(k, [128, 4]) for _ in range(3)])
                dma("sp", Gt[:], norm_final_g.partition_broadcast(128), w=[tG])

                def fa(tc):
                    xi, txi = xin.next()
                    xo, txo = xs.next()
                    sv, tsv = st.next()
                    dma("sp", xi[:], src[tc * 128:(tc + 1) * 128, :], w=[txi])
                    act(junk[:], xi[:], AF.Square, [txi], [tj, tsv], accum_out=sv[:, 0:1])
                    act(sv[:, 1:2], sv[:, 0:1], AF.Sqrt, [tsv, t_eps], [tsv], scale=1.0 / D, bias=epst[:])
                    recip(sv[:, 2:3], sv[:, 1:2], [tsv], [tsv])
                    stt(xo[:], xi[:], sv[:, 2:3], Gt[:], ALU.mult, ALU.mult, [txi, tsv, tG], [txo])
                    return xo, txo
                cur = fa(0)
                for tc in range(16):
                    nxt = fa(tc + 1) if tc + 1 < 16 else None
                    dma("sp", dst[tc * 128:(tc + 1) * 128, :], cur[0][:], r=[cur[1]])
                    cur = nxt
                S.flush()

        scr = [xb_d, xc_d]
        for s in range(NS):
            src = x_in[s]
            for l in range(NLAY):
                dst = scr[l % 2]
                layer(l, src, dst)
                src = dst
            if STOP >= 10: phase_final(src, out[s])
    return nc


def _constants():
    ident = np.eye(128, dtype=np.float32)
    half = 32
    inv_freq = (10000.0 ** (-np.arange(half, dtype=np.float32) / half)).astype(np.float32)
    pos = np.arange(T, dtype=np.float32)
    ang = (pos[None, :] * inv_freq[:, None]).astype(np.float32)
    cos = np.cos(ang).astype(np.float32)
    sin = np.sin(ang).astype(np.float32)
    c_cos = np.tile(cos, (4, 1))
    c_sinA = np.concatenate([sin, sin, -sin, -sin], axis=0)
    tri = (np.arange(64)[None, :] >= (np.arange(128)[:, None] % 64)).astype(np.float32)
    g = (np.arange(128) // 32) % 2
    mk = np.stack([(g == 0), (g == 1)], axis=1).astype(np.float32)
    return {
        "c_ident": ident.astype(ml_dtypes.bfloat16),
        "c_identf": ident,
        "c_cos": np.ascontiguousarray(c_cos),
        "c_sinA": np.ascontiguousarray(c_sinA),
        "c_tri": tri,
        "c_mk": mk,
        "c_rm": np.stack([np.arange(128) < 64, np.arange(128) >= 64], axis=1).astype(np.float32),
        "c_tri2": np.ascontiguousarray(np.stack([tri * (np.arange(128)[:, None] < 64), tri * (np.arange(128)[:, None] >= 64)], axis=1).astype(np.float32)),
    }


def _perm_cols():
    r = np.arange(128)
    comp = (r // 32) % 2
    part = r // 64
    return comp * 64 + part * 32 + (r % 32)


def _prep_shared(inp):
    f = lambda a: np.ascontiguousarray(np.asarray(a, dtype=np.float32))
    w_in = f(inp["w_in"]).copy()
    pc = _perm_cols()
    for base in (D0, D0 + 512):
        for h in range(4):
            c = base + h * 128
            w_in[:, :, c:c + 128] = w_in[:, :, c + pc]
    m = {
        "norm_mix_g": f(inp["norm_mix_g"]), "w_in": w_in, "fox_b_f": f(inp["fox_b_f"]),
        "gmlp_ln_g": f(inp["gmlp_ln_g"]), "gmlp_ln_b": f(inp["gmlp_ln_b"]),
        "gmlp_w_sT": f(np.transpose(np.asarray(inp["gmlp_w_s"]), (0, 1, 3, 2))),
        "gmlp_b_s": f(np.asarray(inp["gmlp_b_s"]).reshape(DEPTH, 512)),
        "hgrn_lb_logits": f(inp["hgrn_lb_logits"]), "hgrn_norm_g": f(inp["hgrn_norm_g"]),
        "diff_lambda": f(np.asarray(inp["diff_lambda"]).reshape(DEPTH, 256)), "diff_norm_g": f(inp["diff_norm_g"]),
        "w_branch": f(inp["w_branch"]), "w_gate": f(inp["w_gate"]), "b_gate": f(inp["b_gate"]),
        "w_out": f(inp["w_out"]), "norm_ffn_g": f(inp["norm_ffn_g"]), "ffn_w_up": f(inp["ffn_w_up"]),
        "ffn_conv_w": f(inp["ffn_conv_w"]), "ffn_conv_b": f(inp["ffn_conv_b"]), "ffn_w_down": f(inp["ffn_w_down"]),
        "norm_final_g": f(inp["norm_final_g"]),
    }
    m.update(_constants())
    return m


def kernel(**inputs):
    x = np.ascontiguousarray(np.asarray(inputs["x"], dtype=np.float32))
    shared = _prep_shared(inputs)
    NS = x.shape[0] // NCORES
    nc = build_nc(NS=NS, NLAY=DEPTH, dbg=False)
    in_maps = []
    for c in range(NCORES):
        m = dict(shared)
        m["x"] = np.ascontiguousarray(x[c * NS:(c + 1) * NS])
        in_maps.append(m)
    res = run_bass_kernel_spmd(nc, in_maps, core_ids=list(range(NCORES)))
    return np.concatenate([np.asarray(r["out"], dtype=np.float32) for r in res.results], axis=0)
```
